# Optimizing a Trainium2 kernel written in Bass

```python
import math
import jax, jax.numpy as jnp
from jax import lax
import numpy as np

D_MODEL = 1024
BATCH = 16
SEQ = 4096
DEPTH = 4

CHUNK = 64
N_META = 16
Q_BLOCK = 128
DA_HEADS = D_MODEL // 128
DA_HEAD_DIM = 32
DA_WIDTH = DA_HEADS * 2 * DA_HEAD_DIM
LRU_WIDTH = D_MODEL
LRU_HEADS = D_MODEL // 64
LRU_BLOCK = LRU_WIDTH // LRU_HEADS
CONV_WIDTH = 4
LRU_C = 8.0
D_FF = 256 * ((8 * D_MODEL // 3 + 255) // 256)
N_EXPERTS = 8
TOP_K = 2
N_DENSE = (DEPTH + 1) // 2
N_MOE = DEPTH // 2
ALPHA = (2 * DEPTH) ** 0.25
BETA = (8 * DEPTH) ** -0.25
LN_EPS = 1e-5
RMS_EPS = 1e-5
PROJ_WIDTH = 3 * DA_WIDTH + 2 * LRU_WIDTH + 2 * D_MODEL
PROJ_SPLITS = (DA_WIDTH, 2 * DA_WIDTH, 3 * DA_WIDTH, 3 * DA_WIDTH + LRU_WIDTH,
               3 * DA_WIDTH + 2 * LRU_WIDTH, 3 * DA_WIDTH + 2 * LRU_WIDTH + D_MODEL)

kernel_name = 'hybrid_diffattn_rglru_moe_deepnorm'


def layer_norm(x, g, b):
    xf = x.astype(jnp.float32)
    mu = jnp.mean(xf, axis=-1, keepdims=True)
    var = jnp.mean(jnp.square(xf - mu), axis=-1, keepdims=True)
    y = (xf - mu) * lax.rsqrt(var + LN_EPS)
    return (y * g.astype(jnp.float32) + b.astype(jnp.float32)).astype(x.dtype)


def diff_attention(q, k, v, lam, lam_init, subln_g):
    B, T = q.shape[0], q.shape[1]
    q = q * (DA_HEAD_DIM ** -0.5)

    def attend(qb, kb, vb, mask):
        s = jnp.einsum('bqhmd,bkhmd->bhmqk', qb, kb).astype(jnp.float32)
        if mask is not None:
            s = jnp.where(mask, s, -jnp.inf)
        p = jax.nn.softmax(s, axis=-1)
        a = p[:, :, 0] - lam * p[:, :, 1]
        return jnp.einsum('bhqk,bkhe->bqhe', a.astype(vb.dtype), vb)

    outs = [attend(q[:, :N_META], k[:, :N_META], v[:, :N_META], None)]
    cidx = np.arange(Q_BLOCK) // CHUNK
    diag = jnp.asarray(cidx[None, :] <= cidx[:, None])
    n_blocks = (T - N_META) // Q_BLOCK
    for blk in range(n_blocks):
        q0 = N_META + blk * Q_BLOCK
        k_end = q0 + Q_BLOCK
        mask = jnp.concatenate([jnp.ones((Q_BLOCK, q0), dtype=bool), diag], axis=1)
        outs.append(attend(q[:, q0:k_end], k[:, :k_end], v[:, :k_end], mask))
    o = jnp.concatenate(outs, axis=1)
    of = o.astype(jnp.float32)
    of = of * lax.rsqrt(jnp.mean(jnp.square(of), axis=-1, keepdims=True) + RMS_EPS)
    of = of * subln_g.astype(jnp.float32) * (1.0 - lam_init)
    return of.astype(o.dtype).reshape(B, T, DA_WIDTH)


def causal_depthwise_conv(x, w, b):
    T = x.shape[1]
    xp = jnp.pad(x, ((0, 0), (CONV_WIDTH - 1, 0), (0, 0)))
    y = xp[:, 0:T] * w[0]
    for j in range(1, CONV_WIDTH):
        y = y + xp[:, j:j + T] * w[j]
    return y + b


def rg_lru(x, w_r, b_r, w_i, b_i, lam_param):
    B, T, C = x.shape
    xb = x.reshape(B, T, LRU_HEADS, LRU_BLOCK)
    r = jax.nn.sigmoid((jnp.einsum('bthi,hij->bthj', xb, w_r).reshape(B, T, C) + b_r).astype(jnp.float32))
    i = jax.nn.sigmoid((jnp.einsum('bthi,hij->bthj', xb, w_i).reshape(B, T, C) + b_i).astype(jnp.float32))
    log_a = -LRU_C * r * jax.nn.softplus(-lam_param.astype(jnp.float32))
    a = jnp.exp(log_a)
    mult = jnp.sqrt(-jnp.expm1(2.0 * log_a))
    u = mult * i * x.astype(jnp.float32)

    def combine(left, right):
        a_l, u_l = left
        a_r, u_r = right
        return a_l * a_r, a_r * u_l + u_r

    _, h = lax.associative_scan(combine, (a, u), axis=1)
    return h.astype(x.dtype)


def token_mixer(h, layer, w_in, b_gate, da_lambda, da_subln_g, conv_w, conv_b,
                lru_wr, lru_br, lru_wi, lru_bi, lru_lambda, w_attn_out, w_lru_out, w_o):
    B, T, _ = h.shape
    z = h @ w_in
    q, k, v, xr, gr, ga, gl = jnp.split(z, PROJ_SPLITS, axis=-1)
    q = q.reshape(B, T, DA_HEADS, 2, DA_HEAD_DIM)
    k = k.reshape(B, T, DA_HEADS, 2, DA_HEAD_DIM)
    v = v.reshape(B, T, DA_HEADS, 2 * DA_HEAD_DIM)
    lam_init = 0.8 - 0.6 * math.exp(-0.3 * layer)
    lf = da_lambda.astype(jnp.float32)
    lam = jnp.exp(jnp.sum(lf[0] * lf[1])) - jnp.exp(jnp.sum(lf[2] * lf[3])) + lam_init
    attn_up = diff_attention(q, k, v, lam, lam_init, da_subln_g) @ w_attn_out
    xr = causal_depthwise_conv(xr, conv_w, conv_b)
    rec = rg_lru(xr, lru_wr, lru_br, lru_wi, lru_bi, lru_lambda) * jax.nn.gelu(gr)
    lru_up = rec @ w_lru_out
    merged = jax.nn.sigmoid(ga + b_gate[0]) * attn_up + jax.nn.sigmoid(gl + b_gate[1]) * lru_up
    return merged @ w_o


def swiglu(h, w_gate, w_up, w_down):
    return (jax.nn.silu(h @ w_gate) * (h @ w_up)) @ w_down


def moe_swiglu(h, w_router, w_gate, w_up, w_down):
    logits = (h @ w_router).astype(jnp.float32)
    top_vals, top_idx = lax.top_k(logits, TOP_K)
    top_w = jax.nn.softmax(top_vals, axis=-1)
    comb = jnp.sum(jax.nn.one_hot(top_idx, N_EXPERTS, dtype=jnp.float32) * top_w[..., None], axis=-2)
    out = jnp.zeros_like(h)
    for e in range(N_EXPERTS):
        out = out + comb[..., e:e + 1].astype(h.dtype) * swiglu(h, w_gate[e], w_up[e], w_down[e])
    return out


def setup_inputs(seed: int = 0) -> dict:
    key = jax.random.key(seed)
    ks = jax.random.split(key, 32)
    f32 = jnp.float32
    nrm = lambda k, shape, s: jax.random.normal(k, shape, f32) * s
    u = jax.random.uniform(ks[13], (DEPTH, LRU_WIDTH), f32, 0.9, 0.999)
    sig = u ** (1.0 / LRU_C)
    lru_lambda = jnp.log(sig) - jnp.log1p(-sig)
    return {
        'x': nrm(ks[0], (BATCH, SEQ, D_MODEL), 1.0),
        'meta_tokens': nrm(ks[1], (N_META, D_MODEL), 1.0),
        'ln_in_g': 1.0 + nrm(ks[2], (D_MODEL,), 0.02),
        'ln_in_b': nrm(ks[3], (D_MODEL,), 0.02),
        'w_in': nrm(ks[4], (DEPTH, D_MODEL, PROJ_WIDTH), D_MODEL ** -0.5),
        'b_gate': nrm(ks[5], (DEPTH, 2, D_MODEL), 0.02),
        'da_lambda': nrm(ks[6], (DEPTH, 4, DA_HEAD_DIM), 0.1),
        'da_subln_g': 1.0 + nrm(ks[7], (DEPTH, 2 * DA_HEAD_DIM), 0.02),
        'conv_w': nrm(ks[8], (DEPTH, CONV_WIDTH, LRU_WIDTH), CONV_WIDTH ** -0.5),
        'conv_b': nrm(ks[9], (DEPTH, LRU_WIDTH), 0.02),
        'lru_wr': nrm(ks[10], (DEPTH, LRU_HEADS, LRU_BLOCK, LRU_BLOCK), LRU_BLOCK ** -0.5),
        'lru_br': nrm(ks[11], (DEPTH, LRU_WIDTH), 0.02),
        'lru_wi': nrm(ks[12], (DEPTH, LRU_HEADS, LRU_BLOCK, LRU_BLOCK), LRU_BLOCK ** -0.5),
        'lru_bi': nrm(ks[14], (DEPTH, LRU_WIDTH), 0.02),
        'lru_lambda': lru_lambda,
        'w_attn_out': nrm(ks[15], (DEPTH, DA_WIDTH, D_MODEL), DA_WIDTH ** -0.5 * BETA),
        'w_lru_out': nrm(ks[16], (DEPTH, LRU_WIDTH, D_MODEL), LRU_WIDTH ** -0.5 * BETA),
        'w_o': nrm(ks[17], (DEPTH, D_MODEL, D_MODEL), D_MODEL ** -0.5 * BETA),
        'ln_g': 1.0 + nrm(ks[18], (DEPTH, 2, D_MODEL), 0.02),
        'ln_b': nrm(ks[19], (DEPTH, 2, D_MODEL), 0.02),
        'ffn_wg': nrm(ks[20], (N_DENSE, D_MODEL, D_FF), D_MODEL ** -0.5),
        'ffn_wu': nrm(ks[21], (N_DENSE, D_MODEL, D_FF), D_MODEL ** -0.5),
        'ffn_wd': nrm(ks[22], (N_DENSE, D_FF, D_MODEL), D_FF ** -0.5 * BETA),
        'router_w': nrm(ks[23], (N_MOE, D_MODEL, N_EXPERTS), D_MODEL ** -0.5),
        'moe_wg': nrm(ks[24], (N_MOE, N_EXPERTS, D_MODEL, D_FF), D_MODEL ** -0.5),
        'moe_wu': nrm(ks[25], (N_MOE, N_EXPERTS, D_MODEL, D_FF), D_MODEL ** -0.5),
        'moe_wd': nrm(ks[26], (N_MOE, N_EXPERTS, D_FF, D_MODEL), D_FF ** -0.5 * BETA),
    }


def reference(x, meta_tokens, ln_in_g, ln_in_b, w_in, b_gate, da_lambda, da_subln_g,
              conv_w, conv_b, lru_wr, lru_br, lru_wi, lru_bi, lru_lambda,
              w_attn_out, w_lru_out, w_o, ln_g, ln_b, ffn_wg, ffn_wu, ffn_wd,
              router_w, moe_wg, moe_wu, moe_wd):
    B = x.shape[0]
    meta = jnp.broadcast_to(meta_tokens[None].astype(x.dtype), (B, N_META, D_MODEL))
    h = layer_norm(jnp.concatenate([meta, x], axis=1), ln_in_g, ln_in_b)
    for i in range(DEPTH):
        mix = token_mixer(h, i, w_in[i], b_gate[i], da_lambda[i], da_subln_g[i],
                          conv_w[i], conv_b[i], lru_wr[i], lru_br[i], lru_wi[i], lru_bi[i],
                          lru_lambda[i], w_attn_out[i], w_lru_out[i], w_o[i])
        h = layer_norm(ALPHA * h + mix, ln_g[i, 0], ln_b[i, 0])
        if i % 2 == 0:
            f = swiglu(h, ffn_wg[i // 2], ffn_wu[i // 2], ffn_wd[i // 2])
        else:
            f = moe_swiglu(h, router_w[i // 2], moe_wg[i // 2], moe_wu[i // 2], moe_wd[i // 2])
        h = layer_norm(ALPHA * h + f, ln_g[i, 1], ln_b[i, 1])
    return h[:, N_META:]
```

```python
import math
import numpy as np
import concourse.bass as bass
import concourse.mybir as mybir
from concourse.bass_utils import run_bass_kernel_spmd

F32 = mybir.dt.float32
BF16 = mybir.dt.bfloat16
AF = mybir.ActivationFunctionType
ALU = mybir.AluOpType
AX = mybir.AxisListType

D = 1024
NM = 16
DA = 512
FF = 2816
NE = 8
PW = 5632
LN_EPS = 1e-5
RMS_EPS = 1e-5
NFT = FF // 128


class Reg:
    __slots__ = ("name", "w", "r", "dsem", "dcnt")

    def __init__(self, name):
        self.name = name
        self.w = None
        self.r = {}
        self.dsem = None
        self.dcnt = 0


class V:
    __slots__ = ("ap", "regs", "kind")

    def __init__(self, ap, regs, kind):
        self.ap = ap
        self.regs = regs
        self.kind = kind


class Buf:
    def __init__(self, name, handle, nreg, kind):
        self.kind = kind
        self.name = name
        self.h = handle
        self.regs = [Reg(f"{name}_{i}") for i in range(nreg)]

    def __getitem__(self, idx):
        return V(self.h[idx], self.regs, self.kind)

    def v(self, ap, ridx=None):
        if ridx is None:
            return V(ap, self.regs, self.kind)
        if isinstance(ridx, int):
            ridx = (ridx,)
        return V(ap, [self.regs[i] for i in ridx], self.kind)

    def p(self, ridx, idx):
        if isinstance(ridx, int):
            ridx = (ridx,)
        return V(self.h[idx], [self.regs[i] for i in ridx], self.kind)


WRITE_KW = ("out", "accum_out", "ap")


class FW:
    ENG = ("pe", "act", "dve", "pool", "sp")

    def __init__(self, nc):
        self.nc = nc
        self.eng = {"pe": nc.tensor, "act": nc.scalar, "dve": nc.vector,
                    "pool": nc.gpsimd, "sp": nc.sync}
        self.sem = {}
        self.cnt = {}
        self.known = {}
        for e in self.ENG:
            self.sem[e] = nc.alloc_semaphore(name=f"s_{e}")
            self.cnt[e] = 0
            self.known[e] = {}
        self.nsem = len(self.ENG)
        self.nwaits = 0
        self.ninst = 0

    def sb(self, name, shape, dtype, nreg=1):
        return Buf(name, self.nc.alloc_sbuf_tensor(name, list(shape), dtype), nreg, "sb")

    def ps(self, name, shape, dtype=F32, nreg=1):
        return Buf(name, self.nc.alloc_psum_tensor(name, list(shape), dtype), nreg, "ps")

    def dram(self, name, shape, dtype, kind="Internal", nreg=1):
        return Buf(name, self.nc.dram_tensor(name, list(shape), dtype, kind=kind), nreg, "dram")

    def newsem(self, name):
        self.sem[name] = self.nc.alloc_semaphore(name=name)
        self.nsem += 1
        return name

    def _wait(self, e, key, val):
        kn = self.known[e]
        if kn.get(key, 0) >= val:
            return
        if key == e and e == "pe":
            return
        kn[key] = val
        self.eng[e].wait_ge(self.sem[key], val)
        self.nwaits += 1

    def _deps(self, e, reads, writes):
        for v in reads:
            for r in v.regs:
                if r.w is not None:
                    self._wait(e, r.w[0], r.w[1])
        for v in writes:
            for r in v.regs:
                if r.w is not None:
                    self._wait(e, r.w[0], r.w[1])
                for k, val in r.r.items():
                    self._wait(e, k, val)

    def _mark(self, ev, reads, writes):
        for v in reads:
            for r in v.regs:
                r.r[ev[0]] = ev[1]
        for v in writes:
            for r in v.regs:
                r.w = ev
                r.r = {}

    def op(self, e, fn, *, reads=(), writes=(), **kw):
        rd = list(reads)
        wr = list(writes)
        args = {}
        for k, v in kw.items():
            if isinstance(v, V):
                (wr if k in WRITE_KW else rd).append(v)
                args[k] = v.ap
            else:
                args[k] = v
        self._deps(e, rd, wr)
        ins = getattr(self.eng[e], fn)(**args)
        self.cnt[e] += 1
        ins.then_inc(self.sem[e], 1)
        self._mark((e, self.cnt[e]), rd, wr)
        self.ninst += 1
        return ins

    def dma(self, q, out, in_, sem_of=None, serialize=True, **kw):
        owner = sem_of
        if owner is None:
            for v in (out, in_):
                if v.kind == "sb":
                    owner = v.regs[0]
                    break
        if owner is None:
            owner = out.regs[0]
        if owner.dsem is None:
            owner.dsem = self.newsem(f"d_{owner.name}")
        self._deps(q, [in_], [out])
        if serialize:
            self._wait(q, owner.dsem, owner.dcnt)
        ins = self.eng[q].dma_start(out=out.ap, in_=in_.ap, **kw)
        owner.dcnt += 16
        ins.then_inc(self.sem[owner.dsem], 16)
        self._mark((owner.dsem, owner.dcnt), [in_], [out])
        self.ninst += 1
        return ins

    def finish(self, outs, q="sp"):
        for v in outs:
            for r in v.regs:
                if r.w is not None:
                    self._wait(q, r.w[0], r.w[1])


WNAMES = ["x", "meta_tokens", "ln_in_g", "ln_in_b", "w_in", "b_gate", "da_lambda",
          "da_subln_g", "conv_w", "conv_b", "lru_wr", "lru_br", "lru_wi", "lru_bi",
          "lru_lambda", "w_attn_out", "w_lru_out", "w_o", "ln_g", "ln_b", "ffn_wg",
          "ffn_wu", "ffn_wd", "router_w", "moe_wg", "moe_wu", "moe_wd"]


def build_program(n_seq, seq, depth, alpha, dbg_layers=False, stop_after=None, tap=None):
    nc = bass.Bass("TRN2", target_bir_lowering=False)
    fw = FW(nc)
    L = depth
    Nd = (L + 1) // 2
    Nm = L // 2
    T = NM + seq
    NX = seq // 512
    assert seq % 512 == 0
    tiles = [(0, NM)] + [(NM + 512 * i, 512) for i in range(NX)]
    NTL = len(tiles)
    NKT = 1 + seq // 128

    shapes = {
        "x": [n_seq, seq, D], "meta_tokens": [NM, D], "ln_in_g": [D], "ln_in_b": [D],
        "w_in": [L, D, PW], "b_gate": [L, 2, D], "da_lambda": [L, 4, 32],
        "da_subln_g": [L, 64], "conv_w": [L, 4, D], "conv_b": [L, D],
        "lru_wr": [L, 16, 64, 64], "lru_br": [L, D], "lru_wi": [L, 16, 64, 64],
        "lru_bi": [L, D], "lru_lambda": [L, D], "w_attn_out": [L, DA, D],
        "w_lru_out": [L, D, D], "w_o": [L, D, D], "ln_g": [L, 2, D], "ln_b": [L, 2, D],
        "ffn_wg": [Nd, D, FF], "ffn_wu": [Nd, D, FF], "ffn_wd": [Nd, FF, D],
        "router_w": [max(Nm, 1), D, NE], "moe_wg": [max(Nm, 1), NE, D, FF],
        "moe_wu": [max(Nm, 1), NE, D, FF], "moe_wd": [max(Nm, 1), NE, FF, D],
    }
    I = {k: fw.dram(k, shapes[k], F32, kind="ExternalInput") for k in WNAMES}
    OUT = fw.dram("out", [n_seq, seq, D], F32, kind="ExternalOutput", nreg=n_seq * NTL)
    H32 = fw.dram("h32", [n_seq, D, T], F32, nreg=n_seq * NTL)
    HB = fw.dram("hb", [n_seq, D, T], BF16, nreg=n_seq * NTL)
    DBG = None
    if dbg_layers:
        DBG = fw.dram("dbg", [L + 1, n_seq, D, T], F32, kind="ExternalOutput")
    TAPS = {}

    def tapout(name, view, shape, dtype):
        if tap is None:
            return
        if name not in TAPS:
            TAPS[name] = fw.dram("tap_" + name, shape, dtype, kind="ExternalOutput")
        fw.dma("sp", TAPS[name][:], view)
    WB = {
        "w_in": fw.dram("wb_in", [L, D, PW], BF16, nreg=L),
        "w_attn_out": fw.dram("wb_ao", [L, DA, D], BF16, nreg=L),
        "w_lru_out": fw.dram("wb_lo", [L, D, D], BF16, nreg=L),
        "w_o": fw.dram("wb_o", [L, D, D], BF16, nreg=L),
        "ffn_wg": fw.dram("wb_fg", [Nd, D, FF], BF16, nreg=Nd),
        "ffn_wu": fw.dram("wb_fu", [Nd, D, FF], BF16, nreg=Nd),
        "ffn_wd": fw.dram("wb_fd", [Nd, FF, D], BF16, nreg=Nd),
        "moe_wg": fw.dram("wb_mg", [max(Nm, 1), NE, D, FF], BF16, nreg=max(Nm, 1) * NE),
        "moe_wu": fw.dram("wb_mu", [max(Nm, 1), NE, D, FF], BF16, nreg=max(Nm, 1) * NE),
        "moe_wd": fw.dram("wb_md", [max(Nm, 1), NE, FF, D], BF16, nreg=max(Nm, 1) * NE),
    }

    KT = fw.sb("KT", [128, 4, T], BF16)
    VR = fw.sb("VR", [128, NKT, 8, 65], BF16)
    Y = fw.sb("Y", [128, 8, 512], F32, nreg=8)
    XB = fw.sb("XB", [128, 8, 512], BF16, nreg=8)
    AT = fw.sb("AT", [128, NFT, 512], BF16, nreg=NFT)
    MT = fw.sb("MT", [128, 8, 512], BF16, nreg=8)
    NF = 8
    Fp = [fw.sb(f"F{i}", [128, 512], F32) for i in range(NF + 1)]
    LNR = fw.sb("LNR", [128, 512], F32)
    CBT = fw.sb("CBT", [128, 512], F32)
    XR = [fw.sb(f"XR{i}", [128, 516], BF16) for i in range(2)]
    PTb = [fw.sb(f"PT{i}", [128, 2, 512], BF16) for i in range(3)]
    NSLOT = 8
    RING = [fw.sb(f"RG{i}", [128, 2048], BF16) for i in range(NSLOT)]
    XL = [fw.sb("XL0", [128, 1024], F32)] * 2
    WRB = fw.sb("WRB", [128, 8, 128], F32)
    WIB = fw.sb("WIB", [128, 8, 128], F32)
    CD = fw.sb("CD", [128, 8, 4, 128], BF16)
    WRT = fw.sb("WRT", [128, 8, NE], F32)
    PTB = fw.sb("PTB", [128, L, 112], F32)
    LNIN = fw.sb("LNIN", [128, 16], F32)
    CNEG = fw.sb("CNEG", [128, L, 8], F32)
    STG = fw.sb("STG", [128, 128], F32)
    IDF = fw.sb("IDF", [128, 128], F32)
    IDB = fw.sb("IDB", [128, 128], BF16)
    ONESD = fw.sb("ONESD", [128, 128], F32)
    ONE64 = fw.sb("ONE64", [128, 128], F32)
    HIST = fw.sb("HIST", [128, 8, 4], BF16)
    HST = fw.sb("HST", [128, 8], F32)
    LAMW = fw.sb("LAMW", [128, 4, 32], F32)
    LAMS = fw.sb("LAMS", [128, 8], F32)
    NLAM = fw.sb("NLAM", [128, L], F32)
    GS = fw.sb("GS", [64, L], F32)
    LG = fw.sb("LG", [128, 4, NE], F32)
    M8 = fw.sb("M8", [128, 4, 8], F32)
    CMB = fw.sb("CMB", [128, 4, NE], F32)
    SM = fw.sb("SM", [128, 16], F32)

    PS = [fw.ps(f"PS{i}", [128, 2, 512], F32, nreg=2) for i in range(4)]
    bank_rr = [0]

    def bank(b):
        return PS[b // 2], b % 2

    def next_bank():
        b = bank_rr[0]
        bank_rr[0] = (b + 1) % 8
        return b

    def bv(b, pr, cols):
        buf, hf = bank(b)
        return buf.p(hf, (pr, hf, cols))

    f_rr = [0]

    def Fn():
        i = f_rr[0]
        f_rr[0] = (i + 1) % NF
        return Fp[i]

    ALLP = slice(0, 128)

    fw.op("pool", "memset", ap=IDF[:], constant=0.0)
    fw.op("pool", "affine_select", out=IDF[:], in_=IDF[:], pattern=[[-1, 128]],
          compare_op=ALU.not_equal, fill=1.0, base=0, channel_multiplier=1)
    fw.op("pool", "tensor_copy", out=IDB[:], in_=IDF[:])
    fw.op("pool", "memset", ap=ONESD[:], constant=1.0 / D)
    fw.op("pool", "memset", ap=ONE64[:], constant=1.0)
    fw.op("pool", "memset", ap=WRB[:], constant=0.0)
    fw.op("pool", "memset", ap=WIB[:], constant=0.0)
    fw.op("pool", "memset", ap=VR[:], constant=1.0)

    def stage_rows(dst_v, loads):
        r0 = 0
        for src_ap, nrows in loads:
            fw.dma("sp", STG[r0:r0 + nrows, :], src_ap)
            r0 += nrows
        b = next_bank()
        fw.op("pe", "transpose", out=bv(b, ALLP, slice(0, r0)), in_=STG[0:r0, :], identity=IDF[0:r0, 0:r0])
        fw.op("dve", "tensor_copy", out=dst_v, in_=bv(b, ALLP, slice(0, r0)))

    def rows(buf, ap, n):
        return (buf.v(ap.rearrange("(r p) -> r p", p=128)), n)

    for l in range(L):
        loads = [
            (I["b_gate"].v(I["b_gate"].h[l].rearrange("i (r p) -> (i r) p", p=128)), 16),
            (I["conv_w"].v(I["conv_w"].h[l].rearrange("i (r p) -> (i r) p", p=128)), 32),
            rows(I["conv_b"], I["conv_b"].h[l], 8),
            rows(I["lru_br"], I["lru_br"].h[l], 8),
            rows(I["lru_bi"], I["lru_bi"].h[l], 8),
            rows(I["lru_lambda"], I["lru_lambda"].h[l], 8),
            (I["ln_g"].v(I["ln_g"].h[l].rearrange("i (r p) -> (i r) p", p=128)), 16),
            (I["ln_b"].v(I["ln_b"].h[l].rearrange("i (r p) -> (i r) p", p=128)), 16),
        ]
        stage_rows(PTB[:, l, :], loads)
    stage_rows(LNIN[:, :], [rows(I["ln_in_g"], I["ln_in_g"].h[:], 8), rows(I["ln_in_b"], I["ln_in_b"].h[:], 8)])
    C_BG, C_CW, C_CB, C_BR, C_BI, C_LAM, C_LNG, C_LNB = 0, 16, 48, 56, 64, 72, 80, 96

    def pcol(l, c):
        return PTB[:, l, c:c + 1]

    for l in range(L):
        t = Fn()
        fw.op("act", "activation", out=t[:, 0:8], in_=PTB[:, l, C_LAM:C_LAM + 8], func=AF.Exp, scale=-1.0)
        fw.op("act", "activation", out=t[:, 8:16], in_=t[:, 0:8], func=AF.Ln, bias=1.0)
        fw.op("dve", "tensor_scalar", out=CNEG[:, l, :], in0=t[:, 8:16], scalar1=-8.0, scalar2=None, op0=ALU.mult)
    for l in range(L):
        lam_init = 0.8 - 0.6 * math.exp(-0.3 * l)
        src = bass.AP(I["da_lambda"].h, l * 128, [[0, 128], [1, 128]])
        fw.dma("sp", LAMW.v(LAMW.h[:].rearrange("p a b -> p (a b)")), I["da_lambda"].v(src))
        fw.op("dve", "tensor_tensor", out=LAMW[:, 0, :], in0=LAMW[:, 0, :], in1=LAMW[:, 1, :], op=ALU.mult)
        fw.op("dve", "tensor_tensor", out=LAMW[:, 2, :], in0=LAMW[:, 2, :], in1=LAMW[:, 3, :], op=ALU.mult)
        fw.op("dve", "reduce_sum", out=LAMS[:, 0:1], in_=LAMW[:, 0, :], axis=AX.X)
        fw.op("dve", "reduce_sum", out=LAMS[:, 1:2], in_=LAMW[:, 2, :], axis=AX.X)
        fw.op("act", "activation", out=LAMS[:, 2:4], in_=LAMS[:, 0:2], func=AF.Exp)
        fw.op("dve", "tensor_tensor", out=LAMS[:, 4:5], in0=LAMS[:, 3:4], in1=LAMS[:, 2:3], op=ALU.subtract)
        fw.op("dve", "tensor_scalar", out=NLAM[:, l:l + 1], in0=LAMS[:, 4:5], scalar1=-lam_init, scalar2=None, op0=ALU.add)
        fw.dma("sp", GS[:, l:l + 1], I["da_subln_g"].v(I["da_subln_g"].h[l].rearrange("(p o) -> p o", o=1)))
        fw.op("dve", "tensor_scalar", out=GS[:, l:l + 1], in0=GS[:, l:l + 1], scalar1=1.0 - lam_init, scalar2=None, op0=ALU.mult)

    cast_ev = []
    CAST_INFLIGHT = 2

    def cast2d(dst_buf, dst_ap, src_buf, src_ap, reg, rows_, cols):
        c = cols
        for cand in (2048, 1408, 1024, 512):
            if cols % cand == 0:
                c = cand
                break
        rstep = 256 if rows_ % 256 == 0 and cols > 2048 else rows_
        for r0 in range(0, rows_, rstep):
            d = dst_ap[r0:r0 + rstep, :].rearrange("r (a c) -> r a c", c=c)
            s = src_ap[r0:r0 + rstep, :].rearrange("r (a c) -> r a c", c=c)
            if len(cast_ev) >= CAST_INFLIGHT:
                fw._wait("pool", *cast_ev[-CAST_INFLIGHT])
            fw.dma("pool", dst_buf.v(d, reg), src_buf.v(s), sem_of=dst_buf.regs[reg], serialize=False)
            cast_ev.append(dst_buf.regs[reg].w)

    def cast_layer_mixer(l):
        cast2d(WB["w_in"], WB["w_in"].h[l], I["w_in"], I["w_in"].h[l], l, D, PW)
        cast2d(WB["w_attn_out"], WB["w_attn_out"].h[l], I["w_attn_out"], I["w_attn_out"].h[l], l, DA, D)
        cast2d(WB["w_lru_out"], WB["w_lru_out"].h[l], I["w_lru_out"], I["w_lru_out"].h[l], l, D, D)
        cast2d(WB["w_o"], WB["w_o"].h[l], I["w_o"], I["w_o"].h[l], l, D, D)

    def cast_layer_ffn(l):
        if l % 2 == 0:
            i = l // 2
            cast2d(WB["ffn_wg"], WB["ffn_wg"].h[i], I["ffn_wg"], I["ffn_wg"].h[i], i, D, FF)
            cast2d(WB["ffn_wu"], WB["ffn_wu"].h[i], I["ffn_wu"], I["ffn_wu"].h[i], i, D, FF)
            cast2d(WB["ffn_wd"], WB["ffn_wd"].h[i], I["ffn_wd"], I["ffn_wd"].h[i], i, FF, D)
        else:
            i = l // 2
            for e in range(NE):
                cast2d(WB["moe_wg"], WB["moe_wg"].h[i, e], I["moe_wg"], I["moe_wg"].h[i, e], i * NE + e, D, FF)
                cast2d(WB["moe_wu"], WB["moe_wu"].h[i, e], I["moe_wu"], I["moe_wu"].h[i, e], i * NE + e, D, FF)
                cast2d(WB["moe_wd"], WB["moe_wd"].h[i, e], I["moe_wd"], I["moe_wd"].h[i, e], i * NE + e, FF, D)

    for l in range(L):
        cast_layer_mixer(l)
        cast_layer_ffn(l)

    class WStream:
        def __init__(self):
            self.plan = []
            self.issued = 0
            self.taken = 0

        def add(self, specs):
            self.plan.extend(specs)

        def _issue(self, i):
            buf, reg, ap, shape = self.plan[i]
            slot = RING[i % NSLOT]
            n = 1
            for s_ in shape[1:]:
                n *= s_
            dst = slot.h[0:shape[0], 0:n]
            if len(shape) == 3:
                dst = dst.rearrange("p (a b) -> p a b", b=shape[2])
            fw.dma("sp", slot.v(dst), buf.v(ap, reg))

        def get(self):
            i = self.taken
            assert i < len(self.plan), "weight plan exhausted"
            while self.issued < min(len(self.plan), i + NSLOT - 3):
                self._issue(self.issued)
                self.issued += 1
            self.taken += 1
            buf, reg, ap, shape = self.plan[i]
            slot = RING[i % NSLOT]
            n = 1
            for s_ in shape[1:]:
                n *= s_
            v = slot.h[0:shape[0], 0:n]
            if len(shape) == 3:
                v = v.rearrange("p (a b) -> p a b", b=shape[2])
            return slot.v(v)

    wq = WStream()

    def kcols(buf, reg, mat_ap, c0, ncol, krows=128):
        ap = mat_ap[:, c0:c0 + ncol].rearrange("(k p) c -> p k c", p=krows)
        return (buf, reg, ap, [krows, ap.shape[1], ncol])

    def chunks_for(l):
        sp = []
        win = WB["w_in"].h[l]
        for ci in range(4):
            sp.append(kcols(WB["w_in"], l, win, 256 * ci, 256))
        for ci in range(2):
            sp.append(kcols(WB["w_in"], l, win, 1024 + 256 * ci, 256))
        for cp in range(4):
            sp.append(kcols(WB["w_in"], l, win, 1536 + 256 * cp, 256))
            sp.append(kcols(WB["w_in"], l, win, 2560 + 256 * cp, 256))
        for dp in range(4):
            sp.append(kcols(WB["w_attn_out"], l, WB["w_attn_out"].h[l], 256 * dp, 256, krows=64))
            sp.append(kcols(WB["w_lru_out"], l, WB["w_lru_out"].h[l], 256 * dp, 256))
            sp.append(kcols(WB["w_in"], l, win, 3584 + 256 * dp, 256))
            sp.append(kcols(WB["w_in"], l, win, 4608 + 256 * dp, 256))
        for dp in range(4):
            sp.append(kcols(WB["w_o"], l, WB["w_o"].h[l], 256 * dp, 256))
        i = l // 2
        if l % 2 == 0:
            exps = [(WB["ffn_wg"], WB["ffn_wu"], WB["ffn_wd"], i, WB["ffn_wg"].h[i], WB["ffn_wu"].h[i], WB["ffn_wd"].h[i])]
        else:
            exps = [(WB["moe_wg"], WB["moe_wu"], WB["moe_wd"], i * NE + e, WB["moe_wg"].h[i, e],
                     WB["moe_wu"].h[i, e], WB["moe_wd"].h[i, e]) for e in range(NE)]
        for (bg_, bu_, bd_, reg, g_ap, u_ap, d_ap) in exps:
            for fp_ in range(NFT // 2):
                sp.append(kcols(bg_, reg, g_ap, 256 * fp_, 256))
                sp.append(kcols(bu_, reg, u_ap, 256 * fp_, 256))
            for fp_ in range(NFT // 2):
                ap = d_ap[256 * fp_:256 * fp_ + 256, :].rearrange("(f p) c -> p f c", p=128)
                sp.append((bd_, reg, ap, [128, 2, D]))
        return sp

    def hview(buf, s, ti):
        t0, n = tiles[ti]
        ap = buf.h[s].rearrange("(k p) t -> p k t", p=128)[:, :, t0:t0 + n]
        return buf.v(ap, s * NTL + ti)

    def ln_fm(n, gcol, bcol, eps):
        bA = next_bank()
        bB = next_bank()
        for k in range(8):
            sq = Fn()
            fw.op("act", "activation", out=sq[:, 0:n], in_=Y.p(k, (ALLP, k, slice(0, n))), func=AF.Square)
            fw.op("pe", "matmul", out=bv(bA, ALLP, slice(0, n)), lhsT=ONESD[:], rhs=Y.p(k, (ALLP, k, slice(0, n))),
                  start=(k == 0), stop=(k == 7))
            fw.op("pe", "matmul", out=bv(bB, ALLP, slice(0, n)), lhsT=ONESD[:], rhs=sq[:, 0:n],
                  start=(k == 0), stop=(k == 7))
        msq = Fn()
        fw.op("act", "activation", out=msq[:, 0:n], in_=bv(bA, ALLP, slice(0, n)), func=AF.Square)
        var = Fn()
        fw.op("dve", "tensor_tensor", out=var[:, 0:n], in0=bv(bB, ALLP, slice(0, n)), in1=msq[:, 0:n], op=ALU.subtract)
        fw.op("dve", "tensor_scalar", out=var[:, 0:n], in0=var[:, 0:n], scalar1=0.0, scalar2=eps, op0=ALU.max, op1=ALU.add)
        fw.op("act", "activation", out=var[:, 0:n], in_=var[:, 0:n], func=AF.Sqrt)
        rstd = LNR
        fw.op("dve", "reciprocal", out=rstd[:, 0:n], in_=var[:, 0:n])
        for k in range(8):
            t = Fn()
            yk = Y.p(k, (ALLP, k, slice(0, n)))
            fw.op("dve", "tensor_tensor", out=t[:, 0:n], in0=yk, in1=bv(bA, ALLP, slice(0, n)), op=ALU.subtract)
            fw.op("pool", "tensor_tensor", out=t[:, 0:n], in0=t[:, 0:n], in1=rstd[:, 0:n], op=ALU.mult)
            fw.op("act", "activation", out=yk, in_=t[:, 0:n], func=AF.Identity, scale=gcol(k), bias=bcol(k))
            fw.op("pool", "tensor_copy", out=XB.p(k, (ALLP, k, slice(0, n))), in_=yk)

    def store_h(s, ti, n):
        fw.dma("sp", hview(H32, s, ti), Y[:, :, 0:n])
        fw.dma("sp", hview(HB, s, ti), XB[:, :, 0:n])

    def dbg_store(li, s, ti, n):
        if DBG is not None:
            t0, _ = tiles[ti]
            ap = DBG.h[li, s].rearrange("(k p) t -> p k t", p=128)[:, :, t0:t0 + n]
            fw.dma("sp", DBG.v(ap), Y[:, :, 0:n])

    for s in range(n_seq):
        for ti, (t0, n) in enumerate(tiles):
            nsub = (n + 127) // 128
            for sub in range(nsub):
                ns = min(128, n - 128 * sub)
                xl = XL[sub % 2]
                if ti == 0:
                    fw.dma("sp", xl[0:ns, :], I["meta_tokens"][0:ns, :])
                else:
                    r0 = t0 - NM + 128 * sub
                    fw.dma("sp", xl[0:ns, :], I["x"].v(I["x"].h[s, r0:r0 + ns, :]))
                for half in range(2):
                    b = next_bank()
                    for q in range(4):
                        k = 4 * half + q
                        fw.op("pe", "transpose", out=bv(b, ALLP, slice(128 * q, 128 * q + ns)),
                              in_=xl[0:ns, 128 * k:128 * k + 128], identity=IDF[0:ns, 0:ns])
                    buf, hf = bank(b)
                    src = buf.p(hf, (ALLP, hf, slice(0, 512))).ap.rearrange("p (q t) -> p q t", t=128)[:, :, 0:ns]
                    dst = Y.h[:, 4 * half:4 * half + 4, 128 * sub:128 * sub + ns]
                    fw.op("dve", "tensor_copy", out=Y.v(dst, range(4 * half, 4 * half + 4)), in_=buf.v(src, hf))
            ln_fm(n, lambda k: LNIN[:, k:k + 1], lambda k: LNIN[:, 8 + k:9 + k], LN_EPS)
            store_h(s, ti, n)
            dbg_store(0, s, ti, n)

    QSCALE = 32 ** -0.5

    def layer_setup(l):
        for (dst, src) in ((WRB, I["lru_wr"]), (WIB, I["lru_wi"])):
            for hh in range(2):
                sap = src.h[l].rearrange("(c hh) i j -> hh i c j", hh=2)[hh]
                fw.dma("sp", dst[64 * hh:64 * hh + 64, :, 64 * hh:64 * hh + 64], src.v(sap))
        for c in range(8):
            for j in range(4):
                fw.op("pool", "tensor_scalar", out=CD[:, c, j, :], in0=IDB[:], scalar1=pcol(l, C_CW + 8 * j + c),
                      scalar2=None, op0=ALU.mult)
        if l % 2 == 1:
            ap = I["router_w"].h[l // 2].rearrange("(k p) e -> p k e", p=128)
            fw.dma("sp", WRT[:], I["router_w"].v(ap))

    def tile_step(l, s, ti, last_layer):
        t0, n = tiles[ti]
        nsub = (n + 127) // 128
        N = slice(0, n)
        is_moe = (l % 2 == 1)
        fw.dma("sp", Y[:, :, 0:n], hview(H32, s, ti))
        fw.dma("sp", XB[:, :, 0:n], hview(HB, s, ti))
        kt0 = 0 if ti == 0 else 1 + 4 * (ti - 1)

        def chain(b, lhs_fn, nk=8, rhs_fn=None, pr=ALLP):
            for k in range(nk):
                rhs = rhs_fn(k) if rhs_fn else XB.p(k, (ALLP, k, N))
                fw.op("pe", "matmul", out=bv(b, pr, N), lhsT=lhs_fn(k), rhs=rhs, start=(k == 0), stop=(k == nk - 1))

        for ci in range(4):
            w = wq.get()
            for gi in range(2):
                g = (ci % 2) * 2 + gi
                b = next_bank()
                chain(b, lambda k: V(w.ap[:, k, 128 * gi:128 * gi + 128], w.regs, "sb"))
                if ci < 2:
                    fw.op("act", "activation", out=AT.p(16 + g, (ALLP, 16 + g, N)), in_=bv(b, ALLP, N),
                          func=AF.Copy, scale=QSCALE)
                else:
                    fw.op("dve", "tensor_copy", out=KT[:, g, t0:t0 + n], in_=bv(b, ALLP, N))
        for ci in range(2):
            w = wq.get()
            for sub in range(nsub):
                ns = min(128, n - 128 * sub)
                b = next_bank()
                for k in range(8):
                    fw.op("pe", "matmul", out=bv(b, slice(0, ns), slice(0, 256)),
                          lhsT=XB.p(k, (ALLP, k, slice(128 * sub, 128 * sub + ns))),
                          rhs=V(w.ap[:, k, :], w.regs, "sb"), start=(k == 0), stop=(k == 7))
                buf, hf = bank(b)
                src = buf.h[0:ns, hf, 0:256].rearrange("p (h e) -> p h e", e=64)
                fw.op("dve", "tensor_copy", out=VR[0:ns, kt0 + sub, 4 * ci:4 * ci + 4, 0:64], in_=buf.v(src, hf))

        for cp in range(4):
            wx = wq.get()
            wg = wq.get()
            for ci in range(2):
                c = 2 * cp + ci
                cs = slice(128 * ci, 128 * ci + 128)
                b = next_bank()
                chain(b, lambda k: V(wx.ap[:, k, cs], wx.regs, "sb"))
                xr = XR[c % 2]
                fw.op("pool", "tensor_copy", out=xr[:, 0:3], in_=HIST[:, c, 0:3])
                fw.op("dve", "tensor_copy", out=xr[:, 3:3 + n], in_=bv(b, ALLP, N))
                fw.op("pool", "tensor_copy", out=HIST[:, c, 0:3], in_=xr[:, n:n + 3])
                b2 = next_bank()
                for j in range(4):
                    fw.op("pe", "matmul", out=bv(b2, ALLP, N), lhsT=CD[:, c, j, :], rhs=xr[:, j:j + n],
                          start=(j == 0), stop=(j == 3))
                xc = Fn()
                fw.op("act", "activation", out=xc[:, N], in_=bv(b2, ALLP, N), func=AF.Identity,
                      bias=pcol(l, C_CB + c))
                br_ = next_bank()
                fw.op("pe", "matmul", out=bv(br_, ALLP, N), lhsT=WRB[:, c, :], rhs=xc[:, N], start=True, stop=True)
                bi_ = next_bank()
                fw.op("pe", "matmul", out=bv(bi_, ALLP, N), lhsT=WIB[:, c, :], rhs=xc[:, N], start=True, stop=True)
                r = Fn()
                fw.op("act", "activation", out=r[:, N], in_=bv(br_, ALLP, N), func=AF.Sigmoid, bias=pcol(l, C_BR + c))
                ig = Fn()
                fw.op("act", "activation", out=ig[:, N], in_=bv(bi_, ALLP, N), func=AF.Sigmoid, bias=pcol(l, C_BI + c))
                a = Fn()
                fw.op("act", "activation", out=a[:, N], in_=r[:, N], func=AF.Exp, scale=CNEG[:, l, c:c + 1])
                fw.op("dve", "tensor_tensor", out=r[:, N], in0=a[:, N], in1=a[:, N], op=ALU.mult)
                fw.op("dve", "tensor_scalar", out=r[:, N], in0=r[:, N], scalar1=-1.0, scalar2=1.0, op0=ALU.mult, op1=ALU.add)
                fw.op("act", "activation", out=r[:, N], in_=r[:, N], func=AF.Sqrt)
                fw.op("dve", "tensor_tensor", out=ig[:, N], in0=ig[:, N], in1=xc[:, N], op=ALU.mult)
                fw.op("dve", "tensor_tensor", out=ig[:, N], in0=ig[:, N], in1=r[:, N], op=ALU.mult)
                hh = Fn()
                fw.op("dve", "tensor_tensor_scan", out=hh[:, N], data0=a[:, N], data1=ig[:, N],
                      initial=HST[:, c:c + 1], op0=ALU.mult, op1=ALU.add)
                fw.op("pool", "tensor_copy", out=HST[:, c:c + 1], in_=hh[:, n - 1:n])
                bg_ = next_bank()
                chain(bg_, lambda k: V(wg.ap[:, k, cs], wg.regs, "sb"))
                gg = Fn()
                fw.op("act", "activation", out=gg[:, N], in_=bv(bg_, ALLP, N), func=AF.Gelu_apprx_tanh)
                fw.op("dve", "tensor_tensor", out=AT.p(c, (ALLP, c, N)), in0=hh[:, N], in1=gg[:, N], op=ALU.mult)

        tp = (tap is not None and (l, s, ti) == tuple(tap))
        if tp:
            tapout("QT", AT[:, 16:20, :], [128, 4, 512], BF16)
            tapout("KT", KT[:, :, :], [128, 4, T], BF16)
            tapout("VR", VR[:, :, :, :], [128, NKT, 8, 65], BF16)
            tapout("REC", AT[:, 0:8, :], [128, 8, 512], BF16)
        if ti == 0:
            ktl = [(0, NM, 0, False)]
        else:
            ktl = [(0, NM, 0, False)] + [(kt, 128, 0, False) for kt in range(1, kt0)] + \
                  [(kt0 + d_, 128, 128 * d_, True) for d_ in range(4)]
        pt_i = 0
        for h in range(8):
            g = h // 2
            acc = PS[2]
            for ki, (kt, nk, q0, diag) in enumerate(ktl):
                sp_ = PS[ki % 2]
                kc0 = 0 if kt == 0 else NM + 128 * (kt - 1)
                for m in range(2):
                    pb = (h % 2) * 64 + 32 * m
                    fw.op("pe", "matmul", out=sp_.p(m, (slice(0, nk), m, slice(q0, n))),
                          lhsT=KT[pb:pb + 32, g, kc0:kc0 + nk],
                          rhs=AT.p(16 + g, (slice(pb, pb + 32), 16 + g, slice(q0, n))),
                          start=True, stop=True, tile_position=(pb, 0))
                pt = PTb[pt_i % 3]
                pt_i += 1
                fw.op("act", "activation", out=pt[0:nk, :, q0:n], in_=sp_[0:nk, :, q0:n], func=AF.Exp)
                if diag:
                    fw.op("pool", "memset", ap=pt[64:128, :, q0:q0 + 64], constant=0.0)
                for m in range(2):
                    fw.op("pe", "matmul", out=acc.p(m, (slice(0, 65), m, slice(q0, n))),
                          lhsT=VR[0:nk, kt, h, :], rhs=pt[0:nk, m, q0:n],
                          start=(ki == 0), stop=(ki == len(ktl) - 1))
            E = PS[3]
            for m in range(2):
                rr = Fn()
                fw.op("dve", "reciprocal", out=rr[64:65, N], in_=acc.p(m, (slice(64, 65), m, N)))
                fw.op("pe", "matmul", out=E.p(m, (slice(0, 64), m, N)), lhsT=ONE64[64:65, 0:64], rhs=rr[64:65, N],
                      start=True, stop=True)
            rb0 = Fn()
            rb1 = Fn()
            fw.op("dve", "tensor_copy", out=rb0[0:64, N], in_=E.p(0, (slice(0, 64), 0, N)))
            fw.op("dve", "tensor_copy", out=rb1[0:64, N], in_=E.p(1, (slice(0, 64), 1, N)))
            fw.op("dve", "tensor_tensor", out=rb0[0:64, N], in0=acc.p(0, (slice(0, 64), 0, N)), in1=rb0[0:64, N], op=ALU.mult)
            fw.op("dve", "tensor_tensor", out=rb1[0:64, N], in0=acc.p(1, (slice(0, 64), 1, N)), in1=rb1[0:64, N], op=ALU.mult)
            o = Fn()
            fw.op("dve", "scalar_tensor_tensor", out=o[0:64, N], in0=rb1[0:64, N], scalar=NLAM[0:64, l:l + 1],
                  in1=rb0[0:64, N], op0=ALU.mult, op1=ALU.add)
            fw.op("pool", "tensor_tensor", out=rb0[0:64, N], in0=o[0:64, N], in1=o[0:64, N], op=ALU.mult)
            fw.op("pe", "matmul", out=E.p(0, (slice(0, 64), 0, N)), lhsT=ONE64[0:64, 0:64], rhs=rb0[0:64, N],
                  start=True, stop=True)
            fw.op("act", "activation", out=rb1[0:64, N], in_=E.p(0, (slice(0, 64), 0, N)), func=AF.Ln,
                  scale=1.0 / 64, bias=RMS_EPS)
            fw.op("act", "activation", out=rb1[0:64, N], in_=rb1[0:64, N], func=AF.Exp, scale=-0.5)
            fw.op("dve", "scalar_tensor_tensor", out=AT.p(8 + h, (slice(0, 64), 8 + h, N)), in0=o[0:64, N],
                  scalar=GS[:, l:l + 1], in1=rb1[0:64, N], op0=ALU.mult, op1=ALU.mult)

        if tp:
            tapout("ATT", AT[:, 8:16, :], [128, 8, 512], BF16)
        for dp in range(4):
            wao = wq.get()
            wlo = wq.get()
            wga = wq.get()
            wgl = wq.get()
            for di in range(2):
                dt = 2 * dp + di
                cs = slice(128 * di, 128 * di + 128)
                b1 = next_bank()
                chain(b1, lambda k: V(wao.ap[0:64, k, cs], wao.regs, "sb"),
                      rhs_fn=lambda k: AT.p(8 + k, (slice(0, 64), 8 + k, N)))
                b2 = next_bank()
                chain(b2, lambda k: V(wlo.ap[:, k, cs], wlo.regs, "sb"), rhs_fn=lambda k: AT.p(k, (ALLP, k, N)))
                b3 = next_bank()
                chain(b3, lambda k: V(wga.ap[:, k, cs], wga.regs, "sb"))
                sa = Fn()
                fw.op("act", "activation", out=sa[:, N], in_=bv(b3, ALLP, N), func=AF.Sigmoid, bias=pcol(l, C_BG + dt))
                b4 = next_bank()
                chain(b4, lambda k: V(wgl.ap[:, k, cs], wgl.regs, "sb"))
                sl = Fn()
                fw.op("act", "activation", out=sl[:, N], in_=bv(b4, ALLP, N), func=AF.Sigmoid, bias=pcol(l, C_BG + 8 + dt))
                fw.op("dve", "tensor_tensor", out=sa[:, N], in0=sa[:, N], in1=bv(b1, ALLP, N), op=ALU.mult)
                fw.op("dve", "tensor_tensor", out=sl[:, N], in0=sl[:, N], in1=bv(b2, ALLP, N), op=ALU.mult)
                fw.op("pool", "tensor_tensor", out=MT.p(dt, (ALLP, dt, N)), in0=sa[:, N], in1=sl[:, N], op=ALU.add)
        for dp in range(4):
            wo = wq.get()
            for di in range(2):
                dt = 2 * dp + di
                cs = slice(128 * di, 128 * di + 128)
                b = next_bank()
                chain(b, lambda k: V(wo.ap[:, k, cs], wo.regs, "sb"), rhs_fn=lambda k: MT.p(k, (ALLP, k, N)))
                yk = Y.p(dt, (ALLP, dt, N))
                fw.op("dve", "scalar_tensor_tensor", out=yk, in0=yk, scalar=float(alpha), in1=bv(b, ALLP, N),
                      op0=ALU.mult, op1=ALU.add)
        if tp:
            tapout("MT", MT[:, :, :], [128, 8, 512], BF16)
            tapout("Y1", Y[:, :, :], [128, 8, 512], F32)
        ln_fm(n, lambda k: pcol(l, C_LNG + k), lambda k: pcol(l, C_LNB + k), LN_EPS)
        if tp:
            tapout("H1", Y[:, :, :], [128, 8, 512], F32)

        if is_moe:
            b = next_bank()
            for k in range(8):
                fw.op("pe", "matmul", out=bv(b, slice(0, NE), N), lhsT=WRT[:, k, :], rhs=Y.p(k, (ALLP, k, N)),
                      start=(k == 0), stop=(k == 7))
            LGT = Fn()
            fw.op("dve", "tensor_copy", out=LGT[0:NE, N], in_=bv(b, slice(0, NE), N))
            b = next_bank()
            for sub in range(nsub):
                ns = min(128, n - 128 * sub)
                fw.op("pe", "transpose", out=bv(b, slice(0, ns), slice(8 * sub, 8 * sub + 8)),
                      in_=LGT[0:NE, 128 * sub:128 * sub + ns], identity=IDF[0:NE, 0:NE])
            for sub in range(nsub):
                ns = min(128, n - 128 * sub)
                P_ = slice(0, ns)
                fw.op("dve", "tensor_copy", out=LG[P_, sub, :], in_=bv(b, P_, slice(8 * sub, 8 * sub + 8)))
                fw.op("dve", "max", out=M8[P_, sub, :], in_=LG[P_, sub, :])
                fw.op("dve", "tensor_scalar", out=CMB[P_, sub, :], in0=LG[P_, sub, :], scalar1=M8[P_, sub, 1:2],
                      scalar2=None, op0=ALU.is_ge)
                fw.op("dve", "tensor_scalar", out=SM[P_, 0:1], in0=M8[P_, sub, 0:1], scalar1=-1.0, scalar2=None, op0=ALU.mult)
                fw.op("act", "activation", out=LG[P_, sub, :], in_=LG[P_, sub, :], func=AF.Exp, bias=SM[P_, 0:1])
                fw.op("dve", "tensor_tensor", out=CMB[P_, sub, :], in0=CMB[P_, sub, :], in1=LG[P_, sub, :], op=ALU.mult)
                fw.op("dve", "reduce_sum", out=SM[P_, 1:2], in_=CMB[P_, sub, :], axis=AX.X)
                fw.op("dve", "reciprocal", out=SM[P_, 2:3], in_=SM[P_, 1:2])
                fw.op("dve", "tensor_scalar", out=CMB[P_, sub, :], in0=CMB[P_, sub, :], scalar1=SM[P_, 2:3],
                      scalar2=None, op0=ALU.mult)
            b = next_bank()
            for sub in range(nsub):
                ns = min(128, n - 128 * sub)
                fw.op("pe", "transpose", out=bv(b, slice(0, NE), slice(128 * sub, 128 * sub + ns)),
                      in_=CMB[0:ns, sub, :], identity=IDF[0:ns, 0:ns])
            CT = Fp[NF]
            fw.op("dve", "tensor_copy", out=CT[0:NE, N], in_=bv(b, slice(0, NE), N))

        nexp = NE if is_moe else 1
        if is_moe:
            for k in range(8):
                yk = Y.p(k, (ALLP, k, N))
                fw.op("pool", "tensor_scalar", out=yk, in0=yk, scalar1=float(alpha), scalar2=None, op0=ALU.mult)
        for e in range(nexp):
            for fp_ in range(NFT // 2):
                wg_ = wq.get()
                wu_ = wq.get()
                for fi in range(2):
                    f = 2 * fp_ + fi
                    cs = slice(128 * fi, 128 * fi + 128)
                    b1 = next_bank()
                    chain(b1, lambda k: V(wg_.ap[:, k, cs], wg_.regs, "sb"))
                    b2 = next_bank()
                    chain(b2, lambda k: V(wu_.ap[:, k, cs], wu_.regs, "sb"))
                    sg = Fn()
                    fw.op("act", "activation", out=sg[:, N], in_=bv(b1, ALLP, N), func=AF.Silu)
                    fw.op("dve", "tensor_tensor", out=AT.p(f, (ALLP, f, N)), in0=sg[:, N], in1=bv(b2, ALLP, N), op=ALU.mult)
            if is_moe:
                bc = next_bank()
                cte = Fn()
                fw.op("dve", "tensor_scalar", out=cte[0:NE, N], in0=CT[0:NE, N], scalar1=IDF[0:NE, e:e + 1],
                      scalar2=None, op0=ALU.mult)
                fw.op("pe", "matmul", out=bv(bc, ALLP, N), lhsT=ONE64[0:NE, :], rhs=cte[0:NE, N], start=True, stop=True)
                cb = CBT
                fw.op("act", "activation", out=cb[:, N], in_=bv(bc, ALLP, N), func=AF.Copy)
            for fp_ in range(NFT // 2):
                wd_ = wq.get()
                for fi in range(2):
                    f = 2 * fp_ + fi
                    for dt in range(8):
                        fw.op("pe", "matmul", out=bv(dt, ALLP, N), lhsT=V(wd_.ap[:, fi, 128 * dt:128 * dt + 128], wd_.regs, "sb"),
                              rhs=AT.p(f, (ALLP, f, N)), start=(f == 0), stop=(f == NFT - 1))
            for dt in range(8):
                yk = Y.p(dt, (ALLP, dt, N))
                if is_moe:
                    t = Fn()
                    fw.op("dve", "tensor_tensor", out=t[:, N], in0=bv(dt, ALLP, N), in1=cb[:, N], op=ALU.mult)
                    fw.op("pool", "tensor_tensor", out=yk, in0=yk, in1=t[:, N], op=ALU.add)
                else:
                    fw.op("dve", "scalar_tensor_tensor", out=yk, in0=yk, scalar=float(alpha), in1=bv(dt, ALLP, N),
                          op0=ALU.mult, op1=ALU.add)
        bank_rr[0] = 0
        if tp:
            tapout("Y2", Y[:, :, :], [128, 8, 512], F32)
        ln_fm(n, lambda k: pcol(l, C_LNG + 8 + k), lambda k: pcol(l, C_LNB + 8 + k), LN_EPS)
        dbg_store(l + 1, s, ti, n)
        if not last_layer:
            store_h(s, ti, n)
        elif ti > 0:
            for sub in range(nsub):
                ns = min(128, n - 128 * sub)
                xl = XL[sub % 2]
                for half in range(2):
                    b = next_bank()
                    for q in range(4):
                        k = 4 * half + q
                        fw.op("pe", "transpose", out=bv(b, slice(0, ns), slice(128 * q, 128 * q + 128)),
                              in_=Y.p(k, (ALLP, k, slice(128 * sub, 128 * sub + ns))), identity=IDF[:])
                    fw.op("act", "activation", out=xl[0:ns, 512 * half:512 * half + 512], in_=bv(b, slice(0, ns), slice(0, 512)),
                          func=AF.Copy)
                r0 = t0 - NM + 128 * sub
                fw.dma("sp", OUT.v(OUT.h[s, r0:r0 + ns, :], s * NTL + ti), xl[0:ns, :])

    nl = L if stop_after is None else stop_after
    for l in range(nl):
        wq.add(chunks_for(l) * (n_seq * NTL))
    for l in range(nl):
        layer_setup(l)
        for s in range(n_seq):
            fw.op("pool", "memset", ap=HIST[:], constant=0.0)
            fw.op("pool", "memset", ap=HST[:], constant=0.0)
            for ti in range(NTL):
                tile_step(l, s, ti, last_layer=(l == L - 1))
    outs = [OUT[:]] + ([DBG[:]] if DBG is not None else []) + [t[:] for t in TAPS.values()]
    fw.finish(outs)
    return nc, fw


_PROG = {}


def kernel(**inputs):
    n_cores = 8
    x = np.ascontiguousarray(inputs["x"], dtype=np.float32)
    B, S, _ = x.shape
    n_seq = B // n_cores
    depth = inputs["w_in"].shape[0]
    alpha = (2 * depth) ** 0.25
    key = (n_seq, S, depth)
    if key not in _PROG:
        _PROG[key] = build_program(n_seq, S, depth, alpha)[0]
    nc = _PROG[key]
    shared = {k: np.ascontiguousarray(inputs[k], dtype=np.float32) for k in WNAMES if k != "x"}
    in_maps = []
    for c in range(n_cores):
        m = dict(shared)
        m["x"] = x[c * n_seq:(c + 1) * n_seq]
        in_maps.append(m)
    res = run_bass_kernel_spmd(nc, in_maps, core_ids=list(range(n_cores)))
    return np.concatenate([r["out"] for r in res.results], axis=0).astype(np.float32)
```

```python
import math
import numpy as np
import concourse.bass as bass
import concourse.mybir as mybir
from concourse.bass_utils import run_bass_kernel_spmd

F32 = mybir.dt.float32
BF16 = mybir.dt.bfloat16
AF = mybir.ActivationFunctionType
ALU = mybir.AluOpType
AX = mybir.AxisListType

D = 1024
NM = 16
DA = 512
FF = 2816
NE = 8
PW = 5632
LN_EPS = 1e-5
RMS_EPS = 1e-5
NFT = FF // 128
EAGER_CAST = False


class Reg:
    __slots__ = ("name", "w", "r", "dsem", "dcnt")

    def __init__(self, name):
        self.name = name
        self.w = None
        self.r = {}
        self.dsem = None
        self.dcnt = 0


class V:
    __slots__ = ("ap", "regs", "kind")

    def __init__(self, ap, regs, kind):
        self.ap = ap
        self.regs = regs
        self.kind = kind


class Buf:
    def __init__(self, name, handle, nreg, kind):
        self.kind = kind
        self.name = name
        self.h = handle
        self.regs = [Reg(f"{name}_{i}") for i in range(nreg)]

    def __getitem__(self, idx):
        return V(self.h[idx], self.regs, self.kind)

    def v(self, ap, ridx=None):
        if ridx is None:
            return V(ap, self.regs, self.kind)
        if isinstance(ridx, int):
            ridx = (ridx,)
        return V(ap, [self.regs[i] for i in ridx], self.kind)

    def p(self, ridx, idx):
        if isinstance(ridx, int):
            ridx = (ridx,)
        return V(self.h[idx], [self.regs[i] for i in ridx], self.kind)


WRITE_KW = ("out", "accum_out", "ap")


class FW:
    ENG = ("pe", "act", "dve", "pool", "sp")

    def __init__(self, nc):
        self.nc = nc
        self.eng = {"pe": nc.tensor, "act": nc.scalar, "dve": nc.vector,
                    "pool": nc.gpsimd, "sp": nc.sync}
        self.sem = {}
        self.cnt = {}
        self.known = {}
        for e in self.ENG:
            self.sem[e] = nc.alloc_semaphore(name=f"s_{e}")
            self.cnt[e] = 0
            self.known[e] = {}
        self.nsem = len(self.ENG)
        self.nwaits = 0
        self.ninst = 0

    def sb(self, name, shape, dtype, nreg=1):
        return Buf(name, self.nc.alloc_sbuf_tensor(name, list(shape), dtype), nreg, "sb")

    def ps(self, name, shape, dtype=F32, nreg=1):
        return Buf(name, self.nc.alloc_psum_tensor(name, list(shape), dtype), nreg, "ps")

    def dram(self, name, shape, dtype, kind="Internal", nreg=1):
        return Buf(name, self.nc.dram_tensor(name, list(shape), dtype, kind=kind), nreg, "dram")

    def newsem(self, name):
        self.sem[name] = self.nc.alloc_semaphore(name=name)
        self.nsem += 1
        return name

    def _wait(self, e, key, val):
        kn = self.known[e]
        if kn.get(key, 0) >= val:
            return
        if key == e and e == "pe":
            return
        kn[key] = val
        self.eng[e].wait_ge(self.sem[key], val)
        self.nwaits += 1

    def _deps(self, e, reads, writes):
        for v in reads:
            for r in v.regs:
                if r.w is not None:
                    self._wait(e, r.w[0], r.w[1])
        for v in writes:
            for r in v.regs:
                if r.w is not None:
                    self._wait(e, r.w[0], r.w[1])
                for k, val in r.r.items():
                    self._wait(e, k, val)

    def _mark(self, ev, reads, writes):
        for v in reads:
            for r in v.regs:
                r.r[ev[0]] = ev[1]
        for v in writes:
            for r in v.regs:
                r.w = ev
                r.r = {}

    def op(self, e, fn, *, reads=(), writes=(), **kw):
        rd = list(reads)
        wr = list(writes)
        args = {}
        for k, v in kw.items():
            if isinstance(v, V):
                (wr if k in WRITE_KW else rd).append(v)
                args[k] = v.ap
            else:
                args[k] = v
        self._deps(e, rd, wr)
        ins = getattr(self.eng[e], fn)(**args)
        self.cnt[e] += 1
        ins.then_inc(self.sem[e], 1)
        self._mark((e, self.cnt[e]), rd, wr)
        self.ninst += 1
        return ins

    def dma(self, q, out, in_, sem_of=None, serialize=True, **kw):
        owner = sem_of
        if owner is None:
            for v in (out, in_):
                if v.kind == "sb":
                    owner = v.regs[0]
                    break
        if owner is None:
            owner = out.regs[0]
        if owner.dsem is None:
            owner.dsem = self.newsem(f"d_{owner.name}")
        self._deps(q, [in_], [out])
        if serialize:
            self._wait(q, owner.dsem, owner.dcnt)
        ins = self.eng[q].dma_start(out=out.ap, in_=in_.ap, **kw)
        owner.dcnt += 16
        ins.then_inc(self.sem[owner.dsem], 16)
        self._mark((owner.dsem, owner.dcnt), [in_], [out])
        self.ninst += 1
        return ins

    def finish(self, outs, q="sp"):
        for v in outs:
            for r in v.regs:
                if r.w is not None:
                    self._wait(q, r.w[0], r.w[1])


WNAMES = ["x", "meta_tokens", "ln_in_g", "ln_in_b", "w_in", "b_gate", "da_lambda",
          "da_subln_g", "conv_w", "conv_b", "lru_wr", "lru_br", "lru_wi", "lru_bi",
          "lru_lambda", "w_attn_out", "w_lru_out", "w_o", "ln_g", "ln_b", "ffn_wg",
          "ffn_wu", "ffn_wd", "router_w", "moe_wg", "moe_wu", "moe_wd"]


def build_program(n_seq, seq, depth, alpha, dbg_layers=False, stop_after=None, tap=None):
    nc = bass.Bass("TRN2", target_bir_lowering=False)
    fw = FW(nc)
    L = depth
    Nd = (L + 1) // 2
    Nm = L // 2
    T = NM + seq
    NX = seq // 512
    assert seq % 512 == 0
    tiles = [(0, NM)] + [(NM + 512 * i, 512) for i in range(NX)]
    NTL = len(tiles)
    NKT = 1 + seq // 128

    shapes = {
        "x": [n_seq, seq, D], "meta_tokens": [NM, D], "ln_in_g": [D], "ln_in_b": [D],
        "w_in": [L, D, PW], "b_gate": [L, 2, D], "da_lambda": [L, 4, 32],
        "da_subln_g": [L, 64], "conv_w": [L, 4, D], "conv_b": [L, D],
        "lru_wr": [L, 16, 64, 64], "lru_br": [L, D], "lru_wi": [L, 16, 64, 64],
        "lru_bi": [L, D], "lru_lambda": [L, D], "w_attn_out": [L, DA, D],
        "w_lru_out": [L, D, D], "w_o": [L, D, D], "ln_g": [L, 2, D], "ln_b": [L, 2, D],
        "ffn_wg": [Nd, D, FF], "ffn_wu": [Nd, D, FF], "ffn_wd": [Nd, FF, D],
        "router_w": [max(Nm, 1), D, NE], "moe_wg": [max(Nm, 1), NE, D, FF],
        "moe_wu": [max(Nm, 1), NE, D, FF], "moe_wd": [max(Nm, 1), NE, FF, D],
    }
    I = {k: fw.dram(k, shapes[k], F32, kind="ExternalInput") for k in WNAMES}
    OUT = fw.dram("out", [n_seq, seq, D], F32, kind="ExternalOutput", nreg=n_seq * NTL)
    H32 = fw.dram("h32", [n_seq, D, T], F32, nreg=n_seq * NTL)
    HB = fw.dram("hb", [n_seq, D, T], BF16, nreg=n_seq * NTL)
    DBG = None
    if dbg_layers:
        DBG = fw.dram("dbg", [L + 1, n_seq, D, T], F32, kind="ExternalOutput")
    TAPS = {}

    def tapout(name, view, shape, dtype):
        if tap is None:
            return
        if name not in TAPS:
            TAPS[name] = fw.dram("tap_" + name, shape, dtype, kind="ExternalOutput")
        fw.dma("sp", TAPS[name][:], view)
    WB = {
        "w_in": fw.dram("wb_in", [L, D, PW], BF16, nreg=L),
        "w_attn_out": fw.dram("wb_ao", [L, DA, D], BF16, nreg=L),
        "w_lru_out": fw.dram("wb_lo", [L, D, D], BF16, nreg=L),
        "w_o": fw.dram("wb_o", [L, D, D], BF16, nreg=L),
        "ffn_wg": fw.dram("wb_fg", [Nd, D, FF], BF16, nreg=Nd),
        "ffn_wu": fw.dram("wb_fu", [Nd, D, FF], BF16, nreg=Nd),
        "ffn_wd": fw.dram("wb_fd", [Nd, FF, D], BF16, nreg=Nd),
        "moe_wg": fw.dram("wb_mg", [max(Nm, 1), NE, D, FF], BF16, nreg=max(Nm, 1) * NE),
        "moe_wu": fw.dram("wb_mu", [max(Nm, 1), NE, D, FF], BF16, nreg=max(Nm, 1) * NE),
        "moe_wd": fw.dram("wb_md", [max(Nm, 1), NE, FF, D], BF16, nreg=max(Nm, 1) * NE),
    }

    KT = fw.sb("KT", [128, 4, T], BF16)
    VR = fw.sb("VR", [128, NKT, 8, 64], BF16)
    ONESB = fw.sb("ONESB", [128, 64], BF16)
    Y = fw.sb("Y", [128, 8, 512], F32, nreg=8)
    XB = fw.sb("XB", [128, 8, 512], BF16, nreg=8)
    AT = fw.sb("AT", [128, NFT, 512], BF16, nreg=NFT)
    MT = fw.sb("MT", [128, 8, 512], BF16, nreg=8)
    NF = 8
    Fp = [fw.sb(f"F{i}", [128, 512], F32) for i in range(NF + 1)]
    LNR = fw.sb("LNR", [128, 512], F32)
    CBT = fw.sb("CBT", [128, 512], F32)
    XR = [fw.sb(f"XR{i}", [128, 516], BF16) for i in range(2)]
    PTb = [fw.sb(f"PT{i}", [128, 2, 512], BF16) for i in range(3)]
    NSLOT = 8
    RING = [fw.sb(f"RG{i}", [128, 2048], BF16) for i in range(NSLOT)]
    XL = [fw.sb("XL0", [128, 1024], F32)] * 2
    WRB = fw.sb("WRB", [128, 8, 128], F32)
    WIB = fw.sb("WIB", [128, 8, 128], F32)
    CD = fw.sb("CD", [128, 8, 4, 128], BF16)
    WRT = fw.sb("WRT", [128, 8, NE], F32)
    PTB = fw.sb("PTB", [128, L, 112], F32)
    LNIN = fw.sb("LNIN", [128, 16], F32)
    CNEG = fw.sb("CNEG", [128, L, 8], F32)
    HPB = fw.sb("HPB", [128, L, 24], F32)
    STG = fw.sb("STG", [128, 128], F32)
    IDF = fw.sb("IDF", [128, 128], F32)
    IDB = fw.sb("IDB", [128, 128], BF16)
    ONESD = fw.sb("ONESD", [128, 128], F32)
    ONE64 = fw.sb("ONE64", [128, 128], F32)
    HIST = fw.sb("HIST", [128, 8, 4], BF16)
    HST = fw.sb("HST", [128, 8], F32)
    LAMW = fw.sb("LAMW", [128, 4, 32], F32)
    LAMS = fw.sb("LAMS", [128, 8], F32)
    NLAM = fw.sb("NLAM", [128, L], F32)
    GS = fw.sb("GS", [64, L], F32)
    LG = fw.sb("LG", [128, 4, NE], F32)
    M8 = fw.sb("M8", [128, 4, 8], F32)
    CMB = fw.sb("CMB", [128, 4, NE], F32)
    SM = fw.sb("SM", [128, 16], F32)

    PS = [fw.ps(f"PS{i}", [128, 2, 512], F32, nreg=2) for i in range(4)]
    bank_rr = [0]

    def bank(b):
        return PS[b // 2], b % 2

    def next_bank():
        b = bank_rr[0]
        bank_rr[0] = (b + 1) % 8
        return b

    def bv(b, pr, cols):
        buf, hf = bank(b)
        return buf.p(hf, (pr, hf, cols))

    f_rr = [0]

    def Fn():
        i = f_rr[0]
        f_rr[0] = (i + 1) % NF
        return Fp[i]

    ALLP = slice(0, 128)

    fw.op("pool", "memset", ap=IDF[:], constant=0.0)
    fw.op("pool", "affine_select", out=IDF[:], in_=IDF[:], pattern=[[-1, 128]],
          compare_op=ALU.not_equal, fill=1.0, base=0, channel_multiplier=1)
    fw.op("pool", "tensor_copy", out=IDB[:], in_=IDF[:])
    fw.op("pool", "memset", ap=ONESD[:], constant=1.0 / D)
    fw.op("pool", "memset", ap=ONE64[:], constant=1.0)
    fw.op("pool", "memset", ap=WRB[:], constant=0.0)
    fw.op("pool", "memset", ap=WIB[:], constant=0.0)
    fw.op("pool", "memset", ap=ONESB[:], constant=1.0)

    def stage_rows(dst_v, loads):
        r0 = 0
        for src_ap, nrows in loads:
            fw.dma("sp", STG[r0:r0 + nrows, :], src_ap)
            r0 += nrows
        b = next_bank()
        fw.op("pe", "transpose", out=bv(b, ALLP, slice(0, r0)), in_=STG[0:r0, :], identity=IDF[0:r0, 0:r0])
        fw.op("dve", "tensor_copy", out=dst_v, in_=bv(b, ALLP, slice(0, r0)))

    def rows(buf, ap, n):
        return (buf.v(ap.rearrange("(r p) -> r p", p=128)), n)

    for l in range(L):
        loads = [
            (I["b_gate"].v(I["b_gate"].h[l].rearrange("i (r p) -> (i r) p", p=128)), 16),
            (I["conv_w"].v(I["conv_w"].h[l].rearrange("i (r p) -> (i r) p", p=128)), 32),
            rows(I["conv_b"], I["conv_b"].h[l], 8),
            rows(I["lru_br"], I["lru_br"].h[l], 8),
            rows(I["lru_bi"], I["lru_bi"].h[l], 8),
            rows(I["lru_lambda"], I["lru_lambda"].h[l], 8),
            (I["ln_g"].v(I["ln_g"].h[l].rearrange("i (r p) -> (i r) p", p=128)), 16),
            (I["ln_b"].v(I["ln_b"].h[l].rearrange("i (r p) -> (i r) p", p=128)), 16),
        ]
        stage_rows(PTB[:, l, :], loads)
    stage_rows(LNIN[:, :], [rows(I["ln_in_g"], I["ln_in_g"].h[:], 8), rows(I["ln_in_b"], I["ln_in_b"].h[:], 8)])
    C_BG, C_CW, C_CB, C_BR, C_BI, C_LAM, C_LNG, C_LNB = 0, 16, 48, 56, 64, 72, 80, 96

    def pcol(l, c):
        return PTB[:, l, c:c + 1]

    for l in range(L):
        t = Fn()
        fw.op("act", "activation", out=t[:, 0:8], in_=PTB[:, l, C_LAM:C_LAM + 8], func=AF.Exp, scale=-1.0)
        fw.op("act", "activation", out=t[:, 8:16], in_=t[:, 0:8], func=AF.Ln, bias=1.0)
        fw.op("dve", "tensor_scalar", out=CNEG[:, l, :], in0=t[:, 8:16], scalar1=-8.0, scalar2=None, op0=ALU.mult)
        fw.op("dve", "tensor_scalar", out=HPB[:, l, 16:24], in0=t[:, 8:16], scalar1=-4.0, scalar2=None, op0=ALU.mult)
        fw.op("dve", "tensor_scalar", out=HPB[:, l, 0:16], in0=PTB[:, l, C_BR:C_BR + 16], scalar1=0.5, scalar2=None, op0=ALU.mult)
    for l in range(L):
        lam_init = 0.8 - 0.6 * math.exp(-0.3 * l)
        src = bass.AP(I["da_lambda"].h, l * 128, [[0, 128], [1, 128]])
        fw.dma("sp", LAMW.v(LAMW.h[:].rearrange("p a b -> p (a b)")), I["da_lambda"].v(src))
        fw.op("dve", "tensor_tensor", out=LAMW[:, 0, :], in0=LAMW[:, 0, :], in1=LAMW[:, 1, :], op=ALU.mult)
        fw.op("dve", "tensor_tensor", out=LAMW[:, 2, :], in0=LAMW[:, 2, :], in1=LAMW[:, 3, :], op=ALU.mult)
        fw.op("dve", "reduce_sum", out=LAMS[:, 0:1], in_=LAMW[:, 0, :], axis=AX.X)
        fw.op("dve", "reduce_sum", out=LAMS[:, 1:2], in_=LAMW[:, 2, :], axis=AX.X)
        fw.op("act", "activation", out=LAMS[:, 2:4], in_=LAMS[:, 0:2], func=AF.Exp)
        fw.op("dve", "tensor_tensor", out=LAMS[:, 4:5], in0=LAMS[:, 3:4], in1=LAMS[:, 2:3], op=ALU.subtract)
        fw.op("dve", "tensor_scalar", out=NLAM[:, l:l + 1], in0=LAMS[:, 4:5], scalar1=-lam_init, scalar2=None, op0=ALU.add)
        fw.dma("sp", GS[:, l:l + 1], I["da_subln_g"].v(I["da_subln_g"].h[l].rearrange("(p o) -> p o", o=1)))
        fw.op("dve", "tensor_scalar", out=GS[:, l:l + 1], in0=GS[:, l:l + 1], scalar1=1.0 - lam_init, scalar2=None, op0=ALU.mult)

    cast_ev = []
    CAST_INFLIGHT = 2
    cast_jobs = {l_: [] for l_ in range(L)}

    def cast2d(jobs, dst_buf, dst_ap, src_buf, src_ap, reg, rows_, cols):
        c = cols
        for cand in (2048, 1408, 1024, 512):
            if cols % cand == 0:
                c = cand
                break
        rstep = 256 if rows_ % 256 == 0 and cols > 2048 else rows_
        for r0 in range(0, rows_, rstep):
            d = dst_ap[r0:r0 + rstep, :].rearrange("r (a c) -> r a c", c=c)
            s_ = src_ap[r0:r0 + rstep, :].rearrange("r (a c) -> r a c", c=c)

            def job(d=d, s_=s_):
                if len(cast_ev) >= CAST_INFLIGHT:
                    fw._wait("pool", *cast_ev[-CAST_INFLIGHT])
                fw.dma("pool", dst_buf.v(d, reg), src_buf.v(s_), sem_of=dst_buf.regs[reg], serialize=False)
                cast_ev.append(dst_buf.regs[reg].w)
            jobs.append(job)

    for l in range(L):
        J = cast_jobs[l]
        cast2d(J, WB["w_in"], WB["w_in"].h[l], I["w_in"], I["w_in"].h[l], l, D, PW)
        cast2d(J, WB["w_attn_out"], WB["w_attn_out"].h[l], I["w_attn_out"], I["w_attn_out"].h[l], l, DA, D)
        cast2d(J, WB["w_lru_out"], WB["w_lru_out"].h[l], I["w_lru_out"], I["w_lru_out"].h[l], l, D, D)
        cast2d(J, WB["w_o"], WB["w_o"].h[l], I["w_o"], I["w_o"].h[l], l, D, D)
        i = l // 2
        if l % 2 == 0:
            cast2d(J, WB["ffn_wg"], WB["ffn_wg"].h[i], I["ffn_wg"], I["ffn_wg"].h[i], i, D, FF)
            cast2d(J, WB["ffn_wu"], WB["ffn_wu"].h[i], I["ffn_wu"], I["ffn_wu"].h[i], i, D, FF)
            cast2d(J, WB["ffn_wd"], WB["ffn_wd"].h[i], I["ffn_wd"], I["ffn_wd"].h[i], i, FF, D)
        else:
            for e in range(NE):
                cast2d(J, WB["moe_wg"], WB["moe_wg"].h[i, e], I["moe_wg"], I["moe_wg"].h[i, e], i * NE + e, D, FF)
                cast2d(J, WB["moe_wu"], WB["moe_wu"].h[i, e], I["moe_wu"], I["moe_wu"].h[i, e], i * NE + e, D, FF)
                cast2d(J, WB["moe_wd"], WB["moe_wd"].h[i, e], I["moe_wd"], I["moe_wd"].h[i, e], i * NE + e, FF, D)

    def run_cast_jobs(l_, k=None):
        J = cast_jobs.get(l_)
        if not J:
            return
        k = len(J) if k is None else min(k, len(J))
        for _ in range(k):
            J.pop(0)()

    for l_ in range(L if EAGER_CAST else 1):
        run_cast_jobs(l_)

    class WStream:
        def __init__(self):
            self.plan = []
            self.issued = 0
            self.taken = 0

        def add(self, specs):
            self.plan.extend(specs)

        def _issue(self, i):
            buf, reg, ap, shape = self.plan[i]
            slot = RING[i % NSLOT]
            n = 1
            for s_ in shape[1:]:
                n *= s_
            dst = slot.h[0:shape[0], 0:n]
            if len(shape) == 3:
                dst = dst.rearrange("p (a b) -> p a b", b=shape[2])
            fw.dma("sp", slot.v(dst), buf.v(ap, reg))

        def get(self):
            i = self.taken
            assert i < len(self.plan), "weight plan exhausted"
            while self.issued < min(len(self.plan), i + NSLOT - 3):
                self._issue(self.issued)
                self.issued += 1
            self.taken += 1
            buf, reg, ap, shape = self.plan[i]
            slot = RING[i % NSLOT]
            n = 1
            for s_ in shape[1:]:
                n *= s_
            v = slot.h[0:shape[0], 0:n]
            if len(shape) == 3:
                v = v.rearrange("p (a b) -> p a b", b=shape[2])
            return slot.v(v)

    wq = WStream()

    def kcols(buf, reg, mat_ap, c0, ncol, krows=128):
        ap = mat_ap[:, c0:c0 + ncol].rearrange("(k p) c -> p k c", p=krows)
        return (buf, reg, ap, [krows, ap.shape[1], ncol])

    def chunks_for(l):
        sp = []
        win = WB["w_in"].h[l]
        for ci in range(4):
            sp.append(kcols(WB["w_in"], l, win, 256 * ci, 256))
        for ci in range(2):
            sp.append(kcols(WB["w_in"], l, win, 1024 + 256 * ci, 256))
        for cp in range(4):
            sp.append(kcols(WB["w_in"], l, win, 1536 + 256 * cp, 256))
            sp.append(kcols(WB["w_in"], l, win, 2560 + 256 * cp, 256))
        for dp in range(4):
            sp.append(kcols(WB["w_attn_out"], l, WB["w_attn_out"].h[l], 256 * dp, 256, krows=64))
            sp.append(kcols(WB["w_lru_out"], l, WB["w_lru_out"].h[l], 256 * dp, 256))
            sp.append(kcols(WB["w_in"], l, win, 3584 + 256 * dp, 256))
            sp.append(kcols(WB["w_in"], l, win, 4608 + 256 * dp, 256))
        for dp in range(4):
            sp.append(kcols(WB["w_o"], l, WB["w_o"].h[l], 256 * dp, 256))
        i = l // 2
        if l % 2 == 0:
            exps = [(WB["ffn_wg"], WB["ffn_wu"], WB["ffn_wd"], i, WB["ffn_wg"].h[i], WB["ffn_wu"].h[i], WB["ffn_wd"].h[i])]
        else:
            exps = [(WB["moe_wg"], WB["moe_wu"], WB["moe_wd"], i * NE + e, WB["moe_wg"].h[i, e],
                     WB["moe_wu"].h[i, e], WB["moe_wd"].h[i, e]) for e in range(NE)]
        for (bg_, bu_, bd_, reg, g_ap, u_ap, d_ap) in exps:
            for fp_ in range(NFT // 2):
                sp.append(kcols(bg_, reg, g_ap, 256 * fp_, 256))
                sp.append(kcols(bu_, reg, u_ap, 256 * fp_, 256))
            for fp_ in range(NFT // 2):
                ap = d_ap[256 * fp_:256 * fp_ + 256, :].rearrange("(f p) c -> p f c", p=128)
                sp.append((bd_, reg, ap, [128, 2, D]))
        return sp

    def hview(buf, s, ti):
        t0, n = tiles[ti]
        ap = buf.h[s].rearrange("(k p) t -> p k t", p=128)[:, :, t0:t0 + n]
        return buf.v(ap, s * NTL + ti)

    def ln_fm(n, gcol, bcol, eps):
        bA = next_bank()
        bB = next_bank()
        for k in range(8):
            sq = Fn()
            fw.op("act", "activation", out=sq[:, 0:n], in_=Y.p(k, (ALLP, k, slice(0, n))), func=AF.Square)
            fw.op("pe", "matmul", out=bv(bA, ALLP, slice(0, n)), lhsT=ONESD[:], rhs=Y.p(k, (ALLP, k, slice(0, n))),
                  start=(k == 0), stop=(k == 7))
            fw.op("pe", "matmul", out=bv(bB, ALLP, slice(0, n)), lhsT=ONESD[:], rhs=sq[:, 0:n],
                  start=(k == 0), stop=(k == 7))
        msq = Fn()
        fw.op("act", "activation", out=msq[:, 0:n], in_=bv(bA, ALLP, slice(0, n)), func=AF.Square)
        var = Fn()
        fw.op("dve", "tensor_tensor", out=var[:, 0:n], in0=bv(bB, ALLP, slice(0, n)), in1=msq[:, 0:n], op=ALU.subtract)
        fw.op("dve", "tensor_scalar", out=var[:, 0:n], in0=var[:, 0:n], scalar1=0.0, scalar2=eps, op0=ALU.max, op1=ALU.add)
        fw.op("act", "activation", out=var[:, 0:n], in_=var[:, 0:n], func=AF.Sqrt)
        rstd = LNR
        fw.op("dve", "reciprocal", out=rstd[:, 0:n], in_=var[:, 0:n])
        for k in range(8):
            t = Fn()
            yk = Y.p(k, (ALLP, k, slice(0, n)))
            fw.op("dve", "tensor_tensor", out=t[:, 0:n], in0=yk, in1=bv(bA, ALLP, slice(0, n)), op=ALU.subtract)
            fw.op("dve", "tensor_tensor", out=t[:, 0:n], in0=t[:, 0:n], in1=rstd[:, 0:n], op=ALU.mult)
            fw.op("act", "activation", out=yk, in_=t[:, 0:n], func=AF.Identity, scale=gcol(k), bias=bcol(k))
            fw.op("dve", "tensor_copy", out=XB.p(k, (ALLP, k, slice(0, n))), in_=yk)

    def store_h(s, ti, n):
        fw.dma("sp", hview(H32, s, ti), Y[:, :, 0:n])
        fw.dma("sp", hview(HB, s, ti), XB[:, :, 0:n])

    def dbg_store(li, s, ti, n):
        if DBG is not None:
            t0, _ = tiles[ti]
            ap = DBG.h[li, s].rearrange("(k p) t -> p k t", p=128)[:, :, t0:t0 + n]
            fw.dma("sp", DBG.v(ap), Y[:, :, 0:n])

    for s in range(n_seq):
        for ti, (t0, n) in enumerate(tiles):
            nsub = (n + 127) // 128
            for sub in range(nsub):
                ns = min(128, n - 128 * sub)
                xl = XL[sub % 2]
                if ti == 0:
                    fw.dma("sp", xl[0:ns, :], I["meta_tokens"][0:ns, :])
                else:
                    r0 = t0 - NM + 128 * sub
                    fw.dma("sp", xl[0:ns, :], I["x"].v(I["x"].h[s, r0:r0 + ns, :]))
                for half in range(2):
                    b = next_bank()
                    for q in range(4):
                        k = 4 * half + q
                        fw.op("pe", "transpose", out=bv(b, ALLP, slice(128 * q, 128 * q + ns)),
                              in_=xl[0:ns, 128 * k:128 * k + 128], identity=IDF[0:ns, 0:ns])
                    buf, hf = bank(b)
                    src = buf.p(hf, (ALLP, hf, slice(0, 512))).ap.rearrange("p (q t) -> p q t", t=128)[:, :, 0:ns]
                    dst = Y.h[:, 4 * half:4 * half + 4, 128 * sub:128 * sub + ns]
                    fw.op("dve", "tensor_copy", out=Y.v(dst, range(4 * half, 4 * half + 4)), in_=buf.v(src, hf))
            ln_fm(n, lambda k: LNIN[:, k:k + 1], lambda k: LNIN[:, 8 + k:9 + k], LN_EPS)
            store_h(s, ti, n)
            dbg_store(0, s, ti, n)

    QSCALE = 32 ** -0.5

    def layer_setup(l):
        for (dst, src) in ((WRB, I["lru_wr"]), (WIB, I["lru_wi"])):
            for hh in range(2):
                sap = src.h[l].rearrange("(c hh) i j -> hh i c j", hh=2)[hh]
                fw.dma("sp", dst[64 * hh:64 * hh + 64, :, 64 * hh:64 * hh + 64], src.v(sap))
        for c in range(8):
            for j in range(4):
                fw.op("dve", "tensor_scalar", out=CD[:, c, j, :], in0=IDB[:], scalar1=pcol(l, C_CW + 8 * j + c),
                      scalar2=None, op0=ALU.mult)
        if l % 2 == 1:
            ap = I["router_w"].h[l // 2].rearrange("(k p) e -> p k e", p=128)
            fw.dma("sp", WRT[:], I["router_w"].v(ap))

    def tile_step(l, s, ti, last_layer):
        t0, n = tiles[ti]
        nsub = (n + 127) // 128
        N = slice(0, n)
        is_moe = (l % 2 == 1)
        fw.dma("sp", Y[:, :, 0:n], hview(H32, s, ti))
        fw.dma("sp", XB[:, :, 0:n], hview(HB, s, ti))
        kt0 = 0 if ti == 0 else 1 + 4 * (ti - 1)

        def chain(b, lhs_fn, nk=8, rhs_fn=None, pr=ALLP):
            for k in range(nk):
                rhs = rhs_fn(k) if rhs_fn else XB.p(k, (ALLP, k, N))
                fw.op("pe", "matmul", out=bv(b, pr, N), lhsT=lhs_fn(k), rhs=rhs, start=(k == 0), stop=(k == nk - 1))

        for ci in range(4):
            w = wq.get()
            for gi in range(2):
                g = (ci % 2) * 2 + gi
                b = next_bank()
                chain(b, lambda k: V(w.ap[:, k, 128 * gi:128 * gi + 128], w.regs, "sb"))
                if ci < 2:
                    fw.op("act", "activation", out=AT.p(16 + g, (ALLP, 16 + g, N)), in_=bv(b, ALLP, N),
                          func=AF.Copy, scale=QSCALE)
                else:
                    fw.op("dve", "tensor_copy", out=KT[:, g, t0:t0 + n], in_=bv(b, ALLP, N))
        for ci in range(2):
            w = wq.get()
            for sub in range(nsub):
                ns = min(128, n - 128 * sub)
                b = next_bank()
                for k in range(8):
                    fw.op("pe", "matmul", out=bv(b, slice(0, ns), slice(0, 256)),
                          lhsT=XB.p(k, (ALLP, k, slice(128 * sub, 128 * sub + ns))),
                          rhs=V(w.ap[:, k, :], w.regs, "sb"), start=(k == 0), stop=(k == 7))
                buf, hf = bank(b)
                src = buf.h[0:ns, hf, 0:256].rearrange("p (h e) -> p h e", e=64)
                fw.op("dve", "tensor_copy", out=VR[0:ns, kt0 + sub, 4 * ci:4 * ci + 4, 0:64], in_=buf.v(src, hf))

        for cp in range(4):
            wx = wq.get()
            wg = wq.get()
            for ci in range(2):
                c = 2 * cp + ci
                cs = slice(128 * ci, 128 * ci + 128)
                b = next_bank()
                chain(b, lambda k: V(wx.ap[:, k, cs], wx.regs, "sb"))
                xr = XR[c % 2]
                fw.op("dve", "tensor_copy", out=xr[:, 0:3], in_=HIST[:, c, 0:3])
                fw.op("dve", "tensor_copy", out=xr[:, 3:3 + n], in_=bv(b, ALLP, N))
                fw.op("dve", "tensor_copy", out=HIST[:, c, 0:3], in_=xr[:, n:n + 3])
                b2 = next_bank()
                for j in range(4):
                    fw.op("pe", "matmul", out=bv(b2, ALLP, N), lhsT=CD[:, c, j, :], rhs=xr[:, j:j + n],
                          start=(j == 0), stop=(j == 3))
                xc = Fn()
                fw.op("act", "activation", out=xc[:, N], in_=bv(b2, ALLP, N), func=AF.Identity,
                      bias=pcol(l, C_CB + c))
                br_ = next_bank()
                fw.op("pe", "matmul", out=bv(br_, ALLP, N), lhsT=WRB[:, c, :], rhs=xc[:, N], start=True, stop=True)
                bi_ = next_bank()
                fw.op("pe", "matmul", out=bv(bi_, ALLP, N), lhsT=WIB[:, c, :], rhs=xc[:, N], start=True, stop=True)
                r = Fn()
                fw.op("act", "activation", out=r[:, N], in_=bv(br_, ALLP, N), func=AF.Tanh, scale=0.5, bias=HPB[:, l, c:c + 1])
                ig = Fn()
                fw.op("act", "activation", out=ig[:, N], in_=bv(bi_, ALLP, N), func=AF.Tanh, scale=0.5, bias=HPB[:, l, 8 + c:9 + c])
                a = Fn()
                fw.op("act", "activation", out=a[:, N], in_=r[:, N], func=AF.Exp, scale=HPB[:, l, 16 + c:17 + c], bias=HPB[:, l, 16 + c:17 + c])
                fw.op("dve", "tensor_tensor", out=r[:, N], in0=a[:, N], in1=a[:, N], op=ALU.mult)
                fw.op("dve", "tensor_scalar", out=r[:, N], in0=r[:, N], scalar1=-0.25, scalar2=0.25, op0=ALU.mult, op1=ALU.add)
                fw.op("act", "activation", out=r[:, N], in_=r[:, N], func=AF.Sqrt)
                fw.op("dve", "scalar_tensor_tensor", out=ig[:, N], in0=ig[:, N], scalar=1.0, in1=xc[:, N], op0=ALU.add, op1=ALU.mult)
                fw.op("dve", "tensor_tensor", out=ig[:, N], in0=ig[:, N], in1=r[:, N], op=ALU.mult)
                hh = Fn()
                fw.op("dve", "tensor_tensor_scan", out=hh[:, N], data0=a[:, N], data1=ig[:, N],
                      initial=HST[:, c:c + 1], op0=ALU.mult, op1=ALU.add)
                fw.op("dve", "tensor_copy", out=HST[:, c:c + 1], in_=hh[:, n - 1:n])
                bg_ = next_bank()
                chain(bg_, lambda k: V(wg.ap[:, k, cs], wg.regs, "sb"))
                gg = Fn()
                fw.op("act", "activation", out=gg[:, N], in_=bv(bg_, ALLP, N), func=AF.Gelu_apprx_tanh)
                fw.op("dve", "tensor_tensor", out=AT.p(c, (ALLP, c, N)), in0=hh[:, N], in1=gg[:, N], op=ALU.mult)

        tp = (tap is not None and (l, s, ti) == tuple(tap))
        if tp:
            tapout("QT", AT[:, 16:20, :], [128, 4, 512], BF16)
            tapout("KT", KT[:, :, :], [128, 4, T], BF16)
            tapout("VR", VR[:, :, :, :], [128, NKT, 8, 64], BF16)
            tapout("REC", AT[:, 0:8, :], [128, 8, 512], BF16)
        if ti == 0:
            ktl = [(0, NM, 0, False)]
        else:
            ktl = [(0, NM, 0, False)] + [(kt, 128, 0, False) for kt in range(1, kt0)] + \
                  [(kt0 + d_, 128, 128 * d_, True) for d_ in range(4)]
        acc = PS[2]
        nkt = len(ktl)

        def qk_exp(h, ki):
            g = h // 2
            kt, nk, q0, diag = ktl[ki]
            sp_ = PS[ki % 2]
            kc0 = 0 if kt == 0 else NM + 128 * (kt - 1)
            for m in range(2):
                pb = (h % 2) * 64 + 32 * m
                fw.op("pe", "matmul", out=sp_.p(m, (slice(0, nk), m, slice(q0, n))),
                      lhsT=KT[pb:pb + 32, g, kc0:kc0 + nk],
                      rhs=AT.p(16 + g, (slice(pb, pb + 32), 16 + g, slice(q0, n))),
                      start=True, stop=True, tile_position=(pb, 0))
            pt = PTb[ki % 3]
            fw.op("act", "activation", out=pt[0:nk, :, q0:n], in_=sp_[0:nk, :, q0:n], func=AF.Exp)
            if diag:
                fw.op("dve", "memset", ap=pt[64:128, :, q0:q0 + 64], constant=0.0)

        def pv(h, ki):
            kt, nk, q0, diag = ktl[ki]
            pt = PTb[ki % 3]
            for m in range(2):
                fw.op("pe", "matmul", out=acc.p(m, (slice(0, 64), m, slice(q0, n))),
                      lhsT=VR[0:nk, kt, h, :], rhs=pt[0:nk, m, q0:n],
                      start=(ki == 0), stop=(ki == nkt - 1), tile_position=(0, 0))
                fw.op("pe", "matmul", out=acc.p(m, (slice(64, 128), m, slice(q0, n))),
                      lhsT=ONESB[0:nk, :], rhs=pt[0:nk, m, q0:n],
                      start=(ki == 0), stop=(ki == nkt - 1), tile_position=(0, 64))

        def epi1(h):
            cp = [Fn(), Fn()]
            for m in range(2):
                fw.op("dve", "tensor_copy", out=cp[m][:, N], in_=acc.p(m, (ALLP, m, N)))
            r = [Fn(), Fn()]
            for m in range(2):
                fw.op("dve", "reciprocal", out=r[m][0:64, N], in_=cp[m][64:128, N])
                fw.op("dve", "tensor_tensor", out=r[m][0:64, N], in0=cp[m][0:64, N], in1=r[m][0:64, N], op=ALU.mult)
            o = Fn()
            fw.op("dve", "scalar_tensor_tensor", out=o[0:64, N], in0=r[1][0:64, N], scalar=NLAM[0:64, l:l + 1],
                  in1=r[0][0:64, N], op0=ALU.mult, op1=ALU.add)
            fw.op("dve", "tensor_tensor", out=r[0][0:64, N], in0=o[0:64, N], in1=o[0:64, N], op=ALU.mult)

            def epi2():
                E = PS[3]
                fw.op("pe", "matmul", out=E.p(0, (slice(0, 64), 0, N)), lhsT=ONE64[0:64, 0:64], rhs=r[0][0:64, N],
                      start=True, stop=True)
                fw.op("act", "activation", out=r[1][0:64, N], in_=E.p(0, (slice(0, 64), 0, N)), func=AF.Ln,
                      scale=1.0 / 64, bias=RMS_EPS)
                fw.op("act", "activation", out=r[1][0:64, N], in_=r[1][0:64, N], func=AF.Exp, scale=-0.5)
                fw.op("dve", "scalar_tensor_tensor", out=AT.p(8 + h, (slice(0, 64), 8 + h, N)), in0=o[0:64, N],
                      scalar=GS[:, l:l + 1], in1=r[1][0:64, N], op0=ALU.mult, op1=ALU.mult)
            return epi2

        pending = None
        defer_at = min(6, nkt - 1)
        for h in range(8):
            for ki in range(nkt + 1):
                if ki < nkt:
                    qk_exp(h, ki)
                if ki >= 1:
                    pv(h, ki - 1)
                if ki == defer_at and pending is not None:
                    pending()
                    pending = None
            if pending is not None:
                pending()
            pending = epi1(h)
        pending()

        if tp:
            tapout("ATT", AT[:, 8:16, :], [128, 8, 512], BF16)
        for dp in range(4):
            wao = wq.get()
            wlo = wq.get()
            wga = wq.get()
            wgl = wq.get()
            for di in range(2):
                dt = 2 * dp + di
                cs = slice(128 * di, 128 * di + 128)
                b1 = next_bank()
                chain(b1, lambda k: V(wao.ap[0:64, k, cs], wao.regs, "sb"),
                      rhs_fn=lambda k: AT.p(8 + k, (slice(0, 64), 8 + k, N)))
                b2 = next_bank()
                chain(b2, lambda k: V(wlo.ap[:, k, cs], wlo.regs, "sb"), rhs_fn=lambda k: AT.p(k, (ALLP, k, N)))
                b3 = next_bank()
                chain(b3, lambda k: V(wga.ap[:, k, cs], wga.regs, "sb"))
                sa = Fn()
                fw.op("act", "activation", out=sa[:, N], in_=bv(b3, ALLP, N), func=AF.Sigmoid, bias=pcol(l, C_BG + dt))
                b4 = next_bank()
                chain(b4, lambda k: V(wgl.ap[:, k, cs], wgl.regs, "sb"))
                sl = Fn()
                fw.op("act", "activation", out=sl[:, N], in_=bv(b4, ALLP, N), func=AF.Sigmoid, bias=pcol(l, C_BG + 8 + dt))
                fw.op("dve", "tensor_tensor", out=sa[:, N], in0=sa[:, N], in1=bv(b1, ALLP, N), op=ALU.mult)
                fw.op("dve", "tensor_tensor", out=sl[:, N], in0=sl[:, N], in1=bv(b2, ALLP, N), op=ALU.mult)
                fw.op("dve", "tensor_tensor", out=MT.p(dt, (ALLP, dt, N)), in0=sa[:, N], in1=sl[:, N], op=ALU.add)
        for dp in range(4):
            wo = wq.get()
            for di in range(2):
                dt = 2 * dp + di
                cs = slice(128 * di, 128 * di + 128)
                b = next_bank()
                chain(b, lambda k: V(wo.ap[:, k, cs], wo.regs, "sb"), rhs_fn=lambda k: MT.p(k, (ALLP, k, N)))
                yk = Y.p(dt, (ALLP, dt, N))
                fw.op("dve", "scalar_tensor_tensor", out=yk, in0=yk, scalar=float(alpha), in1=bv(b, ALLP, N),
                      op0=ALU.mult, op1=ALU.add)
        if tp:
            tapout("MT", MT[:, :, :], [128, 8, 512], BF16)
            tapout("Y1", Y[:, :, :], [128, 8, 512], F32)
        ln_fm(n, lambda k: pcol(l, C_LNG + k), lambda k: pcol(l, C_LNB + k), LN_EPS)
        if tp:
            tapout("H1", Y[:, :, :], [128, 8, 512], F32)

        if is_moe:
            b = next_bank()
            for k in range(8):
                fw.op("pe", "matmul", out=bv(b, slice(0, NE), N), lhsT=WRT[:, k, :], rhs=Y.p(k, (ALLP, k, N)),
                      start=(k == 0), stop=(k == 7))
            LGT = Fn()
            fw.op("dve", "tensor_copy", out=LGT[0:NE, N], in_=bv(b, slice(0, NE), N))
            b = next_bank()
            for sub in range(nsub):
                ns = min(128, n - 128 * sub)
                fw.op("pe", "transpose", out=bv(b, slice(0, ns), slice(8 * sub, 8 * sub + 8)),
                      in_=LGT[0:NE, 128 * sub:128 * sub + ns], identity=IDF[0:NE, 0:NE])
            for sub in range(nsub):
                ns = min(128, n - 128 * sub)
                P_ = slice(0, ns)
                fw.op("dve", "tensor_copy", out=LG[P_, sub, :], in_=bv(b, P_, slice(8 * sub, 8 * sub + 8)))
                fw.op("dve", "max", out=M8[P_, sub, :], in_=LG[P_, sub, :])
                fw.op("dve", "tensor_scalar", out=CMB[P_, sub, :], in0=LG[P_, sub, :], scalar1=M8[P_, sub, 1:2],
                      scalar2=None, op0=ALU.is_ge)
                fw.op("dve", "tensor_scalar", out=SM[P_, 0:1], in0=M8[P_, sub, 0:1], scalar1=-1.0, scalar2=None, op0=ALU.mult)
                fw.op("act", "activation", out=LG[P_, sub, :], in_=LG[P_, sub, :], func=AF.Exp, bias=SM[P_, 0:1])
                fw.op("dve", "tensor_tensor", out=CMB[P_, sub, :], in0=CMB[P_, sub, :], in1=LG[P_, sub, :], op=ALU.mult)
                fw.op("dve", "reduce_sum", out=SM[P_, 1:2], in_=CMB[P_, sub, :], axis=AX.X)
                fw.op("dve", "reciprocal", out=SM[P_, 2:3], in_=SM[P_, 1:2])
                fw.op("dve", "tensor_scalar", out=CMB[P_, sub, :], in0=CMB[P_, sub, :], scalar1=SM[P_, 2:3],
                      scalar2=None, op0=ALU.mult)
            b = next_bank()
            for sub in range(nsub):
                ns = min(128, n - 128 * sub)
                fw.op("pe", "transpose", out=bv(b, slice(0, NE), slice(128 * sub, 128 * sub + ns)),
                      in_=CMB[0:ns, sub, :], identity=IDF[0:ns, 0:ns])
            CT = Fp[NF]
            fw.op("dve", "tensor_copy", out=CT[0:NE, N], in_=bv(b, slice(0, NE), N))

        nexp = NE if is_moe else 1
        if is_moe:
            for k in range(8):
                yk = Y.p(k, (ALLP, k, N))
                fw.op("act", "activation", out=yk, in_=yk, func=AF.Copy, scale=float(alpha))
        for e in range(nexp):
            if is_moe:
                bc = next_bank()
                cte = Fn()
                fw.op("dve", "tensor_scalar", out=cte[0:NE, N], in0=CT[0:NE, N], scalar1=IDF[0:NE, e:e + 1],
                      scalar2=None, op0=ALU.mult)
                fw.op("pe", "matmul", out=bv(bc, ALLP, N), lhsT=ONE64[0:NE, :], rhs=cte[0:NE, N], start=True, stop=True)
                cb = CBT
                fw.op("act", "activation", out=cb[:, N], in_=bv(bc, ALLP, N), func=AF.Copy)
            for fp_ in range(NFT // 2):
                wg_ = wq.get()
                wu_ = wq.get()
                for fi in range(2):
                    f = 2 * fp_ + fi
                    cs = slice(128 * fi, 128 * fi + 128)
                    b1 = next_bank()
                    chain(b1, lambda k: V(wg_.ap[:, k, cs], wg_.regs, "sb"))
                    b2 = next_bank()
                    chain(b2, lambda k: V(wu_.ap[:, k, cs], wu_.regs, "sb"))
                    sg = Fn()
                    fw.op("act", "activation", out=sg[:, N], in_=bv(b1, ALLP, N), func=AF.Silu)
                    if is_moe:
                        fw.op("dve", "tensor_tensor", out=sg[:, N], in0=sg[:, N], in1=CBT[:, N], op=ALU.mult)
                    fw.op("dve", "tensor_tensor", out=AT.p(f, (ALLP, f, N)), in0=sg[:, N], in1=bv(b2, ALLP, N), op=ALU.mult)
            for fp_ in range(NFT // 2):
                wd_ = wq.get()
                for fi in range(2):
                    f = 2 * fp_ + fi
                    for dt in range(8):
                        fw.op("pe", "matmul", out=bv(dt, ALLP, N), lhsT=V(wd_.ap[:, fi, 128 * dt:128 * dt + 128], wd_.regs, "sb"),
                              rhs=AT.p(f, (ALLP, f, N)), start=(f == 0), stop=(f == NFT - 1))
            bank_rr[0] = 0
            for dt in range(8):
                yk = Y.p(dt, (ALLP, dt, N))
                if is_moe:
                    fw.op("dve", "tensor_tensor", out=yk, in0=yk, in1=bv(dt, ALLP, N), op=ALU.add)
                else:
                    fw.op("dve", "scalar_tensor_tensor", out=yk, in0=yk, scalar=float(alpha), in1=bv(dt, ALLP, N),
                          op0=ALU.mult, op1=ALU.add)
        bank_rr[0] = 0
        if tp:
            tapout("Y2", Y[:, :, :], [128, 8, 512], F32)
        ln_fm(n, lambda k: pcol(l, C_LNG + 8 + k), lambda k: pcol(l, C_LNB + 8 + k), LN_EPS)
        dbg_store(l + 1, s, ti, n)
        if not last_layer:
            store_h(s, ti, n)
        elif ti > 0:
            for sub in range(nsub):
                ns = min(128, n - 128 * sub)
                xl = XL[sub % 2]
                for half in range(2):
                    b = next_bank()
                    for q in range(4):
                        k = 4 * half + q
                        fw.op("pe", "transpose", out=bv(b, slice(0, ns), slice(128 * q, 128 * q + 128)),
                              in_=Y.p(k, (ALLP, k, slice(128 * sub, 128 * sub + ns))), identity=IDF[:])
                    fw.op("act", "activation", out=xl[0:ns, 512 * half:512 * half + 512], in_=bv(b, slice(0, ns), slice(0, 512)),
                          func=AF.Copy)
                r0 = t0 - NM + 128 * sub
                fw.dma("sp", OUT.v(OUT.h[s, r0:r0 + ns, :], s * NTL + ti), xl[0:ns, :])

    nl = L if stop_after is None else stop_after
    for l in range(nl):
        wq.add(chunks_for(l) * (n_seq * NTL))
    nsteps = n_seq * NTL
    for l in range(nl):
        layer_setup(l)
        per_step = -(-len(cast_jobs.get(l + 1, [])) // max(nsteps - 1, 1))
        for s in range(n_seq):
            fw.op("dve", "memset", ap=HIST[:], constant=0.0)
            fw.op("dve", "memset", ap=HST[:], constant=0.0)
            for ti in range(NTL):
                if l + 1 < nl:
                    run_cast_jobs(l + 1, per_step)
                tile_step(l, s, ti, last_layer=(l == L - 1))
        assert not cast_jobs.get(l + 1) or l + 1 >= nl
    outs = [OUT[:]] + ([DBG[:]] if DBG is not None else []) + [t[:] for t in TAPS.values()]
    fw.finish(outs)
    return nc, fw


_PROG = {}


def kernel(**inputs):
    n_cores = 8
    x = np.ascontiguousarray(inputs["x"], dtype=np.float32)
    B, S, _ = x.shape
    n_seq = B // n_cores
    depth = inputs["w_in"].shape[0]
    alpha = (2 * depth) ** 0.25
    key = (n_seq, S, depth)
    if key not in _PROG:
        _PROG[key] = build_program(n_seq, S, depth, alpha)[0]
    nc = _PROG[key]
    shared = {k: np.ascontiguousarray(inputs[k], dtype=np.float32) for k in WNAMES if k != "x"}
    in_maps = []
    for c in range(n_cores):
        m = dict(shared)
        m["x"] = x[c * n_seq:(c + 1) * n_seq]
        in_maps.append(m)
    res = run_bass_kernel_spmd(nc, in_maps, core_ids=list(range(n_cores)))
    return np.concatenate([r["out"] for r in res.results], axis=0).astype(np.float32)
```

```python
import math
import numpy as np
import concourse.bass as bass
import concourse.mybir as mybir
from concourse.bass_utils import run_bass_kernel_spmd

F32 = mybir.dt.float32
BF16 = mybir.dt.bfloat16
AF = mybir.ActivationFunctionType
ALU = mybir.AluOpType
AX = mybir.AxisListType

D = 1024
NM = 16
DA = 512
FF = 2816
NE = 8
PW = 5632
LN_EPS = 1e-5
RMS_EPS = 1e-5
NFT = FF // 128
EAGER_CAST = False


class Reg:
    __slots__ = ("name", "w", "r", "dsem", "dcnt")

    def __init__(self, name):
        self.name = name
        self.w = None
        self.r = {}
        self.dsem = None
        self.dcnt = 0


class V:
    __slots__ = ("ap", "regs", "kind")

    def __init__(self, ap, regs, kind):
        self.ap = ap
        self.regs = regs
        self.kind = kind


class Buf:
    def __init__(self, name, handle, nreg, kind):
        self.kind = kind
        self.name = name
        self.h = handle
        self.regs = [Reg(f"{name}_{i}") for i in range(nreg)]

    def __getitem__(self, idx):
        return V(self.h[idx], self.regs, self.kind)

    def v(self, ap, ridx=None):
        if ridx is None:
            return V(ap, self.regs, self.kind)
        if isinstance(ridx, int):
            ridx = (ridx,)
        return V(ap, [self.regs[i] for i in ridx], self.kind)

    def p(self, ridx, idx):
        if isinstance(ridx, int):
            ridx = (ridx,)
        return V(self.h[idx], [self.regs[i] for i in ridx], self.kind)


WRITE_KW = ("out", "accum_out", "ap")


class FW:
    ENG = ("pe", "act", "dve", "pool", "sp")

    def __init__(self, nc, targets=None):
        self.nc = nc
        self.targets = targets
        self.rec = {e: set() for e in self.ENG}
        self.semval = {e: 0 for e in self.ENG}
        self.vmap = {e: {} for e in self.ENG}
        self.eng = {"pe": nc.tensor, "act": nc.scalar, "dve": nc.vector,
                    "pool": nc.gpsimd, "sp": nc.sync}
        self.sem = {}
        self.cnt = {}
        self.known = {}
        for e in self.ENG:
            self.sem[e] = nc.alloc_semaphore(name=f"s_{e}")
            self.cnt[e] = 0
            self.known[e] = {}
        self.nsem = len(self.ENG)
        self.nwaits = 0
        self.ninst = 0

    def sb(self, name, shape, dtype, nreg=1):
        return Buf(name, self.nc.alloc_sbuf_tensor(name, list(shape), dtype), nreg, "sb")

    def ps(self, name, shape, dtype=F32, nreg=1):
        return Buf(name, self.nc.alloc_psum_tensor(name, list(shape), dtype), nreg, "ps")

    def dram(self, name, shape, dtype, kind="Internal", nreg=1):
        return Buf(name, self.nc.dram_tensor(name, list(shape), dtype, kind=kind), nreg, "dram")

    def newsem(self, name):
        self.sem[name] = self.nc.alloc_semaphore(name=name)
        self.nsem += 1
        return name

    def _wait(self, e, key, val):
        kn = self.known[e]
        if kn.get(key, 0) >= val:
            return
        if key == e and e == "pe":
            return
        kn[key] = val
        v = val
        if key in self.rec:
            self.rec[key].add(val)
            if self.targets is not None:
                v = self.vmap[key][val]
        self.eng[e].wait_ge(self.sem[key], v)
        self.nwaits += 1

    def _deps(self, e, reads, writes):
        for v in reads:
            for r in v.regs:
                if r.w is not None:
                    self._wait(e, r.w[0], r.w[1])
        for v in writes:
            for r in v.regs:
                if r.w is not None:
                    self._wait(e, r.w[0], r.w[1])
                for k, val in r.r.items():
                    self._wait(e, k, val)

    def _mark(self, ev, reads, writes):
        for v in reads:
            for r in v.regs:
                r.r[ev[0]] = ev[1]
        for v in writes:
            for r in v.regs:
                r.w = ev
                r.r = {}

    def op(self, e, fn, *, reads=(), writes=(), **kw):
        rd = list(reads)
        wr = list(writes)
        args = {}
        for k, v in kw.items():
            if isinstance(v, V):
                (wr if k in WRITE_KW else rd).append(v)
                args[k] = v.ap
            else:
                args[k] = v
        self._deps(e, rd, wr)
        ins = getattr(self.eng[e], fn)(**args)
        self.cnt[e] += 1
        if self.targets is None or self.cnt[e] in self.targets[e]:
            self.semval[e] += 1
            self.vmap[e][self.cnt[e]] = self.semval[e]
            ins.then_inc(self.sem[e], 1)
        self._mark((e, self.cnt[e]), rd, wr)
        self.ninst += 1
        return ins

    def dma(self, q, out, in_, sem_of=None, serialize=True, **kw):
        owner = sem_of
        if owner is None:
            for v in (out, in_):
                if v.kind == "sb":
                    owner = v.regs[0]
                    break
        if owner is None:
            owner = out.regs[0]
        if owner.dsem is None:
            owner.dsem = self.newsem(f"d_{owner.name}")
        self._deps(q, [in_], [out])
        if serialize:
            self._wait(q, owner.dsem, owner.dcnt)
        ins = self.eng[q].dma_start(out=out.ap, in_=in_.ap, **kw)
        owner.dcnt += 16
        ins.then_inc(self.sem[owner.dsem], 16)
        self._mark((owner.dsem, owner.dcnt), [in_], [out])
        self.ninst += 1
        return ins

    def finish(self, outs, q="sp"):
        for v in outs:
            for r in v.regs:
                if r.w is not None:
                    self._wait(q, r.w[0], r.w[1])


WNAMES = ["x", "meta_tokens", "ln_in_g", "ln_in_b", "w_in", "b_gate", "da_lambda",
          "da_subln_g", "conv_w", "conv_b", "lru_wr", "lru_br", "lru_wi", "lru_bi",
          "lru_lambda", "w_attn_out", "w_lru_out", "w_o", "ln_g", "ln_b", "ffn_wg",
          "ffn_wu", "ffn_wd", "router_w", "moe_wg", "moe_wu", "moe_wd"]


def build_program(n_seq, seq, depth, alpha, dbg_layers=False, stop_after=None, tap=None):
    _, fw1 = _build_program(None, n_seq, seq, depth, alpha, dbg_layers, stop_after, tap)
    return _build_program(fw1.rec, n_seq, seq, depth, alpha, dbg_layers, stop_after, tap)


def _build_program(targets, n_seq, seq, depth, alpha, dbg_layers=False, stop_after=None, tap=None):
    nc = bass.Bass("TRN2", target_bir_lowering=False)
    fw = FW(nc, targets)
    L = depth
    Nd = (L + 1) // 2
    Nm = L // 2
    T = NM + seq
    NX = seq // 512
    assert seq % 512 == 0
    tiles = [(0, NM)] + [(NM + 512 * i, 512) for i in range(NX)]
    NTL = len(tiles)
    NKT = 1 + seq // 128

    shapes = {
        "x": [n_seq, seq, D], "meta_tokens": [NM, D], "ln_in_g": [D], "ln_in_b": [D],
        "w_in": [L, D, PW], "b_gate": [L, 2, D], "da_lambda": [L, 4, 32],
        "da_subln_g": [L, 64], "conv_w": [L, 4, D], "conv_b": [L, D],
        "lru_wr": [L, 16, 64, 64], "lru_br": [L, D], "lru_wi": [L, 16, 64, 64],
        "lru_bi": [L, D], "lru_lambda": [L, D], "w_attn_out": [L, DA, D],
        "w_lru_out": [L, D, D], "w_o": [L, D, D], "ln_g": [L, 2, D], "ln_b": [L, 2, D],
        "ffn_wg": [Nd, D, FF], "ffn_wu": [Nd, D, FF], "ffn_wd": [Nd, FF, D],
        "router_w": [max(Nm, 1), D, NE], "moe_wg": [max(Nm, 1), NE, D, FF],
        "moe_wu": [max(Nm, 1), NE, D, FF], "moe_wd": [max(Nm, 1), NE, FF, D],
    }
    I = {k: fw.dram(k, shapes[k], F32, kind="ExternalInput") for k in WNAMES}
    OUT = fw.dram("out", [n_seq, seq, D], F32, kind="ExternalOutput", nreg=n_seq * NTL)
    H32 = fw.dram("h32", [n_seq, D, T], F32, nreg=n_seq * NTL)
    HB = fw.dram("hb", [n_seq, D, T], BF16, nreg=n_seq * NTL)
    DBG = None
    if dbg_layers:
        DBG = fw.dram("dbg", [L + 1, n_seq, D, T], F32, kind="ExternalOutput")
    TAPS = {}

    def tapout(name, view, shape, dtype):
        if tap is None:
            return
        if name not in TAPS:
            TAPS[name] = fw.dram("tap_" + name, shape, dtype, kind="ExternalOutput")
        fw.dma("sp", TAPS[name][:], view)
    WB = {
        "w_in": fw.dram("wb_in", [L, D, PW], BF16, nreg=L),
        "w_attn_out": fw.dram("wb_ao", [L, DA, D], BF16, nreg=L),
        "w_lru_out": fw.dram("wb_lo", [L, D, D], BF16, nreg=L),
        "w_o": fw.dram("wb_o", [L, D, D], BF16, nreg=L),
        "ffn_wg": fw.dram("wb_fg", [Nd, D, FF], BF16, nreg=Nd),
        "ffn_wu": fw.dram("wb_fu", [Nd, D, FF], BF16, nreg=Nd),
        "ffn_wd": fw.dram("wb_fd", [Nd, FF, D], BF16, nreg=Nd),
        "moe_wg": fw.dram("wb_mg", [max(Nm, 1), NE, D, FF], BF16, nreg=max(Nm, 1) * NE),
        "moe_wu": fw.dram("wb_mu", [max(Nm, 1), NE, D, FF], BF16, nreg=max(Nm, 1) * NE),
        "moe_wd": fw.dram("wb_md", [max(Nm, 1), NE, FF, D], BF16, nreg=max(Nm, 1) * NE),
    }

    KT = fw.sb("KT", [128, 4, T], BF16)
    VR = fw.sb("VR", [128, NKT, 512], BF16)
    ONESB = fw.sb("ONESB", [128, 64], BF16)
    Y = fw.sb("Y", [128, 8, 512], F32, nreg=8)
    XBS = [fw.sb("XB0", [128, 8, 512], BF16, nreg=8), fw.sb("XB1", [128, 8, 512], BF16, nreg=8)]
    xb_cur = [XBS[0]]
    AT = fw.sb("AT", [128, NFT, 512], BF16, nreg=NFT)
    MT = fw.sb("MT", [128, 8, 512], BF16, nreg=8)
    NF = 8
    Fp = [fw.sb(f"F{i}", [128, 512], F32) for i in range(NF + 1)]
    LNR = fw.sb("LNR", [128, 512], F32)
    CBT = fw.sb("CBT", [128, 512], F32)
    XR = [fw.sb(f"XR{i}", [128, 516], BF16) for i in range(2)]
    PTb = [fw.sb(f"PT{i}", [128, 2, 512], BF16) for i in range(3)]
    NSLOT = 6
    RING = [fw.sb(f"RG{i}", [128, 2048], BF16) for i in range(NSLOT)]
    WRB = fw.sb("WRB", [128, 8, 128], F32)
    WIB = fw.sb("WIB", [128, 8, 128], F32)
    CD = fw.sb("CD", [128, 8, 4, 128], BF16)
    WRT = fw.sb("WRT", [128, 8, NE], F32)
    PTB = fw.sb("PTB", [128, L, 112], F32)
    LNIN = fw.sb("LNIN", [128, 16], F32)
    CNEG = fw.sb("CNEG", [128, L, 8], F32)
    HPB = fw.sb("HPB", [128, L, 24], F32)
    STG = fw.sb("STG", [128, 128], F32)
    IDF = fw.sb("IDF", [128, 128], F32)
    IDB = fw.sb("IDB", [128, 128], BF16)
    ONESD = fw.sb("ONESD", [128, 128], F32)
    ONE64 = fw.sb("ONE64", [128, 128], F32)
    HIST = fw.sb("HIST", [128, 8, 4], BF16)
    HST = fw.sb("HST", [128, 8], F32)
    LAMW = fw.sb("LAMW", [128, 4, 32], F32)
    LAMS = fw.sb("LAMS", [128, 8], F32)
    NLAM = fw.sb("NLAM", [128, L], F32)
    GS = fw.sb("GS", [64, L], F32)
    LG = fw.sb("LG", [128, 4, NE], F32)
    M8 = fw.sb("M8", [128, 4, 8], F32)
    CMB = fw.sb("CMB", [128, 4, NE], F32)
    SM = fw.sb("SM", [128, 16], F32)

    PS = [fw.ps(f"PS{i}", [128, 2, 512], F32, nreg=2) for i in range(4)]
    bank_rr = [0]

    def bank(b):
        return PS[b // 2], b % 2

    def next_bank():
        b = bank_rr[0]
        bank_rr[0] = (b + 1) % 8
        return b

    def bv(b, pr, cols):
        buf, hf = bank(b)
        return buf.p(hf, (pr, hf, cols))

    f_rr = [0]

    def Fn():
        i = f_rr[0]
        f_rr[0] = (i + 1) % NF
        return Fp[i]

    ALLP = slice(0, 128)

    fw.op("pool", "memset", ap=IDF[:], constant=0.0)
    fw.op("pool", "affine_select", out=IDF[:], in_=IDF[:], pattern=[[-1, 128]],
          compare_op=ALU.not_equal, fill=1.0, base=0, channel_multiplier=1)
    fw.op("pool", "tensor_copy", out=IDB[:], in_=IDF[:])
    fw.op("pool", "memset", ap=ONESD[:], constant=1.0 / D)
    fw.op("pool", "memset", ap=ONE64[:], constant=1.0)
    fw.op("pool", "memset", ap=WRB[:], constant=0.0)
    fw.op("pool", "memset", ap=WIB[:], constant=0.0)
    fw.op("pool", "memset", ap=ONESB[:], constant=1.0)

    def stage_rows(dst_v, loads):
        r0 = 0
        for src_ap, nrows in loads:
            fw.dma("sp", STG[r0:r0 + nrows, :], src_ap)
            r0 += nrows
        b = next_bank()
        fw.op("pe", "transpose", out=bv(b, ALLP, slice(0, r0)), in_=STG[0:r0, :], identity=IDF[0:r0, 0:r0])
        fw.op("dve", "tensor_copy", out=dst_v, in_=bv(b, ALLP, slice(0, r0)))

    def rows(buf, ap, n):
        return (buf.v(ap.rearrange("(r p) -> r p", p=128)), n)

    for l in range(L):
        loads = [
            (I["b_gate"].v(I["b_gate"].h[l].rearrange("i (r p) -> (i r) p", p=128)), 16),
            (I["conv_w"].v(I["conv_w"].h[l].rearrange("i (r p) -> (i r) p", p=128)), 32),
            rows(I["conv_b"], I["conv_b"].h[l], 8),
            rows(I["lru_br"], I["lru_br"].h[l], 8),
            rows(I["lru_bi"], I["lru_bi"].h[l], 8),
            rows(I["lru_lambda"], I["lru_lambda"].h[l], 8),
            (I["ln_g"].v(I["ln_g"].h[l].rearrange("i (r p) -> (i r) p", p=128)), 16),
            (I["ln_b"].v(I["ln_b"].h[l].rearrange("i (r p) -> (i r) p", p=128)), 16),
        ]
        stage_rows(PTB[:, l, :], loads)
    stage_rows(LNIN[:, :], [rows(I["ln_in_g"], I["ln_in_g"].h[:], 8), rows(I["ln_in_b"], I["ln_in_b"].h[:], 8)])
    C_BG, C_CW, C_CB, C_BR, C_BI, C_LAM, C_LNG, C_LNB = 0, 16, 48, 56, 64, 72, 80, 96

    def pcol(l, c):
        return PTB[:, l, c:c + 1]

    for l in range(L):
        t = Fn()
        fw.op("act", "activation", out=t[:, 0:8], in_=PTB[:, l, C_LAM:C_LAM + 8], func=AF.Exp, scale=-1.0)
        fw.op("act", "activation", out=t[:, 8:16], in_=t[:, 0:8], func=AF.Ln, bias=1.0)
        fw.op("dve", "tensor_scalar", out=CNEG[:, l, :], in0=t[:, 8:16], scalar1=-8.0, scalar2=None, op0=ALU.mult)
        fw.op("dve", "tensor_scalar", out=HPB[:, l, 16:24], in0=t[:, 8:16], scalar1=-4.0, scalar2=None, op0=ALU.mult)
        fw.op("dve", "tensor_scalar", out=HPB[:, l, 0:16], in0=PTB[:, l, C_BR:C_BR + 16], scalar1=0.5, scalar2=None, op0=ALU.mult)
    for l in range(L):
        lam_init = 0.8 - 0.6 * math.exp(-0.3 * l)
        src = bass.AP(I["da_lambda"].h, l * 128, [[0, 128], [1, 128]])
        fw.dma("sp", LAMW.v(LAMW.h[:].rearrange("p a b -> p (a b)")), I["da_lambda"].v(src))
        fw.op("dve", "tensor_tensor", out=LAMW[:, 0, :], in0=LAMW[:, 0, :], in1=LAMW[:, 1, :], op=ALU.mult)
        fw.op("dve", "tensor_tensor", out=LAMW[:, 2, :], in0=LAMW[:, 2, :], in1=LAMW[:, 3, :], op=ALU.mult)
        fw.op("dve", "reduce_sum", out=LAMS[:, 0:1], in_=LAMW[:, 0, :], axis=AX.X)
        fw.op("dve", "reduce_sum", out=LAMS[:, 1:2], in_=LAMW[:, 2, :], axis=AX.X)
        fw.op("act", "activation", out=LAMS[:, 2:4], in_=LAMS[:, 0:2], func=AF.Exp)
        fw.op("dve", "tensor_tensor", out=LAMS[:, 4:5], in0=LAMS[:, 3:4], in1=LAMS[:, 2:3], op=ALU.subtract)
        fw.op("dve", "tensor_scalar", out=NLAM[:, l:l + 1], in0=LAMS[:, 4:5], scalar1=-lam_init, scalar2=None, op0=ALU.add)
        fw.dma("sp", GS[:, l:l + 1], I["da_subln_g"].v(I["da_subln_g"].h[l].rearrange("(p o) -> p o", o=1)))
        fw.op("dve", "tensor_scalar", out=GS[:, l:l + 1], in0=GS[:, l:l + 1], scalar1=1.0 - lam_init, scalar2=None, op0=ALU.mult)

    cast_ev = []
    CAST_INFLIGHT = 2
    cast_jobs = {l_: [] for l_ in range(L)}

    def cast2d(jobs, dst_buf, dst_ap, src_buf, src_ap, reg, rows_, cols):
        c = cols
        for cand in (2048, 1408, 1024, 512):
            if cols % cand == 0:
                c = cand
                break
        rstep = 256 if rows_ % 256 == 0 and cols > 2048 else rows_
        for r0 in range(0, rows_, rstep):
            d = dst_ap[r0:r0 + rstep, :].rearrange("r (a c) -> r a c", c=c)
            s_ = src_ap[r0:r0 + rstep, :].rearrange("r (a c) -> r a c", c=c)

            def job(d=d, s_=s_):
                if len(cast_ev) >= CAST_INFLIGHT:
                    fw._wait("pool", *cast_ev[-CAST_INFLIGHT])
                fw.dma("pool", dst_buf.v(d, reg), src_buf.v(s_), sem_of=dst_buf.regs[reg], serialize=False)
                cast_ev.append(dst_buf.regs[reg].w)
            jobs.append(job)

    for l in range(L):
        J = cast_jobs[l]
        cast2d(J, WB["w_in"], WB["w_in"].h[l], I["w_in"], I["w_in"].h[l], l, D, PW)
        cast2d(J, WB["w_attn_out"], WB["w_attn_out"].h[l], I["w_attn_out"], I["w_attn_out"].h[l], l, DA, D)
        cast2d(J, WB["w_lru_out"], WB["w_lru_out"].h[l], I["w_lru_out"], I["w_lru_out"].h[l], l, D, D)
        cast2d(J, WB["w_o"], WB["w_o"].h[l], I["w_o"], I["w_o"].h[l], l, D, D)
        i = l // 2
        if l % 2 == 0:
            cast2d(J, WB["ffn_wg"], WB["ffn_wg"].h[i], I["ffn_wg"], I["ffn_wg"].h[i], i, D, FF)
            cast2d(J, WB["ffn_wu"], WB["ffn_wu"].h[i], I["ffn_wu"], I["ffn_wu"].h[i], i, D, FF)
            cast2d(J, WB["ffn_wd"], WB["ffn_wd"].h[i], I["ffn_wd"], I["ffn_wd"].h[i], i, FF, D)
        else:
            for e in range(NE):
                cast2d(J, WB["moe_wg"], WB["moe_wg"].h[i, e], I["moe_wg"], I["moe_wg"].h[i, e], i * NE + e, D, FF)
                cast2d(J, WB["moe_wu"], WB["moe_wu"].h[i, e], I["moe_wu"], I["moe_wu"].h[i, e], i * NE + e, D, FF)
                cast2d(J, WB["moe_wd"], WB["moe_wd"].h[i, e], I["moe_wd"], I["moe_wd"].h[i, e], i * NE + e, FF, D)

    def run_cast_jobs(l_, k=None):
        J = cast_jobs.get(l_)
        if not J:
            return
        k = len(J) if k is None else min(k, len(J))
        for _ in range(k):
            J.pop(0)()

    for l_ in range(L if EAGER_CAST else 1):
        run_cast_jobs(l_)

    class WStream:
        def __init__(self):
            self.plan = []
            self.issued = 0
            self.taken = 0

        def add(self, specs):
            self.plan.extend(specs)

        def _issue(self, i):
            buf, reg, ap, shape = self.plan[i]
            slot = RING[i % NSLOT]
            n = 1
            for s_ in shape[1:]:
                n *= s_
            dst = slot.h[0:shape[0], 0:n]
            if len(shape) == 3:
                dst = dst.rearrange("p (a b) -> p a b", b=shape[2])
            fw.dma("sp", slot.v(dst), buf.v(ap, reg))

        def get(self, live=4):
            i = self.taken
            assert i < len(self.plan), "weight plan exhausted"
            while self.issued < min(len(self.plan), i + NSLOT - live + 1):
                self._issue(self.issued)
                self.issued += 1
            self.taken += 1
            buf, reg, ap, shape = self.plan[i]
            slot = RING[i % NSLOT]
            n = 1
            for s_ in shape[1:]:
                n *= s_
            v = slot.h[0:shape[0], 0:n]
            if len(shape) == 3:
                v = v.rearrange("p (a b) -> p a b", b=shape[2])
            return slot.v(v)

    wq = WStream()

    def kcols(buf, reg, mat_ap, c0, ncol, krows=128):
        ap = mat_ap[:, c0:c0 + ncol].rearrange("(k p) c -> p k c", p=krows)
        return (buf, reg, ap, [krows, ap.shape[1], ncol])

    def chunks_for(l):
        sp = []
        win = WB["w_in"].h[l]
        for ci in range(4):
            sp.append(kcols(WB["w_in"], l, win, 256 * ci, 256))
        for ci in range(2):
            sp.append(kcols(WB["w_in"], l, win, 1024 + 256 * ci, 256))
        for cp in range(4):
            sp.append(kcols(WB["w_in"], l, win, 1536 + 256 * cp, 256))
            sp.append(kcols(WB["w_in"], l, win, 2560 + 256 * cp, 256))
        for dp in range(4):
            sp.append(kcols(WB["w_attn_out"], l, WB["w_attn_out"].h[l], 256 * dp, 256, krows=64))
            sp.append(kcols(WB["w_lru_out"], l, WB["w_lru_out"].h[l], 256 * dp, 256))
            sp.append(kcols(WB["w_in"], l, win, 3584 + 256 * dp, 256))
            sp.append(kcols(WB["w_in"], l, win, 4608 + 256 * dp, 256))
        for dp in range(4):
            sp.append(kcols(WB["w_o"], l, WB["w_o"].h[l], 256 * dp, 256))
        i = l // 2
        if l % 2 == 0:
            exps = [(WB["ffn_wg"], WB["ffn_wu"], WB["ffn_wd"], i, WB["ffn_wg"].h[i], WB["ffn_wu"].h[i], WB["ffn_wd"].h[i])]
        else:
            exps = [(WB["moe_wg"], WB["moe_wu"], WB["moe_wd"], i * NE + e, WB["moe_wg"].h[i, e],
                     WB["moe_wu"].h[i, e], WB["moe_wd"].h[i, e]) for e in range(NE)]
        for (bg_, bu_, bd_, reg, g_ap, u_ap, d_ap) in exps:
            for fp_ in range(NFT // 2):
                sp.append(kcols(bg_, reg, g_ap, 256 * fp_, 256))
                sp.append(kcols(bu_, reg, u_ap, 256 * fp_, 256))
            for fp_ in range(NFT // 2):
                ap = d_ap[256 * fp_:256 * fp_ + 256, :].rearrange("(f p) c -> p f c", p=128)
                sp.append((bd_, reg, ap, [128, 2, D]))
        return sp

    def hview(buf, s, ti):
        t0, n = tiles[ti]
        ap = buf.h[s].rearrange("(k p) t -> p k t", p=128)[:, :, t0:t0 + n]
        return buf.v(ap, s * NTL + ti)

    def ln_fm(n, gcol, bcol, eps):
        bA = next_bank()
        bB = next_bank()
        for k in range(8):
            sq = Fn()
            fw.op("act", "activation", out=sq[:, 0:n], in_=Y.p(k, (ALLP, k, slice(0, n))), func=AF.Square)
            fw.op("pe", "matmul", out=bv(bA, ALLP, slice(0, n)), lhsT=ONESD[:], rhs=Y.p(k, (ALLP, k, slice(0, n))),
                  start=(k == 0), stop=(k == 7))
            fw.op("pe", "matmul", out=bv(bB, ALLP, slice(0, n)), lhsT=ONESD[:], rhs=sq[:, 0:n],
                  start=(k == 0), stop=(k == 7))
        msq = Fn()
        fw.op("act", "activation", out=msq[:, 0:n], in_=bv(bA, ALLP, slice(0, n)), func=AF.Square)
        var = Fn()
        fw.op("dve", "tensor_tensor", out=var[:, 0:n], in0=bv(bB, ALLP, slice(0, n)), in1=msq[:, 0:n], op=ALU.subtract)
        fw.op("dve", "tensor_scalar", out=var[:, 0:n], in0=var[:, 0:n], scalar1=0.0, scalar2=eps, op0=ALU.max, op1=ALU.add)
        fw.op("act", "activation", out=var[:, 0:n], in_=var[:, 0:n], func=AF.Sqrt)
        rstd = LNR
        fw.op("dve", "reciprocal", out=rstd[:, 0:n], in_=var[:, 0:n])
        for k in range(8):
            t = Fn()
            yk = Y.p(k, (ALLP, k, slice(0, n)))
            fw.op("dve", "tensor_tensor", out=t[:, 0:n], in0=yk, in1=bv(bA, ALLP, slice(0, n)), op=ALU.subtract)
            fw.op("dve", "tensor_tensor", out=t[:, 0:n], in0=t[:, 0:n], in1=rstd[:, 0:n], op=ALU.mult)
            fw.op("act", "activation", out=yk, in_=t[:, 0:n], func=AF.Identity, scale=gcol(k), bias=bcol(k))
            fw.op("dve", "tensor_copy", out=xb_cur[0].p(k, (ALLP, k, slice(0, n))), in_=yk)

    def store_h(s, ti, n):
        fw.dma("sp", hview(H32, s, ti), Y[:, :, 0:n])
        fw.dma("sp", hview(HB, s, ti), xb_cur[0][:, :, 0:n])

    def dbg_store(li, s, ti, n):
        if DBG is not None:
            t0, _ = tiles[ti]
            ap = DBG.h[li, s].rearrange("(k p) t -> p k t", p=128)[:, :, t0:t0 + n]
            fw.dma("sp", DBG.v(ap), Y[:, :, 0:n])

    for s in range(n_seq):
        for ti, (t0, n) in enumerate(tiles):
            nsub = (n + 127) // 128
            for sub in range(nsub):
                ns = min(128, n - 128 * sub)
                xl2 = [Fn(), Fn()]
                for half in range(2):
                    hs = slice(512 * half, 512 * half + 512)
                    if ti == 0:
                        fw.dma("sp", xl2[half][0:ns, :], I["meta_tokens"][0:ns, hs])
                    else:
                        r0 = t0 - NM + 128 * sub
                        fw.dma("sp", xl2[half][0:ns, :], I["x"].v(I["x"].h[s, r0:r0 + ns, hs]))
                for half in range(2):
                    b = next_bank()
                    for q in range(4):
                        k = 4 * half + q
                        fw.op("pe", "transpose", out=bv(b, ALLP, slice(128 * q, 128 * q + ns)),
                              in_=xl2[half][0:ns, 128 * q:128 * q + 128], identity=IDF[0:ns, 0:ns])
                    buf, hf = bank(b)
                    src = buf.p(hf, (ALLP, hf, slice(0, 512))).ap.rearrange("p (q t) -> p q t", t=128)[:, :, 0:ns]
                    dst = Y.h[:, 4 * half:4 * half + 4, 128 * sub:128 * sub + ns]
                    fw.op("dve", "tensor_copy", out=Y.v(dst, range(4 * half, 4 * half + 4)), in_=buf.v(src, hf))
            ln_fm(n, lambda k: LNIN[:, k:k + 1], lambda k: LNIN[:, 8 + k:9 + k], LN_EPS)
            store_h(s, ti, n)
            dbg_store(0, s, ti, n)

    QSCALE = 32 ** -0.5

    def layer_setup(l):
        for (dst, src) in ((WRB, I["lru_wr"]), (WIB, I["lru_wi"])):
            for hh in range(2):
                sap = src.h[l].rearrange("(c hh) i j -> hh i c j", hh=2)[hh]
                fw.dma("sp", dst[64 * hh:64 * hh + 64, :, 64 * hh:64 * hh + 64], src.v(sap))
        for c in range(8):
            for j in range(4):
                fw.op("dve", "tensor_scalar", out=CD[:, c, j, :], in0=IDB[:], scalar1=pcol(l, C_CW + 8 * j + c),
                      scalar2=None, op0=ALU.mult)
        if l % 2 == 1:
            ap = I["router_w"].h[l // 2].rearrange("(k p) e -> p k e", p=128)
            fw.dma("sp", WRT[:], I["router_w"].v(ap))

    xb_loaded = [False]

    def tile_step(l, s, ti, last_layer, nxt=None):
        t0, n = tiles[ti]
        nsub = (n + 127) // 128
        N = slice(0, n)
        is_moe = (l % 2 == 1)
        fw.dma("sp", Y[:, :, 0:n], hview(H32, s, ti))
        XB = xb_cur[0]
        if not xb_loaded[0]:
            fw.dma("sp", XB[:, :, 0:n], hview(HB, s, ti))
        xb_loaded[0] = False
        kt0 = 0 if ti == 0 else 1 + 4 * (ti - 1)

        def chain(b, lhs_fn, nk=8, rhs_fn=None, pr=ALLP):
            for k in range(nk):
                rhs = rhs_fn(k) if rhs_fn else XB.p(k, (ALLP, k, N))
                fw.op("pe", "matmul", out=bv(b, pr, N), lhsT=lhs_fn(k), rhs=rhs, start=(k == 0), stop=(k == nk - 1))

        for ci in range(4):
            w = wq.get(live=1)
            for gi in range(2):
                g = (ci % 2) * 2 + gi
                b = next_bank()
                chain(b, lambda k: V(w.ap[:, k, 128 * gi:128 * gi + 128], w.regs, "sb"))
                if ci < 2:
                    fw.op("act", "activation", out=AT.p(16 + g, (ALLP, 16 + g, N)), in_=bv(b, ALLP, N),
                          func=AF.Copy, scale=QSCALE)
                else:
                    fw.op("dve", "tensor_copy", out=KT[:, g, t0:t0 + n], in_=bv(b, ALLP, N))
        for ci in range(2):
            w = wq.get(live=1)
            for sub in range(nsub):
                ns = min(128, n - 128 * sub)
                b = next_bank()
                for k in range(8):
                    fw.op("pe", "matmul", out=bv(b, slice(0, ns), slice(0, 256)),
                          lhsT=XB.p(k, (ALLP, k, slice(128 * sub, 128 * sub + ns))),
                          rhs=V(w.ap[:, k, :], w.regs, "sb"), start=(k == 0), stop=(k == 7))
                buf, hf = bank(b)
                fw.op("dve", "tensor_copy", out=VR[0:ns, kt0 + sub, 256 * ci:256 * ci + 256], in_=bv(b, slice(0, ns), slice(0, 256)))

        lru_w = {}
        lru_t = {}

        def lru_front(c):
            cp, ci = divmod(c, 2)
            if ci == 0:
                lru_w[("x", cp)] = wq.get(live=3)
            wx = lru_w[("x", cp)]
            cs = slice(128 * ci, 128 * ci + 128)
            b = next_bank()
            chain(b, lambda k: V(wx.ap[:, k, cs], wx.regs, "sb"))
            xr = XR[c % 2]
            fw.op("dve", "tensor_copy", out=xr[:, 0:3], in_=HIST[:, c, 0:3])
            fw.op("dve", "tensor_copy", out=xr[:, 3:3 + n], in_=bv(b, ALLP, N))
            fw.op("dve", "tensor_copy", out=HIST[:, c, 0:3], in_=xr[:, n:n + 3])
            b2 = next_bank()
            for j in range(4):
                fw.op("pe", "matmul", out=bv(b2, ALLP, N), lhsT=CD[:, c, j, :], rhs=xr[:, j:j + n],
                      start=(j == 0), stop=(j == 3))
            xc = Fn()
            fw.op("act", "activation", out=xc[:, N], in_=bv(b2, ALLP, N), func=AF.Identity,
                  bias=pcol(l, C_CB + c))
            br_ = next_bank()
            fw.op("pe", "matmul", out=bv(br_, ALLP, N), lhsT=WRB[:, c, :], rhs=xc[:, N], start=True, stop=True)
            bi_ = next_bank()
            fw.op("pe", "matmul", out=bv(bi_, ALLP, N), lhsT=WIB[:, c, :], rhs=xc[:, N], start=True, stop=True)
            r = Fn()
            fw.op("act", "activation", out=r[:, N], in_=bv(br_, ALLP, N), func=AF.Tanh, scale=0.5, bias=HPB[:, l, c:c + 1])
            ig = Fn()
            fw.op("act", "activation", out=ig[:, N], in_=bv(bi_, ALLP, N), func=AF.Tanh, scale=0.5, bias=HPB[:, l, 8 + c:9 + c])
            a = Fn()
            fw.op("act", "activation", out=a[:, N], in_=r[:, N], func=AF.Exp, scale=HPB[:, l, 16 + c:17 + c], bias=HPB[:, l, 16 + c:17 + c])
            lru_t[c] = (xc, r, ig, a)

        def lru_back(c):
            cp, ci = divmod(c, 2)
            if ci == 0:
                lru_w[("g", cp)] = wq.get(live=3)
            wg = lru_w[("g", cp)]
            cs = slice(128 * ci, 128 * ci + 128)
            xc, r, ig, a = lru_t.pop(c)
            fw.op("dve", "tensor_tensor", out=r[:, N], in0=a[:, N], in1=a[:, N], op=ALU.mult)
            fw.op("dve", "tensor_scalar", out=r[:, N], in0=r[:, N], scalar1=-0.25, scalar2=0.25, op0=ALU.mult, op1=ALU.add)
            fw.op("act", "activation", out=r[:, N], in_=r[:, N], func=AF.Sqrt)
            fw.op("dve", "scalar_tensor_tensor", out=ig[:, N], in0=ig[:, N], scalar=1.0, in1=xc[:, N], op0=ALU.add, op1=ALU.mult)
            fw.op("dve", "tensor_tensor", out=ig[:, N], in0=ig[:, N], in1=r[:, N], op=ALU.mult)
            hh = xc
            fw.op("dve", "tensor_tensor_scan", out=hh[:, N], data0=a[:, N], data1=ig[:, N],
                  initial=HST[:, c:c + 1], op0=ALU.mult, op1=ALU.add)
            fw.op("dve", "tensor_copy", out=HST[:, c:c + 1], in_=hh[:, n - 1:n])
            bg_ = next_bank()
            chain(bg_, lambda k: V(wg.ap[:, k, cs], wg.regs, "sb"))
            gg = r
            fw.op("act", "activation", out=gg[:, N], in_=bv(bg_, ALLP, N), func=AF.Gelu_apprx_tanh)
            fw.op("dve", "tensor_tensor", out=AT.p(c, (ALLP, c, N)), in0=hh[:, N], in1=gg[:, N], op=ALU.mult)

        for c in range(9):
            if c < 8:
                lru_front(c)
            if c >= 1:
                lru_back(c - 1)

        tp = (tap is not None and (l, s, ti) == tuple(tap))
        if tp:
            tapout("QT", AT[:, 16:20, :], [128, 4, 512], BF16)
            tapout("KT", KT[:, :, :], [128, 4, T], BF16)
            tapout("VR", VR[:, :, 0:512], [128, NKT, 512], BF16)
            tapout("REC", AT[:, 0:8, :], [128, 8, 512], BF16)
        if ti == 0:
            ktl = [(0, NM, 0, False)]
        else:
            ktl = [(0, NM, 0, False)] + [(kt, 128, 0, False) for kt in range(1, kt0)] + \
                  [(kt0 + d_, 128, 128 * d_, True) for d_ in range(4)]
        acc = PS[2]
        nkt = len(ktl)

        def qk_exp(h, ki):
            g = h // 2
            kt, nk, q0, diag = ktl[ki]
            sp_ = PS[ki % 2]
            kc0 = 0 if kt == 0 else NM + 128 * (kt - 1)
            for m in range(2):
                pb = (h % 2) * 64 + 32 * m
                fw.op("pe", "matmul", out=sp_.p(m, (slice(0, nk), m, slice(q0, n))),
                      lhsT=KT[pb:pb + 32, g, kc0:kc0 + nk],
                      rhs=AT.p(16 + g, (slice(pb, pb + 32), 16 + g, slice(q0, n))),
                      start=True, stop=True, tile_position=(pb, 0))
            pt = PTb[ki % 3]
            fw.op("act", "activation", out=pt[0:nk, :, q0:n], in_=sp_[0:nk, :, q0:n], func=AF.Exp)
            if diag:
                fw.op("dve", "memset", ap=pt[64:128, :, q0:q0 + 64], constant=0.0)

        def pv(h, ki):
            kt, nk, q0, diag = ktl[ki]
            pt = PTb[ki % 3]
            for m in range(2):
                fw.op("pe", "matmul", out=acc.p(m, (slice(0, 64), m, slice(q0, n))),
                      lhsT=VR[0:nk, kt, 64 * h:64 * h + 64], rhs=pt[0:nk, m, q0:n],
                      start=(ki == 0), stop=(ki == nkt - 1), tile_position=(0, 0))
                fw.op("pe", "matmul", out=acc.p(m, (slice(64, 128), m, slice(q0, n))),
                      lhsT=ONESB[0:nk, :], rhs=pt[0:nk, m, q0:n],
                      start=(ki == 0), stop=(ki == nkt - 1), tile_position=(0, 64))

        def epi1(h):
            cp = [Fn(), Fn()]
            for m in range(2):
                fw.op("dve", "tensor_copy", out=cp[m][:, N], in_=acc.p(m, (ALLP, m, N)))
            r = [Fn(), Fn()]
            for m in range(2):
                fw.op("dve", "reciprocal", out=r[m][0:64, N], in_=cp[m][64:128, N])
                fw.op("dve", "tensor_tensor", out=r[m][0:64, N], in0=cp[m][0:64, N], in1=r[m][0:64, N], op=ALU.mult)
            o = Fn()
            fw.op("dve", "scalar_tensor_tensor", out=o[0:64, N], in0=r[1][0:64, N], scalar=NLAM[0:64, l:l + 1],
                  in1=r[0][0:64, N], op0=ALU.mult, op1=ALU.add)
            fw.op("dve", "tensor_tensor", out=r[0][0:64, N], in0=o[0:64, N], in1=o[0:64, N], op=ALU.mult)

            def epi2():
                E = PS[3]
                fw.op("pe", "matmul", out=E.p(0, (slice(0, 64), 0, N)), lhsT=ONE64[0:64, 0:64], rhs=r[0][0:64, N],
                      start=True, stop=True)
                fw.op("act", "activation", out=r[1][0:64, N], in_=E.p(0, (slice(0, 64), 0, N)), func=AF.Ln,
                      scale=1.0 / 64, bias=RMS_EPS)
                fw.op("act", "activation", out=r[1][0:64, N], in_=r[1][0:64, N], func=AF.Exp, scale=-0.5)
                fw.op("dve", "scalar_tensor_tensor", out=AT.p(8 + h, (slice(0, 64), 8 + h, N)), in0=o[0:64, N],
                      scalar=GS[:, l:l + 1], in1=r[1][0:64, N], op0=ALU.mult, op1=ALU.mult)
            return epi2

        pending = None
        defer_at = min(6, nkt - 1)
        for h in range(8):
            for ki in range(nkt + 1):
                if ki < nkt:
                    qk_exp(h, ki)
                if ki >= 1:
                    pv(h, ki - 1)
                if ki == defer_at and pending is not None:
                    pending()
                    pending = None
            if pending is not None:
                pending()
            pending = epi1(h)
        pending()

        if tp:
            tapout("ATT", AT[:, 8:16, :], [128, 8, 512], BF16)
        for dp in range(4):
            wao = wq.get(live=1)
            wlo = wq.get(live=2)
            wga = wq.get(live=3)
            wgl = wq.get(live=4)
            for di in range(2):
                dt = 2 * dp + di
                cs = slice(128 * di, 128 * di + 128)
                b1 = next_bank()
                chain(b1, lambda k: V(wao.ap[0:64, k, cs], wao.regs, "sb"),
                      rhs_fn=lambda k: AT.p(8 + k, (slice(0, 64), 8 + k, N)))
                b2 = next_bank()
                chain(b2, lambda k: V(wlo.ap[:, k, cs], wlo.regs, "sb"), rhs_fn=lambda k: AT.p(k, (ALLP, k, N)))
                b3 = next_bank()
                chain(b3, lambda k: V(wga.ap[:, k, cs], wga.regs, "sb"))
                sa = Fn()
                fw.op("act", "activation", out=sa[:, N], in_=bv(b3, ALLP, N), func=AF.Sigmoid, bias=pcol(l, C_BG + dt))
                b4 = next_bank()
                chain(b4, lambda k: V(wgl.ap[:, k, cs], wgl.regs, "sb"))
                sl = Fn()
                fw.op("act", "activation", out=sl[:, N], in_=bv(b4, ALLP, N), func=AF.Sigmoid, bias=pcol(l, C_BG + 8 + dt))
                fw.op("dve", "tensor_tensor", out=sa[:, N], in0=sa[:, N], in1=bv(b1, ALLP, N), op=ALU.mult)
                fw.op("dve", "tensor_tensor", out=sl[:, N], in0=sl[:, N], in1=bv(b2, ALLP, N), op=ALU.mult)
                fw.op("dve", "tensor_tensor", out=MT.p(dt, (ALLP, dt, N)), in0=sa[:, N], in1=sl[:, N], op=ALU.add)
        for dp in range(4):
            wo = wq.get(live=1)
            for di in range(2):
                dt = 2 * dp + di
                cs = slice(128 * di, 128 * di + 128)
                b = next_bank()
                chain(b, lambda k: V(wo.ap[:, k, cs], wo.regs, "sb"), rhs_fn=lambda k: MT.p(k, (ALLP, k, N)))
                yk = Y.p(dt, (ALLP, dt, N))
                fw.op("dve", "scalar_tensor_tensor", out=yk, in0=yk, scalar=float(alpha), in1=bv(b, ALLP, N),
                      op0=ALU.mult, op1=ALU.add)
        if tp:
            tapout("MT", MT[:, :, :], [128, 8, 512], BF16)
            tapout("Y1", Y[:, :, :], [128, 8, 512], F32)
        ln_fm(n, lambda k: pcol(l, C_LNG + k), lambda k: pcol(l, C_LNB + k), LN_EPS)
        if tp:
            tapout("H1", Y[:, :, :], [128, 8, 512], F32)

        if is_moe:
            b = next_bank()
            for k in range(8):
                fw.op("pe", "matmul", out=bv(b, slice(0, NE), N), lhsT=WRT[:, k, :], rhs=Y.p(k, (ALLP, k, N)),
                      start=(k == 0), stop=(k == 7))
            LGT = Fn()
            fw.op("dve", "tensor_copy", out=LGT[0:NE, N], in_=bv(b, slice(0, NE), N))
            b = next_bank()
            for sub in range(nsub):
                ns = min(128, n - 128 * sub)
                fw.op("pe", "transpose", out=bv(b, slice(0, ns), slice(8 * sub, 8 * sub + 8)),
                      in_=LGT[0:NE, 128 * sub:128 * sub + ns], identity=IDF[0:NE, 0:NE])
            for sub in range(nsub):
                ns = min(128, n - 128 * sub)
                P_ = slice(0, ns)
                fw.op("dve", "tensor_copy", out=LG[P_, sub, :], in_=bv(b, P_, slice(8 * sub, 8 * sub + 8)))
                fw.op("dve", "max", out=M8[P_, sub, :], in_=LG[P_, sub, :])
                fw.op("dve", "tensor_scalar", out=CMB[P_, sub, :], in0=LG[P_, sub, :], scalar1=M8[P_, sub, 1:2],
                      scalar2=None, op0=ALU.is_ge)
                fw.op("dve", "tensor_scalar", out=SM[P_, 0:1], in0=M8[P_, sub, 0:1], scalar1=-1.0, scalar2=None, op0=ALU.mult)
                fw.op("act", "activation", out=LG[P_, sub, :], in_=LG[P_, sub, :], func=AF.Exp, bias=SM[P_, 0:1])
                fw.op("dve", "tensor_tensor", out=CMB[P_, sub, :], in0=CMB[P_, sub, :], in1=LG[P_, sub, :], op=ALU.mult)
                fw.op("dve", "reduce_sum", out=SM[P_, 1:2], in_=CMB[P_, sub, :], axis=AX.X)
                fw.op("dve", "reciprocal", out=SM[P_, 2:3], in_=SM[P_, 1:2])
                fw.op("dve", "tensor_scalar", out=CMB[P_, sub, :], in0=CMB[P_, sub, :], scalar1=SM[P_, 2:3],
                      scalar2=None, op0=ALU.mult)
            b = next_bank()
            for sub in range(nsub):
                ns = min(128, n - 128 * sub)
                fw.op("pe", "transpose", out=bv(b, slice(0, NE), slice(128 * sub, 128 * sub + ns)),
                      in_=CMB[0:ns, sub, :], identity=IDF[0:ns, 0:ns])
            CT = Fp[NF]
            fw.op("dve", "tensor_copy", out=CT[0:NE, N], in_=bv(b, slice(0, NE), N))

        if nxt is not None:
            s2, ti2 = nxt
            oth = XBS[1] if XB is XBS[0] else XBS[0]
            fw.dma("sp", oth[:, :, 0:tiles[ti2][1]], hview(HB, s2, ti2))
        nexp = NE if is_moe else 1
        if is_moe:
            for k in range(8):
                yk = Y.p(k, (ALLP, k, N))
                fw.op("act", "activation", out=yk, in_=yk, func=AF.Copy, scale=float(alpha))
        for e in range(nexp):
            if is_moe:
                bc = next_bank()
                cte = Fn()
                fw.op("dve", "tensor_scalar", out=cte[0:NE, N], in0=CT[0:NE, N], scalar1=IDF[0:NE, e:e + 1],
                      scalar2=None, op0=ALU.mult)
                fw.op("pe", "matmul", out=bv(bc, ALLP, N), lhsT=ONE64[0:NE, :], rhs=cte[0:NE, N], start=True, stop=True)
                cb = CBT
                fw.op("act", "activation", out=cb[:, N], in_=bv(bc, ALLP, N), func=AF.Copy)
            for fp_ in range(NFT // 2):
                wg_ = wq.get(live=1)
                wu_ = wq.get(live=2)
                for fi in range(2):
                    f = 2 * fp_ + fi
                    cs = slice(128 * fi, 128 * fi + 128)
                    b1 = next_bank()
                    chain(b1, lambda k: V(wg_.ap[:, k, cs], wg_.regs, "sb"))
                    b2 = next_bank()
                    chain(b2, lambda k: V(wu_.ap[:, k, cs], wu_.regs, "sb"))
                    sg = Fn()
                    fw.op("act", "activation", out=sg[:, N], in_=bv(b1, ALLP, N), func=AF.Silu)
                    if is_moe:
                        fw.op("dve", "tensor_tensor", out=sg[:, N], in0=sg[:, N], in1=CBT[:, N], op=ALU.mult)
                    fw.op("dve", "tensor_tensor", out=AT.p(f, (ALLP, f, N)), in0=sg[:, N], in1=bv(b2, ALLP, N), op=ALU.mult)
            for fp_ in range(NFT // 2):
                wd_ = wq.get(live=1)
                for fi in range(2):
                    f = 2 * fp_ + fi
                    for dt in range(8):
                        fw.op("pe", "matmul", out=bv(dt, ALLP, N), lhsT=V(wd_.ap[:, fi, 128 * dt:128 * dt + 128], wd_.regs, "sb"),
                              rhs=AT.p(f, (ALLP, f, N)), start=(f == 0), stop=(f == NFT - 1))
            bank_rr[0] = 0
            for dt in range(8):
                yk = Y.p(dt, (ALLP, dt, N))
                if is_moe:
                    fw.op("dve", "tensor_tensor", out=yk, in0=yk, in1=bv(dt, ALLP, N), op=ALU.add)
                else:
                    fw.op("dve", "scalar_tensor_tensor", out=yk, in0=yk, scalar=float(alpha), in1=bv(dt, ALLP, N),
                          op0=ALU.mult, op1=ALU.add)
        bank_rr[0] = 0
        if tp:
            tapout("Y2", Y[:, :, :], [128, 8, 512], F32)
        ln_fm(n, lambda k: pcol(l, C_LNG + 8 + k), lambda k: pcol(l, C_LNB + 8 + k), LN_EPS)
        dbg_store(l + 1, s, ti, n)
        if not last_layer:
            store_h(s, ti, n)
        elif ti > 0:
            for sub in range(nsub):
                ns = min(128, n - 128 * sub)
                xl2 = [Fn(), Fn()]
                for half in range(2):
                    b = next_bank()
                    for q in range(4):
                        k = 4 * half + q
                        fw.op("pe", "transpose", out=bv(b, slice(0, ns), slice(128 * q, 128 * q + 128)),
                              in_=Y.p(k, (ALLP, k, slice(128 * sub, 128 * sub + ns))), identity=IDF[:])
                    fw.op("act", "activation", out=xl2[half][0:ns, :], in_=bv(b, slice(0, ns), slice(0, 512)),
                          func=AF.Copy)
                r0 = t0 - NM + 128 * sub
                for half in range(2):
                    fw.dma("sp", OUT.v(OUT.h[s, r0:r0 + ns, 512 * half:512 * half + 512], s * NTL + ti), xl2[half][0:ns, :])

    nl = L if stop_after is None else stop_after
    for l in range(nl):
        wq.add(chunks_for(l) * (n_seq * NTL))
    steps_all = [(l_, s_, t_) for l_ in range(nl) for s_ in range(n_seq) for t_ in range(NTL)]
    nsteps = n_seq * NTL
    for l in range(nl):
        layer_setup(l)
        per_step = -(-len(cast_jobs.get(l + 1, [])) // max(nsteps - 1, 1))
        for s in range(n_seq):
            fw.op("dve", "memset", ap=HIST[:], constant=0.0)
            fw.op("dve", "memset", ap=HST[:], constant=0.0)
            for ti in range(NTL):
                if l + 1 < nl:
                    run_cast_jobs(l + 1, per_step)
                si = steps_all.index((l, s, ti))
                nxt = steps_all[si + 1][1:] if si + 1 < len(steps_all) else None
                tile_step(l, s, ti, last_layer=(l == L - 1), nxt=nxt)
                if nxt is not None:
                    xb_cur[0] = XBS[1] if xb_cur[0] is XBS[0] else XBS[0]
                    xb_loaded[0] = True
        assert not cast_jobs.get(l + 1) or l + 1 >= nl
    outs = [OUT[:]] + ([DBG[:]] if DBG is not None else []) + [t[:] for t in TAPS.values()]
    fw.finish(outs)
    return nc, fw


_PROG = {}


def kernel(**inputs):
    n_cores = 8
    x = np.ascontiguousarray(inputs["x"], dtype=np.float32)
    B, S, _ = x.shape
    n_seq = B // n_cores
    depth = inputs["w_in"].shape[0]
    alpha = (2 * depth) ** 0.25
    key = (n_seq, S, depth)
    if key not in _PROG:
        _PROG[key] = build_program(n_seq, S, depth, alpha)[0]
    nc = _PROG[key]
    shared = {k: np.ascontiguousarray(inputs[k], dtype=np.float32) for k in WNAMES if k != "x"}
    in_maps = []
    for c in range(n_cores):
        m = dict(shared)
        m["x"] = x[c * n_seq:(c + 1) * n_seq]
        in_maps.append(m)
    res = run_bass_kernel_spmd(nc, in_maps, core_ids=list(range(n_cores)))
    return np.concatenate([r["out"] for r in res.results], axis=0).astype(np.float32)
```

```python
import math
import numpy as np
import concourse.bass as bass
import concourse.mybir as mybir
from concourse.bass_utils import run_bass_kernel_spmd

F32 = mybir.dt.float32
BF16 = mybir.dt.bfloat16
AF = mybir.ActivationFunctionType
ALU = mybir.AluOpType
AX = mybir.AxisListType

D = 1024
NM = 16
DA = 512
FF = 2816
NE = 8
PW = 5632
LN_EPS = 1e-5
RMS_EPS = 1e-5
NFT = FF // 128
EAGER_CAST = True


class Reg:
    __slots__ = ("name", "w", "r", "dsem", "dcnt")

    def __init__(self, name):
        self.name = name
        self.w = None
        self.r = {}
        self.dsem = None
        self.dcnt = 0


class V:
    __slots__ = ("ap", "regs", "kind")

    def __init__(self, ap, regs, kind):
        self.ap = ap
        self.regs = regs
        self.kind = kind


class Buf:
    def __init__(self, name, handle, nreg, kind):
        self.kind = kind
        self.name = name
        self.h = handle
        self.regs = [Reg(f"{name}_{i}") for i in range(nreg)]

    def __getitem__(self, idx):
        return V(self.h[idx], self.regs, self.kind)

    def v(self, ap, ridx=None):
        if ridx is None:
            return V(ap, self.regs, self.kind)
        if isinstance(ridx, int):
            ridx = (ridx,)
        return V(ap, [self.regs[i] for i in ridx], self.kind)

    def p(self, ridx, idx):
        if isinstance(ridx, int):
            ridx = (ridx,)
        return V(self.h[idx], [self.regs[i] for i in ridx], self.kind)


WRITE_KW = ("out", "accum_out", "ap")


class FW:
    ENG = ("pe", "act", "dve", "pool", "sp")

    def __init__(self, nc, targets=None):
        self.nc = nc
        self.targets = targets
        self.rec = {e: set() for e in self.ENG}
        self.semval = {e: 0 for e in self.ENG}
        self.vmap = {e: {} for e in self.ENG}
        self.eng = {"pe": nc.tensor, "act": nc.scalar, "dve": nc.vector,
                    "pool": nc.gpsimd, "sp": nc.sync}
        self.sem = {}
        self.cnt = {}
        self.known = {}
        for e in self.ENG:
            self.sem[e] = nc.alloc_semaphore(name=f"s_{e}")
            self.cnt[e] = 0
            self.known[e] = {}
        self.nsem = len(self.ENG)
        self.nwaits = 0
        self.ninst = 0

    def sb(self, name, shape, dtype, nreg=1):
        return Buf(name, self.nc.alloc_sbuf_tensor(name, list(shape), dtype), nreg, "sb")

    def ps(self, name, shape, dtype=F32, nreg=1):
        return Buf(name, self.nc.alloc_psum_tensor(name, list(shape), dtype), nreg, "ps")

    def dram(self, name, shape, dtype, kind="Internal", nreg=1):
        return Buf(name, self.nc.dram_tensor(name, list(shape), dtype, kind=kind), nreg, "dram")

    def newsem(self, name):
        self.sem[name] = self.nc.alloc_semaphore(name=name)
        self.nsem += 1
        return name

    def _wait(self, e, key, val):
        kn = self.known[e]
        if kn.get(key, 0) >= val:
            return
        if key == e and e == "pe":
            return
        kn[key] = val
        v = val
        if key in self.rec:
            self.rec[key].add(val)
            if self.targets is not None:
                v = self.vmap[key][val]
        self.eng[e].wait_ge(self.sem[key], v)
        self.nwaits += 1

    def _deps(self, e, reads, writes):
        for v in reads:
            for r in v.regs:
                if r.w is not None:
                    self._wait(e, r.w[0], r.w[1])
        for v in writes:
            for r in v.regs:
                if r.w is not None:
                    self._wait(e, r.w[0], r.w[1])
                for k, val in r.r.items():
                    self._wait(e, k, val)

    def _mark(self, ev, reads, writes):
        for v in reads:
            for r in v.regs:
                r.r[ev[0]] = ev[1]
        for v in writes:
            for r in v.regs:
                r.w = ev
                r.r = {}

    def op(self, e, fn, *, reads=(), writes=(), **kw):
        rd = list(reads)
        wr = list(writes)
        args = {}
        for k, v in kw.items():
            if isinstance(v, V):
                (wr if k in WRITE_KW else rd).append(v)
                args[k] = v.ap
            else:
                args[k] = v
        self._deps(e, rd, wr)
        ins = getattr(self.eng[e], fn)(**args)
        self.cnt[e] += 1
        if self.targets is None or self.cnt[e] in self.targets[e]:
            self.semval[e] += 1
            self.vmap[e][self.cnt[e]] = self.semval[e]
            ins.then_inc(self.sem[e], 1)
        self._mark((e, self.cnt[e]), rd, wr)
        self.ninst += 1
        return ins

    def dma(self, q, out, in_, sem_of=None, serialize=True, **kw):
        owner = sem_of
        if owner is None:
            for v in (out, in_):
                if v.kind == "sb":
                    owner = v.regs[0]
                    break
        if owner is None:
            owner = out.regs[0]
        if owner.dsem is None:
            owner.dsem = self.newsem(f"d_{owner.name}")
        self._deps(q, [in_], [out])
        if serialize:
            self._wait(q, owner.dsem, owner.dcnt)
        ins = self.eng[q].dma_start(out=out.ap, in_=in_.ap, **kw)
        owner.dcnt += 16
        ins.then_inc(self.sem[owner.dsem], 16)
        self._mark((owner.dsem, owner.dcnt), [in_], [out])
        self.ninst += 1
        return ins

    def finish(self, outs, q="sp"):
        for v in outs:
            for r in v.regs:
                if r.w is not None:
                    self._wait(q, r.w[0], r.w[1])


WNAMES = ["x", "meta_tokens", "ln_in_g", "ln_in_b", "w_in", "b_gate", "da_lambda",
          "da_subln_g", "conv_w", "conv_b", "lru_wr", "lru_br", "lru_wi", "lru_bi",
          "lru_lambda", "w_attn_out", "w_lru_out", "w_o", "ln_g", "ln_b", "ffn_wg",
          "ffn_wu", "ffn_wd", "router_w", "moe_wg", "moe_wu", "moe_wd"]


def build_program(n_seq, seq, depth, alpha, dbg_layers=False, stop_after=None, tap=None):
    _, fw1 = _build_program(None, n_seq, seq, depth, alpha, dbg_layers, stop_after, tap)
    return _build_program(fw1.rec, n_seq, seq, depth, alpha, dbg_layers, stop_after, tap)


def _build_program(targets, n_seq, seq, depth, alpha, dbg_layers=False, stop_after=None, tap=None):
    nc = bass.Bass("TRN2", target_bir_lowering=False)
    fw = FW(nc, targets)
    L = depth
    Nd = (L + 1) // 2
    Nm = L // 2
    T = NM + seq
    NX = seq // 512
    assert seq % 512 == 0
    tiles = [(0, NM)] + [(NM + 512 * i, 512) for i in range(NX)]
    NTL = len(tiles)
    NKT = 1 + seq // 128

    shapes = {
        "x": [n_seq, seq, D], "meta_tokens": [NM, D], "ln_in_g": [D], "ln_in_b": [D],
        "w_in": [L, D, PW], "b_gate": [L, 2, D], "da_lambda": [L, 4, 32],
        "da_subln_g": [L, 64], "conv_w": [L, 4, D], "conv_b": [L, D],
        "lru_wr": [L, 16, 64, 64], "lru_br": [L, D], "lru_wi": [L, 16, 64, 64],
        "lru_bi": [L, D], "lru_lambda": [L, D], "w_attn_out": [L, DA, D],
        "w_lru_out": [L, D, D], "w_o": [L, D, D], "ln_g": [L, 2, D], "ln_b": [L, 2, D],
        "ffn_wg": [Nd, D, FF], "ffn_wu": [Nd, D, FF], "ffn_wd": [Nd, FF, D],
        "router_w": [max(Nm, 1), D, NE], "moe_wg": [max(Nm, 1), NE, D, FF],
        "moe_wu": [max(Nm, 1), NE, D, FF], "moe_wd": [max(Nm, 1), NE, FF, D],
    }
    I = {k: fw.dram(k, shapes[k], F32, kind="ExternalInput") for k in WNAMES}
    OUT = fw.dram("out", [n_seq, seq, D], F32, kind="ExternalOutput", nreg=n_seq * NTL)
    H32 = fw.dram("h32", [n_seq, D, T], F32, nreg=n_seq * NTL)
    HB = fw.dram("hb", [n_seq, D, T], BF16, nreg=n_seq * NTL)
    DBG = None
    if dbg_layers:
        DBG = fw.dram("dbg", [L + 1, n_seq, D, T], F32, kind="ExternalOutput")
    TAPS = {}

    def tapout(name, view, shape, dtype):
        if tap is None:
            return
        if name not in TAPS:
            TAPS[name] = fw.dram("tap_" + name, shape, dtype, kind="ExternalOutput")
        fw.dma("sp", TAPS[name][:], view)
    WB = {
        "w_in": fw.dram("wb_in", [L, D, PW], BF16, nreg=L),
        "w_attn_out": fw.dram("wb_ao", [L, DA, D], BF16, nreg=L),
        "w_lru_out": fw.dram("wb_lo", [L, D, D], BF16, nreg=L),
        "w_o": fw.dram("wb_o", [L, D, D], BF16, nreg=L),
        "ffn_wg": fw.dram("wb_fg", [Nd, D, FF], BF16, nreg=Nd),
        "ffn_wu": fw.dram("wb_fu", [Nd, D, FF], BF16, nreg=Nd),
        "ffn_wd": fw.dram("wb_fd", [Nd, FF, D], BF16, nreg=Nd),
        "moe_wg": fw.dram("wb_mg", [max(Nm, 1), NE, D, FF], BF16, nreg=max(Nm, 1) * NE),
        "moe_wu": fw.dram("wb_mu", [max(Nm, 1), NE, D, FF], BF16, nreg=max(Nm, 1) * NE),
        "moe_wd": fw.dram("wb_md", [max(Nm, 1), NE, FF, D], BF16, nreg=max(Nm, 1) * NE),
    }

    KT = fw.sb("KT", [128, 4, T], BF16)
    VR = fw.sb("VR", [128, NKT, 512], BF16)
    ONESB = fw.sb("ONESB", [128, 64], BF16)
    Y = fw.sb("Y", [128, 8, 512], F32, nreg=8)
    XBS = [fw.sb("XB0", [128, 8, 512], BF16, nreg=8), fw.sb("XB1", [128, 8, 512], BF16, nreg=8)]
    xb_cur = [XBS[0]]
    AT = fw.sb("AT", [128, NFT, 512], BF16, nreg=NFT)
    MT = fw.sb("MT", [128, 8, 512], BF16, nreg=8)
    NF = 8
    Fp = [fw.sb(f"F{i}", [128, 512], F32) for i in range(NF + 1)]
    LNR = fw.sb("LNR", [128, 512], F32)
    CBT = fw.sb("CBT", [128, 512], F32)
    XR = [fw.sb(f"XR{i}", [128, 516], BF16) for i in range(2)]
    PTb = [fw.sb(f"PT{i}", [128, 2, 512], BF16) for i in range(3)]
    NSLOT = 6
    RING = [fw.sb(f"RG{i}", [128, 2048], BF16) for i in range(NSLOT)]
    WRB = fw.sb("WRB", [128, 8, 128], F32)
    WIB = fw.sb("WIB", [128, 8, 128], F32)
    CD = fw.sb("CD", [128, 8, 4, 128], BF16)
    WRT = fw.sb("WRT", [128, 8, NE], F32)
    PTB = fw.sb("PTB", [128, L, 112], F32)
    LNIN = fw.sb("LNIN", [128, 16], F32)
    CNEG = fw.sb("CNEG", [128, L, 8], F32)
    HPB = fw.sb("HPB", [128, L, 24], F32)
    STG = fw.sb("STG", [128, 128], F32)
    IDF = fw.sb("IDF", [128, 128], F32)
    IDB = fw.sb("IDB", [128, 128], BF16)
    ONESD = fw.sb("ONESD", [128, 128], F32)
    ONE64 = fw.sb("ONE64", [128, 128], F32)
    HIST = fw.sb("HIST", [128, 8, 4], BF16)
    HST = fw.sb("HST", [128, 8], F32)
    HST0 = fw.sb("HST0", [128, 8], F32)
    HIST0 = fw.sb("HIST0", [128, 8, 4], BF16)
    LAMW = fw.sb("LAMW", [128, 4, 32], F32)
    LAMS = fw.sb("LAMS", [128, 8], F32)
    NLAM = fw.sb("NLAM", [128, L], F32)
    GS = fw.sb("GS", [64, L], F32)
    LG = fw.sb("LG", [128, 4, NE], F32)
    M8 = fw.sb("M8", [128, 4, 8], F32)
    CMB = fw.sb("CMB", [128, 4, NE], F32)
    SM = fw.sb("SM", [128, 16], F32)

    PS = [fw.ps(f"PS{i}", [128, 2, 512], F32, nreg=2) for i in range(4)]
    bank_rr = [0]

    def bank(b):
        return PS[b // 2], b % 2

    def next_bank():
        b = bank_rr[0]
        bank_rr[0] = (b + 1) % 8
        return b

    def bv(b, pr, cols):
        buf, hf = bank(b)
        return buf.p(hf, (pr, hf, cols))

    f_rr = [0]

    def Fn():
        i = f_rr[0]
        f_rr[0] = (i + 1) % NF
        return Fp[i]

    ALLP = slice(0, 128)

    fw.op("pool", "memset", ap=IDF[:], constant=0.0)
    fw.op("pool", "affine_select", out=IDF[:], in_=IDF[:], pattern=[[-1, 128]],
          compare_op=ALU.not_equal, fill=1.0, base=0, channel_multiplier=1)
    fw.op("pool", "tensor_copy", out=IDB[:], in_=IDF[:])
    fw.op("pool", "memset", ap=ONESD[:], constant=1.0 / D)
    fw.op("pool", "memset", ap=ONE64[:], constant=1.0)
    fw.op("pool", "memset", ap=WRB[:], constant=0.0)
    fw.op("pool", "memset", ap=WIB[:], constant=0.0)
    fw.op("pool", "memset", ap=ONESB[:], constant=1.0)

    def stage_rows(dst_v, loads):
        r0 = 0
        for src_ap, nrows in loads:
            fw.dma("sp", STG[r0:r0 + nrows, :], src_ap)
            r0 += nrows
        b = next_bank()
        fw.op("pe", "transpose", out=bv(b, ALLP, slice(0, r0)), in_=STG[0:r0, :], identity=IDF[0:r0, 0:r0])
        fw.op("dve", "tensor_copy", out=dst_v, in_=bv(b, ALLP, slice(0, r0)))

    def rows(buf, ap, n):
        return (buf.v(ap.rearrange("(r p) -> r p", p=128)), n)

    for l in range(L):
        loads = [
            (I["b_gate"].v(I["b_gate"].h[l].rearrange("i (r p) -> (i r) p", p=128)), 16),
            (I["conv_w"].v(I["conv_w"].h[l].rearrange("i (r p) -> (i r) p", p=128)), 32),
            rows(I["conv_b"], I["conv_b"].h[l], 8),
            rows(I["lru_br"], I["lru_br"].h[l], 8),
            rows(I["lru_bi"], I["lru_bi"].h[l], 8),
            rows(I["lru_lambda"], I["lru_lambda"].h[l], 8),
            (I["ln_g"].v(I["ln_g"].h[l].rearrange("i (r p) -> (i r) p", p=128)), 16),
            (I["ln_b"].v(I["ln_b"].h[l].rearrange("i (r p) -> (i r) p", p=128)), 16),
        ]
        stage_rows(PTB[:, l, :], loads)
    stage_rows(LNIN[:, :], [rows(I["ln_in_g"], I["ln_in_g"].h[:], 8), rows(I["ln_in_b"], I["ln_in_b"].h[:], 8)])
    C_BG, C_CW, C_CB, C_BR, C_BI, C_LAM, C_LNG, C_LNB = 0, 16, 48, 56, 64, 72, 80, 96

    def pcol(l, c):
        return PTB[:, l, c:c + 1]

    for l in range(L):
        t = Fn()
        fw.op("act", "activation", out=t[:, 0:8], in_=PTB[:, l, C_LAM:C_LAM + 8], func=AF.Exp, scale=-1.0)
        fw.op("act", "activation", out=t[:, 8:16], in_=t[:, 0:8], func=AF.Ln, bias=1.0)
        fw.op("dve", "tensor_scalar", out=CNEG[:, l, :], in0=t[:, 8:16], scalar1=-8.0, scalar2=None, op0=ALU.mult)
        fw.op("dve", "tensor_scalar", out=HPB[:, l, 16:24], in0=t[:, 8:16], scalar1=-4.0, scalar2=None, op0=ALU.mult)
        fw.op("dve", "tensor_scalar", out=HPB[:, l, 0:16], in0=PTB[:, l, C_BR:C_BR + 16], scalar1=0.5, scalar2=None, op0=ALU.mult)
    for l in range(L):
        lam_init = 0.8 - 0.6 * math.exp(-0.3 * l)
        src = bass.AP(I["da_lambda"].h, l * 128, [[0, 128], [1, 128]])
        fw.dma("sp", LAMW.v(LAMW.h[:].rearrange("p a b -> p (a b)")), I["da_lambda"].v(src))
        fw.op("dve", "tensor_tensor", out=LAMW[:, 0, :], in0=LAMW[:, 0, :], in1=LAMW[:, 1, :], op=ALU.mult)
        fw.op("dve", "tensor_tensor", out=LAMW[:, 2, :], in0=LAMW[:, 2, :], in1=LAMW[:, 3, :], op=ALU.mult)
        fw.op("dve", "reduce_sum", out=LAMS[:, 0:1], in_=LAMW[:, 0, :], axis=AX.X)
        fw.op("dve", "reduce_sum", out=LAMS[:, 1:2], in_=LAMW[:, 2, :], axis=AX.X)
        fw.op("act", "activation", out=LAMS[:, 2:4], in_=LAMS[:, 0:2], func=AF.Exp)
        fw.op("dve", "tensor_tensor", out=LAMS[:, 4:5], in0=LAMS[:, 3:4], in1=LAMS[:, 2:3], op=ALU.subtract)
        fw.op("dve", "tensor_scalar", out=NLAM[:, l:l + 1], in0=LAMS[:, 4:5], scalar1=-lam_init, scalar2=None, op0=ALU.add)
        fw.dma("sp", GS[:, l:l + 1], I["da_subln_g"].v(I["da_subln_g"].h[l].rearrange("(p o) -> p o", o=1)))
        fw.op("dve", "tensor_scalar", out=GS[:, l:l + 1], in0=GS[:, l:l + 1], scalar1=1.0 - lam_init, scalar2=None, op0=ALU.mult)

    cast_ev = []
    CAST_INFLIGHT = 2
    cast_jobs = {l_: [] for l_ in range(L)}

    def cast2d(jobs, dst_buf, dst_ap, src_buf, src_ap, reg, rows_, cols):
        c = cols
        for cand in (2048, 1408, 1024, 512):
            if cols % cand == 0:
                c = cand
                break
        rstep = 256 if rows_ % 256 == 0 and cols > 2048 else rows_
        for r0 in range(0, rows_, rstep):
            d = dst_ap[r0:r0 + rstep, :].rearrange("r (a c) -> r a c", c=c)
            s_ = src_ap[r0:r0 + rstep, :].rearrange("r (a c) -> r a c", c=c)

            def job(d=d, s_=s_):
                if len(cast_ev) >= CAST_INFLIGHT:
                    fw._wait("pool", *cast_ev[-CAST_INFLIGHT])
                fw.dma("pool", dst_buf.v(d, reg), src_buf.v(s_), sem_of=dst_buf.regs[reg], serialize=False)
                cast_ev.append(dst_buf.regs[reg].w)
            jobs.append(job)

    for l in range(L):
        J = cast_jobs[l]
        cast2d(J, WB["w_in"], WB["w_in"].h[l], I["w_in"], I["w_in"].h[l], l, D, PW)
        cast2d(J, WB["w_attn_out"], WB["w_attn_out"].h[l], I["w_attn_out"], I["w_attn_out"].h[l], l, DA, D)
        cast2d(J, WB["w_lru_out"], WB["w_lru_out"].h[l], I["w_lru_out"], I["w_lru_out"].h[l], l, D, D)
        cast2d(J, WB["w_o"], WB["w_o"].h[l], I["w_o"], I["w_o"].h[l], l, D, D)
        i = l // 2
        if l % 2 == 0:
            cast2d(J, WB["ffn_wg"], WB["ffn_wg"].h[i], I["ffn_wg"], I["ffn_wg"].h[i], i, D, FF)
            cast2d(J, WB["ffn_wu"], WB["ffn_wu"].h[i], I["ffn_wu"], I["ffn_wu"].h[i], i, D, FF)
            cast2d(J, WB["ffn_wd"], WB["ffn_wd"].h[i], I["ffn_wd"], I["ffn_wd"].h[i], i, FF, D)
        else:
            for e in range(NE):
                cast2d(J, WB["moe_wg"], WB["moe_wg"].h[i, e], I["moe_wg"], I["moe_wg"].h[i, e], i * NE + e, D, FF)
                cast2d(J, WB["moe_wu"], WB["moe_wu"].h[i, e], I["moe_wu"], I["moe_wu"].h[i, e], i * NE + e, D, FF)
                cast2d(J, WB["moe_wd"], WB["moe_wd"].h[i, e], I["moe_wd"], I["moe_wd"].h[i, e], i * NE + e, FF, D)

    def run_cast_jobs(l_, k=None):
        J = cast_jobs.get(l_)
        if not J:
            return
        k = len(J) if k is None else min(k, len(J))
        for _ in range(k):
            J.pop(0)()

    for l_ in range(L if EAGER_CAST else 1):
        run_cast_jobs(l_)

    class WStream:
        def __init__(self):
            self.plan = []
            self.issued = 0
            self.taken = 0

        def add(self, specs):
            self.plan.extend(specs)

        def _issue(self, i):
            buf, reg, ap, shape = self.plan[i]
            slot = RING[i % NSLOT]
            n = 1
            for s_ in shape[1:]:
                n *= s_
            dst = slot.h[0:shape[0], 0:n]
            if len(shape) == 3:
                dst = dst.rearrange("p (a b) -> p a b", b=shape[2])
            fw.dma("sp", slot.v(dst), buf.v(ap, reg))

        def get(self, live=4):
            i = self.taken
            assert i < len(self.plan), "weight plan exhausted"
            while self.issued < min(len(self.plan), i + NSLOT - live + 1):
                self._issue(self.issued)
                self.issued += 1
            self.taken += 1
            buf, reg, ap, shape = self.plan[i]
            slot = RING[i % NSLOT]
            n = 1
            for s_ in shape[1:]:
                n *= s_
            v = slot.h[0:shape[0], 0:n]
            if len(shape) == 3:
                v = v.rearrange("p (a b) -> p a b", b=shape[2])
            return slot.v(v)

    wq = WStream()

    def kcols(buf, reg, mat_ap, c0, ncol, krows=128):
        ap = mat_ap[:, c0:c0 + ncol].rearrange("(k p) c -> p k c", p=krows)
        return (buf, reg, ap, [krows, ap.shape[1], ncol])

    def chunks_for(l):
        sp = []
        win = WB["w_in"].h[l]
        for ci in range(4):
            sp.append(kcols(WB["w_in"], l, win, 256 * ci, 256))
        for ci in range(2):
            sp.append(kcols(WB["w_in"], l, win, 1024 + 256 * ci, 256))
        for cp in range(4):
            sp.append(kcols(WB["w_in"], l, win, 1536 + 256 * cp, 256))
            sp.append(kcols(WB["w_in"], l, win, 2560 + 256 * cp, 256))
        for dp in range(4):
            sp.append(kcols(WB["w_attn_out"], l, WB["w_attn_out"].h[l], 256 * dp, 256, krows=64))
            sp.append(kcols(WB["w_lru_out"], l, WB["w_lru_out"].h[l], 256 * dp, 256))
            sp.append(kcols(WB["w_in"], l, win, 3584 + 256 * dp, 256))
            sp.append(kcols(WB["w_in"], l, win, 4608 + 256 * dp, 256))
        for dp in range(4):
            sp.append(kcols(WB["w_o"], l, WB["w_o"].h[l], 256 * dp, 256))
        i = l // 2
        if l % 2 == 0:
            exps = [(WB["ffn_wg"], WB["ffn_wu"], WB["ffn_wd"], i, WB["ffn_wg"].h[i], WB["ffn_wu"].h[i], WB["ffn_wd"].h[i])]
        else:
            exps = [(WB["moe_wg"], WB["moe_wu"], WB["moe_wd"], i * NE + e, WB["moe_wg"].h[i, e],
                     WB["moe_wu"].h[i, e], WB["moe_wd"].h[i, e]) for e in range(NE)]
        for (bg_, bu_, bd_, reg, g_ap, u_ap, d_ap) in exps:
            for fp_ in range(NFT // 2):
                sp.append(kcols(bg_, reg, g_ap, 256 * fp_, 256))
                sp.append(kcols(bu_, reg, u_ap, 256 * fp_, 256))
            for fp_ in range(NFT // 2):
                ap = d_ap[256 * fp_:256 * fp_ + 256, :].rearrange("(f p) c -> p f c", p=128)
                sp.append((bd_, reg, ap, [128, 2, D]))
        return sp

    def hview(buf, s, ti):
        t0, n = tiles[ti]
        ap = buf.h[s].rearrange("(k p) t -> p k t", p=128)[:, :, t0:t0 + n]
        return buf.v(ap, s * NTL + ti)

    def ln_fm(n, gcol, bcol, eps):
        bA = next_bank()
        bB = next_bank()
        for k in range(8):
            sq = Fn()
            fw.op("act", "activation", out=sq[:, 0:n], in_=Y.p(k, (ALLP, k, slice(0, n))), func=AF.Square)
            fw.op("pe", "matmul", out=bv(bA, ALLP, slice(0, n)), lhsT=ONESD[:], rhs=Y.p(k, (ALLP, k, slice(0, n))),
                  start=(k == 0), stop=(k == 7))
            fw.op("pe", "matmul", out=bv(bB, ALLP, slice(0, n)), lhsT=ONESD[:], rhs=sq[:, 0:n],
                  start=(k == 0), stop=(k == 7))
        msq = Fn()
        fw.op("act", "activation", out=msq[:, 0:n], in_=bv(bA, ALLP, slice(0, n)), func=AF.Square)
        var = Fn()
        fw.op("dve", "tensor_tensor", out=var[:, 0:n], in0=bv(bB, ALLP, slice(0, n)), in1=msq[:, 0:n], op=ALU.subtract)
        fw.op("dve", "tensor_scalar", out=var[:, 0:n], in0=var[:, 0:n], scalar1=0.0, scalar2=eps, op0=ALU.max, op1=ALU.add)
        fw.op("act", "activation", out=var[:, 0:n], in_=var[:, 0:n], func=AF.Sqrt)
        rstd = LNR
        fw.op("dve", "reciprocal", out=rstd[:, 0:n], in_=var[:, 0:n])
        for k in range(8):
            t = Fn()
            yk = Y.p(k, (ALLP, k, slice(0, n)))
            fw.op("dve", "tensor_tensor", out=t[:, 0:n], in0=yk, in1=bv(bA, ALLP, slice(0, n)), op=ALU.subtract)
            fw.op("dve", "tensor_tensor", out=t[:, 0:n], in0=t[:, 0:n], in1=rstd[:, 0:n], op=ALU.mult)
            fw.op("act", "activation", out=yk, in_=t[:, 0:n], func=AF.Identity, scale=gcol(k), bias=bcol(k))
            fw.op("dve", "tensor_copy", out=xb_cur[0].p(k, (ALLP, k, slice(0, n))), in_=yk)

    def store_h(s, ti, n):
        fw.dma("act", hview(H32, s, ti), Y[:, :, 0:n])
        fw.dma("act", hview(HB, s, ti), xb_cur[0][:, :, 0:n])

    def dbg_store(li, s, ti, n):
        if DBG is not None:
            t0, _ = tiles[ti]
            ap = DBG.h[li, s].rearrange("(k p) t -> p k t", p=128)[:, :, t0:t0 + n]
            fw.dma("sp", DBG.v(ap), Y[:, :, 0:n])

    for s in range(n_seq):
        for ti, (t0, n) in enumerate(tiles):
            if s > 0 and ti == 0:
                continue
            nsub = (n + 127) // 128
            for sub in range(nsub):
                ns = min(128, n - 128 * sub)
                xl2 = [Fn(), Fn()]
                for half in range(2):
                    hs = slice(512 * half, 512 * half + 512)
                    if ti == 0:
                        fw.dma("sp", xl2[half][0:ns, :], I["meta_tokens"][0:ns, hs])
                    else:
                        r0 = t0 - NM + 128 * sub
                        fw.dma("sp", xl2[half][0:ns, :], I["x"].v(I["x"].h[s, r0:r0 + ns, hs]))
                for half in range(2):
                    b = next_bank()
                    for q in range(4):
                        k = 4 * half + q
                        fw.op("pe", "transpose", out=bv(b, ALLP, slice(128 * q, 128 * q + ns)),
                              in_=xl2[half][0:ns, 128 * q:128 * q + 128], identity=IDF[0:ns, 0:ns])
                    buf, hf = bank(b)
                    src = buf.p(hf, (ALLP, hf, slice(0, 512))).ap.rearrange("p (q t) -> p q t", t=128)[:, :, 0:ns]
                    dst = Y.h[:, 4 * half:4 * half + 4, 128 * sub:128 * sub + ns]
                    fw.op("dve", "tensor_copy", out=Y.v(dst, range(4 * half, 4 * half + 4)), in_=buf.v(src, hf))
            ln_fm(n, lambda k: LNIN[:, k:k + 1], lambda k: LNIN[:, 8 + k:9 + k], LN_EPS)
            store_h(s, ti, n)
            dbg_store(0, s, ti, n)

    QSCALE = 32 ** -0.5

    def layer_setup(l):
        for (dst, src) in ((WRB, I["lru_wr"]), (WIB, I["lru_wi"])):
            for hh in range(2):
                sap = src.h[l].rearrange("(c hh) i j -> hh i c j", hh=2)[hh]
                fw.dma("sp", dst[64 * hh:64 * hh + 64, :, 64 * hh:64 * hh + 64], src.v(sap))
        for c in range(8):
            for j in range(4):
                fw.op("dve", "tensor_scalar", out=CD[:, c, j, :], in0=IDB[:], scalar1=pcol(l, C_CW + 8 * j + c),
                      scalar2=None, op0=ALU.mult)
        if l % 2 == 1:
            ap = I["router_w"].h[l // 2].rearrange("(k p) e -> p k e", p=128)
            fw.dma("sp", WRT[:], I["router_w"].v(ap))

    xb_loaded = [False]

    def tile_step(l, s, ti, last_layer, nxt=None):
        t0, n = tiles[ti]
        nsub = (n + 127) // 128
        N = slice(0, n)
        is_moe = (l % 2 == 1)
        fw.dma("sp", Y[:, :, 0:n], hview(H32, s, ti))
        XB = xb_cur[0]
        if not xb_loaded[0]:
            fw.dma("sp", XB[:, :, 0:n], hview(HB, s, ti))
        xb_loaded[0] = False
        kt0 = 0 if ti == 0 else 1 + 4 * (ti - 1)

        def chain(b, lhs_fn, nk=8, rhs_fn=None, pr=ALLP):
            for k in range(nk):
                rhs = rhs_fn(k) if rhs_fn else XB.p(k, (ALLP, k, N))
                fw.op("pe", "matmul", out=bv(b, pr, N), lhsT=lhs_fn(k), rhs=rhs, start=(k == 0), stop=(k == nk - 1))

        for ci in range(4):
            w = wq.get(live=1)
            for gi in range(2):
                g = (ci % 2) * 2 + gi
                b = next_bank()
                chain(b, lambda k: V(w.ap[:, k, 128 * gi:128 * gi + 128], w.regs, "sb"))
                if ci < 2:
                    fw.op("act", "activation", out=AT.p(16 + g, (ALLP, 16 + g, N)), in_=bv(b, ALLP, N),
                          func=AF.Copy, scale=QSCALE)
                else:
                    fw.op("dve", "tensor_copy", out=KT[:, g, t0:t0 + n], in_=bv(b, ALLP, N))
        for ci in range(2):
            w = wq.get(live=1)
            for sub in range(nsub):
                ns = min(128, n - 128 * sub)
                b = next_bank()
                for k in range(8):
                    fw.op("pe", "matmul", out=bv(b, slice(0, ns), slice(0, 256)),
                          lhsT=XB.p(k, (ALLP, k, slice(128 * sub, 128 * sub + ns))),
                          rhs=V(w.ap[:, k, :], w.regs, "sb"), start=(k == 0), stop=(k == 7))
                buf, hf = bank(b)
                fw.op("dve", "tensor_copy", out=VR[0:ns, kt0 + sub, 256 * ci:256 * ci + 256], in_=bv(b, slice(0, ns), slice(0, 256)))

        lru_w = {}
        lru_t = {}

        def lru_front(c):
            cp, ci = divmod(c, 2)
            if ci == 0:
                lru_w[("x", cp)] = wq.get(live=3)
            wx = lru_w[("x", cp)]
            cs = slice(128 * ci, 128 * ci + 128)
            b = next_bank()
            chain(b, lambda k: V(wx.ap[:, k, cs], wx.regs, "sb"))
            xr = XR[c % 2]
            fw.op("dve", "tensor_copy", out=xr[:, 0:3], in_=HIST[:, c, 0:3])
            fw.op("dve", "tensor_copy", out=xr[:, 3:3 + n], in_=bv(b, ALLP, N))
            fw.op("dve", "tensor_copy", out=HIST[:, c, 0:3], in_=xr[:, n:n + 3])
            b2 = next_bank()
            for j in range(4):
                fw.op("pe", "matmul", out=bv(b2, ALLP, N), lhsT=CD[:, c, j, :], rhs=xr[:, j:j + n],
                      start=(j == 0), stop=(j == 3))
            xc = Fn()
            fw.op("act", "activation", out=xc[:, N], in_=bv(b2, ALLP, N), func=AF.Identity,
                  bias=pcol(l, C_CB + c))
            br_ = next_bank()
            fw.op("pe", "matmul", out=bv(br_, ALLP, N), lhsT=WRB[:, c, :], rhs=xc[:, N], start=True, stop=True)
            bi_ = next_bank()
            fw.op("pe", "matmul", out=bv(bi_, ALLP, N), lhsT=WIB[:, c, :], rhs=xc[:, N], start=True, stop=True)
            lru_t[c] = (xc, br_, bi_)

        def lru_front_b(c):
            xc, br_, bi_ = lru_t[c]
            r = Fn()
            fw.op("act", "activation", out=r[:, N], in_=bv(br_, ALLP, N), func=AF.Tanh, scale=0.5, bias=HPB[:, l, c:c + 1])
            ig = Fn()
            fw.op("act", "activation", out=ig[:, N], in_=bv(bi_, ALLP, N), func=AF.Tanh, scale=0.5, bias=HPB[:, l, 8 + c:9 + c])
            a = Fn()
            fw.op("act", "activation", out=a[:, N], in_=r[:, N], func=AF.Exp, scale=HPB[:, l, 16 + c:17 + c], bias=HPB[:, l, 16 + c:17 + c])
            lru_t[c] = (xc, r, ig, a)

        def lru_back(c):
            cp, ci = divmod(c, 2)
            if ci == 0:
                lru_w[("g", cp)] = wq.get(live=3)
            wg = lru_w[("g", cp)]
            cs = slice(128 * ci, 128 * ci + 128)
            xc, r, ig, a = lru_t.pop(c)
            fw.op("dve", "tensor_tensor", out=r[:, N], in0=a[:, N], in1=a[:, N], op=ALU.mult)
            fw.op("dve", "tensor_scalar", out=r[:, N], in0=r[:, N], scalar1=-0.25, scalar2=0.25, op0=ALU.mult, op1=ALU.add)
            fw.op("act", "activation", out=r[:, N], in_=r[:, N], func=AF.Sqrt)
            fw.op("dve", "scalar_tensor_tensor", out=ig[:, N], in0=ig[:, N], scalar=1.0, in1=xc[:, N], op0=ALU.add, op1=ALU.mult)
            fw.op("dve", "tensor_tensor", out=ig[:, N], in0=ig[:, N], in1=r[:, N], op=ALU.mult)
            hh = xc
            fw.op("dve", "tensor_tensor_scan", out=hh[:, N], data0=a[:, N], data1=ig[:, N],
                  initial=HST[:, c:c + 1], op0=ALU.mult, op1=ALU.add)
            fw.op("dve", "tensor_copy", out=HST[:, c:c + 1], in_=hh[:, n - 1:n])
            bg_ = next_bank()
            chain(bg_, lambda k: V(wg.ap[:, k, cs], wg.regs, "sb"))
            gg = r
            fw.op("act", "activation", out=gg[:, N], in_=bv(bg_, ALLP, N), func=AF.Gelu_apprx_tanh)
            fw.op("dve", "tensor_tensor", out=AT.p(c, (ALLP, c, N)), in0=hh[:, N], in1=gg[:, N], op=ALU.mult)

        for c in range(9):
            if c < 8:
                lru_front(c)
            if c >= 1:
                lru_back(c - 1)
            if c < 8:
                lru_front_b(c)

        tp = (tap is not None and (l, s, ti) == tuple(tap))
        if tp:
            tapout("QT", AT[:, 16:20, :], [128, 4, 512], BF16)
            tapout("KT", KT[:, :, :], [128, 4, T], BF16)
            tapout("VR", VR[:, :, 0:512], [128, NKT, 512], BF16)
            tapout("REC", AT[:, 0:8, :], [128, 8, 512], BF16)
        if ti == 0:
            ktl = [(0, NM, 0, False)]
        else:
            ktl = [(0, NM, 0, False)] + [(kt, 128, 0, False) for kt in range(1, kt0)] + \
                  [(kt0 + d_, 128, 128 * d_, True) for d_ in range(4)]
        acc = PS[2]
        nkt = len(ktl)

        def qk_exp(h, ki):
            g = h // 2
            kt, nk, q0, diag = ktl[ki]
            sp_ = PS[ki % 2]
            kc0 = 0 if kt == 0 else NM + 128 * (kt - 1)
            for m in range(2):
                pb = (h % 2) * 64 + 32 * m
                fw.op("pe", "matmul", out=sp_.p(m, (slice(0, nk), m, slice(q0, n))),
                      lhsT=KT[pb:pb + 32, g, kc0:kc0 + nk],
                      rhs=AT.p(16 + g, (slice(pb, pb + 32), 16 + g, slice(q0, n))),
                      start=True, stop=True, tile_position=(pb, 0))
            pt = PTb[ki % 3]
            fw.op("act", "activation", out=pt[0:nk, :, q0:n], in_=sp_[0:nk, :, q0:n], func=AF.Exp)
            if diag:
                fw.op("dve", "memset", ap=pt[64:128, :, q0:q0 + 64], constant=0.0)

        def pv(h, ki):
            kt, nk, q0, diag = ktl[ki]
            pt = PTb[ki % 3]
            for m in range(2):
                fw.op("pe", "matmul", out=acc.p(m, (slice(0, 64), m, slice(q0, n))),
                      lhsT=VR[0:nk, kt, 64 * h:64 * h + 64], rhs=pt[0:nk, m, q0:n],
                      start=(ki == 0), stop=(ki == nkt - 1), tile_position=(0, 0))
                fw.op("pe", "matmul", out=acc.p(m, (slice(64, 128), m, slice(q0, n))),
                      lhsT=ONESB[0:nk, :], rhs=pt[0:nk, m, q0:n],
                      start=(ki == 0), stop=(ki == nkt - 1), tile_position=(0, 64))

        def epi1(h):
            cp = [Fn(), Fn()]
            for m in range(2):
                fw.op("dve", "tensor_copy", out=cp[m][:, N], in_=acc.p(m, (ALLP, m, N)))
            r = [Fn(), Fn()]
            for m in range(2):
                fw.op("dve", "reciprocal", out=r[m][0:64, N], in_=cp[m][64:128, N])
                fw.op("dve", "tensor_tensor", out=r[m][0:64, N], in0=cp[m][0:64, N], in1=r[m][0:64, N], op=ALU.mult)
            o = Fn()
            fw.op("dve", "scalar_tensor_tensor", out=o[0:64, N], in0=r[1][0:64, N], scalar=NLAM[0:64, l:l + 1],
                  in1=r[0][0:64, N], op0=ALU.mult, op1=ALU.add)
            fw.op("dve", "tensor_tensor", out=r[0][0:64, N], in0=o[0:64, N], in1=o[0:64, N], op=ALU.mult)

            def epi2():
                E = PS[3]
                fw.op("pe", "matmul", out=E.p(0, (slice(0, 64), 0, N)), lhsT=ONE64[0:64, 0:64], rhs=r[0][0:64, N],
                      start=True, stop=True)
                fw.op("act", "activation", out=r[1][0:64, N], in_=E.p(0, (slice(0, 64), 0, N)), func=AF.Ln,
                      scale=1.0 / 64, bias=RMS_EPS)
                fw.op("act", "activation", out=r[1][0:64, N], in_=r[1][0:64, N], func=AF.Exp, scale=-0.5)
                fw.op("dve", "scalar_tensor_tensor", out=AT.p(8 + h, (slice(0, 64), 8 + h, N)), in0=o[0:64, N],
                      scalar=GS[:, l:l + 1], in1=r[1][0:64, N], op0=ALU.mult, op1=ALU.mult)
            return epi2

        pending = None
        defer_at = min(6, nkt - 1)
        for h in range(8):
            for ki in range(nkt + 1):
                if ki < nkt:
                    qk_exp(h, ki)
                if ki >= 1:
                    pv(h, ki - 1)
                if ki == defer_at and pending is not None:
                    pending()
                    pending = None
            if pending is not None:
                pending()
            pending = epi1(h)
        pending()

        if tp:
            tapout("ATT", AT[:, 8:16, :], [128, 8, 512], BF16)
        for dp in range(4):
            wao = wq.get(live=1)
            wlo = wq.get(live=2)
            wga = wq.get(live=3)
            wgl = wq.get(live=4)
            for di in range(2):
                dt = 2 * dp + di
                cs = slice(128 * di, 128 * di + 128)
                b1 = next_bank()
                chain(b1, lambda k: V(wao.ap[0:64, k, cs], wao.regs, "sb"),
                      rhs_fn=lambda k: AT.p(8 + k, (slice(0, 64), 8 + k, N)))
                b2 = next_bank()
                chain(b2, lambda k: V(wlo.ap[:, k, cs], wlo.regs, "sb"), rhs_fn=lambda k: AT.p(k, (ALLP, k, N)))
                b3 = next_bank()
                chain(b3, lambda k: V(wga.ap[:, k, cs], wga.regs, "sb"))
                sa = Fn()
                fw.op("act", "activation", out=sa[:, N], in_=bv(b3, ALLP, N), func=AF.Sigmoid, bias=pcol(l, C_BG + dt))
                b4 = next_bank()
                chain(b4, lambda k: V(wgl.ap[:, k, cs], wgl.regs, "sb"))
                sl = Fn()
                fw.op("act", "activation", out=sl[:, N], in_=bv(b4, ALLP, N), func=AF.Sigmoid, bias=pcol(l, C_BG + 8 + dt))
                fw.op("dve", "tensor_tensor", out=sa[:, N], in0=sa[:, N], in1=bv(b1, ALLP, N), op=ALU.mult)
                fw.op("dve", "tensor_tensor", out=sl[:, N], in0=sl[:, N], in1=bv(b2, ALLP, N), op=ALU.mult)
                fw.op("dve", "tensor_tensor", out=MT.p(dt, (ALLP, dt, N)), in0=sa[:, N], in1=sl[:, N], op=ALU.add)
        for dp in range(4):
            wo = wq.get(live=1)
            for di in range(2):
                dt = 2 * dp + di
                cs = slice(128 * di, 128 * di + 128)
                b = next_bank()
                chain(b, lambda k: V(wo.ap[:, k, cs], wo.regs, "sb"), rhs_fn=lambda k: MT.p(k, (ALLP, k, N)))
                yk = Y.p(dt, (ALLP, dt, N))
                fw.op("dve", "scalar_tensor_tensor", out=yk, in0=yk, scalar=float(alpha), in1=bv(b, ALLP, N),
                      op0=ALU.mult, op1=ALU.add)
        if tp:
            tapout("MT", MT[:, :, :], [128, 8, 512], BF16)
            tapout("Y1", Y[:, :, :], [128, 8, 512], F32)
        ln_fm(n, lambda k: pcol(l, C_LNG + k), lambda k: pcol(l, C_LNB + k), LN_EPS)
        if tp:
            tapout("H1", Y[:, :, :], [128, 8, 512], F32)

        if is_moe:
            b = next_bank()
            for k in range(8):
                fw.op("pe", "matmul", out=bv(b, slice(0, NE), N), lhsT=WRT[:, k, :], rhs=Y.p(k, (ALLP, k, N)),
                      start=(k == 0), stop=(k == 7))
            LGT = Fn()
            fw.op("dve", "tensor_copy", out=LGT[0:NE, N], in_=bv(b, slice(0, NE), N))
            b = next_bank()
            for sub in range(nsub):
                ns = min(128, n - 128 * sub)
                fw.op("pe", "transpose", out=bv(b, slice(0, ns), slice(8 * sub, 8 * sub + 8)),
                      in_=LGT[0:NE, 128 * sub:128 * sub + ns], identity=IDF[0:NE, 0:NE])
            for sub in range(nsub):
                ns = min(128, n - 128 * sub)
                P_ = slice(0, ns)
                fw.op("dve", "tensor_copy", out=LG[P_, sub, :], in_=bv(b, P_, slice(8 * sub, 8 * sub + 8)))
                fw.op("dve", "max", out=M8[P_, sub, :], in_=LG[P_, sub, :])
                fw.op("dve", "tensor_scalar", out=CMB[P_, sub, :], in0=LG[P_, sub, :], scalar1=M8[P_, sub, 1:2],
                      scalar2=None, op0=ALU.is_ge)
                fw.op("dve", "tensor_scalar", out=SM[P_, 0:1], in0=M8[P_, sub, 0:1], scalar1=-1.0, scalar2=None, op0=ALU.mult)
                fw.op("act", "activation", out=LG[P_, sub, :], in_=LG[P_, sub, :], func=AF.Exp, bias=SM[P_, 0:1])
                fw.op("dve", "tensor_tensor", out=CMB[P_, sub, :], in0=CMB[P_, sub, :], in1=LG[P_, sub, :], op=ALU.mult)
                fw.op("dve", "reduce_sum", out=SM[P_, 1:2], in_=CMB[P_, sub, :], axis=AX.X)
                fw.op("dve", "reciprocal", out=SM[P_, 2:3], in_=SM[P_, 1:2])
                fw.op("dve", "tensor_scalar", out=CMB[P_, sub, :], in0=CMB[P_, sub, :], scalar1=SM[P_, 2:3],
                      scalar2=None, op0=ALU.mult)
            b = next_bank()
            for sub in range(nsub):
                ns = min(128, n - 128 * sub)
                fw.op("pe", "transpose", out=bv(b, slice(0, NE), slice(128 * sub, 128 * sub + ns)),
                      in_=CMB[0:ns, sub, :], identity=IDF[0:ns, 0:ns])
            CT = Fp[NF]
            fw.op("dve", "tensor_copy", out=CT[0:NE, N], in_=bv(b, slice(0, NE), N))

        if nxt is not None:
            s2, ti2 = nxt
            oth = XBS[1] if XB is XBS[0] else XBS[0]
            fw.dma("sp", oth[:, :, 0:tiles[ti2][1]], hview(HB, s2, ti2))
        nexp = NE if is_moe else 1
        if is_moe:
            for k in range(8):
                yk = Y.p(k, (ALLP, k, N))
                fw.op("act", "activation", out=yk, in_=yk, func=AF.Copy, scale=float(alpha))
        for e in range(nexp):
            if is_moe:
                bc = next_bank()
                cte = Fn()
                fw.op("dve", "tensor_scalar", out=cte[0:NE, N], in0=CT[0:NE, N], scalar1=IDF[0:NE, e:e + 1],
                      scalar2=None, op0=ALU.mult)
                fw.op("pe", "matmul", out=bv(bc, ALLP, N), lhsT=ONE64[0:NE, :], rhs=cte[0:NE, N], start=True, stop=True)
                cb = CBT
                fw.op("act", "activation", out=cb[:, N], in_=bv(bc, ALLP, N), func=AF.Copy)
            for fp_ in range(NFT // 2):
                wg_ = wq.get(live=1)
                wu_ = wq.get(live=2)
                for fi in range(2):
                    f = 2 * fp_ + fi
                    cs = slice(128 * fi, 128 * fi + 128)
                    b1 = next_bank()
                    chain(b1, lambda k: V(wg_.ap[:, k, cs], wg_.regs, "sb"))
                    b2 = next_bank()
                    chain(b2, lambda k: V(wu_.ap[:, k, cs], wu_.regs, "sb"))
                    sg = Fn()
                    fw.op("act", "activation", out=sg[:, N], in_=bv(b1, ALLP, N), func=AF.Silu)
                    if is_moe:
                        fw.op("dve", "tensor_tensor", out=sg[:, N], in0=sg[:, N], in1=CBT[:, N], op=ALU.mult)
                    fw.op("dve", "tensor_tensor", out=AT.p(f, (ALLP, f, N)), in0=sg[:, N], in1=bv(b2, ALLP, N), op=ALU.mult)
            for fp_ in range(NFT // 2):
                wd_ = wq.get(live=1)
                for fi in range(2):
                    f = 2 * fp_ + fi
                    for dt in range(8):
                        fw.op("pe", "matmul", out=bv(dt, ALLP, N), lhsT=V(wd_.ap[:, fi, 128 * dt:128 * dt + 128], wd_.regs, "sb"),
                              rhs=AT.p(f, (ALLP, f, N)), start=(f == 0), stop=(f == NFT - 1))
            bank_rr[0] = 0
            for dt in range(8):
                yk = Y.p(dt, (ALLP, dt, N))
                if is_moe:
                    fw.op("dve", "tensor_tensor", out=yk, in0=yk, in1=bv(dt, ALLP, N), op=ALU.add)
                else:
                    fw.op("dve", "scalar_tensor_tensor", out=yk, in0=yk, scalar=float(alpha), in1=bv(dt, ALLP, N),
                          op0=ALU.mult, op1=ALU.add)
        bank_rr[0] = 0
        if tp:
            tapout("Y2", Y[:, :, :], [128, 8, 512], F32)
        ln_fm(n, lambda k: pcol(l, C_LNG + 8 + k), lambda k: pcol(l, C_LNB + 8 + k), LN_EPS)
        dbg_store(l + 1, s, ti, n)
        if not last_layer:
            store_h(s, ti, n)
        elif ti > 0:
            for sub in range(nsub):
                ns = min(128, n - 128 * sub)
                xl2 = [Fn(), Fn()]
                for half in range(2):
                    b = next_bank()
                    for q in range(4):
                        k = 4 * half + q
                        fw.op("pe", "transpose", out=bv(b, slice(0, ns), slice(128 * q, 128 * q + 128)),
                              in_=Y.p(k, (ALLP, k, slice(128 * sub, 128 * sub + ns))), identity=IDF[:])
                    fw.op("act", "activation", out=xl2[half][0:ns, :], in_=bv(b, slice(0, ns), slice(0, 512)),
                          func=AF.Copy)
                r0 = t0 - NM + 128 * sub
                for half in range(2):
                    fw.dma("act", OUT.v(OUT.h[s, r0:r0 + ns, 512 * half:512 * half + 512], s * NTL + ti), xl2[half][0:ns, :])

    nl = L if stop_after is None else stop_after
    for l in range(nl):
        wq.add(chunks_for(l) * (n_seq * NTL - (n_seq - 1)))
    steps_all = [(l_, s_, t_) for l_ in range(nl) for s_ in range(n_seq) for t_ in range(NTL) if not (s_ > 0 and t_ == 0)]
    nsteps = n_seq * NTL - (n_seq - 1)
    for l in range(nl):
        layer_setup(l)
        per_step = -(-len(cast_jobs.get(l + 1, [])) // max(nsteps - 1, 1))
        for s in range(n_seq):
            if s == 0:
                fw.op("dve", "memset", ap=HIST[:], constant=0.0)
                fw.op("dve", "memset", ap=HST[:], constant=0.0)
            for ti in range(NTL):
                if s > 0 and ti == 0:
                    fw.op("dve", "tensor_copy", out=HST[:], in_=HST0[:])
                    fw.op("dve", "tensor_copy", out=HIST[:], in_=HIST0[:])
                    continue
                if l + 1 < nl:
                    run_cast_jobs(l + 1, per_step)
                si = steps_all.index((l, s, ti))
                nxt = steps_all[si + 1][1:] if si + 1 < len(steps_all) else None
                tile_step(l, s, ti, last_layer=(l == L - 1), nxt=nxt)
                if nxt is not None:
                    xb_cur[0] = XBS[1] if xb_cur[0] is XBS[0] else XBS[0]
                    xb_loaded[0] = True
                if s == 0 and ti == 0:
                    fw.op("dve", "tensor_copy", out=HST0[:], in_=HST[:])
                    fw.op("dve", "tensor_copy", out=HIST0[:], in_=HIST[:])
        assert not cast_jobs.get(l + 1) or l + 1 >= nl
    outs = [OUT[:]] + ([DBG[:]] if DBG is not None else []) + [t[:] for t in TAPS.values()]
    fw.finish(outs)
    return nc, fw


_PROG = {}


def kernel(**inputs):
    n_cores = 8
    x = np.ascontiguousarray(inputs["x"], dtype=np.float32)
    B, S, _ = x.shape
    n_seq = B // n_cores
    depth = inputs["w_in"].shape[0]
    alpha = (2 * depth) ** 0.25
    key = (n_seq, S, depth)
    if key not in _PROG:
        _PROG[key] = build_program(n_seq, S, depth, alpha)[0]
    nc = _PROG[key]
    shared = {k: np.ascontiguousarray(inputs[k], dtype=np.float32) for k in WNAMES if k != "x"}
    in_maps = []
    for c in range(n_cores):
        m = dict(shared)
        m["x"] = x[c * n_seq:(c + 1) * n_seq]
        in_maps.append(m)
    res = run_bass_kernel_spmd(nc, in_maps, core_ids=list(range(n_cores)))
    return np.concatenate([r["out"] for r in res.results], axis=0).astype(np.float32)
```
